# Optimizing a Trainium2 kernel written in Bass

```python
import math
import jax, jax.numpy as jnp
from jax import lax
import numpy as np

D_MODEL = 2048
BATCH = 8
SEQ = 2048
DEPTH = 1

N_DIFF_HEADS = 8
DIFF_HEAD_DIM = 64
DIFF_V_DIM = 2 * DIFF_HEAD_DIM
DIFF_WIDTH = N_DIFF_HEADS * DIFF_V_DIM
N_RET_HEADS = 8
RET_QK_DIM = 64
RET_V_DIM = 128
RET_WIDTH = N_RET_HEADS * RET_V_DIM
MIX_WIDTH = DIFF_WIDTH + RET_WIDTH
COL_SPLITS = (
    N_DIFF_HEADS * 2 * DIFF_HEAD_DIM,
    N_DIFF_HEADS * 2 * DIFF_HEAD_DIM,
    DIFF_WIDTH,
    N_RET_HEADS * RET_QK_DIM,
    N_RET_HEADS * RET_QK_DIM,
    RET_WIDTH,
    RET_WIDTH,
)
IN_COLS = sum(COL_SPLITS)
Q_BLOCK = 128
RET_CHUNK = 128
ROPE_BASE = 10000.0
N_BUCKETS = 32
MAX_DISTANCE = 128
N_GROUPS = 4
EXPERTS_PER_GROUP = 8
N_EXPERTS = N_GROUPS * EXPERTS_PER_GROUP
TOP_K_INNER = 2
D_FF_EXPERT = 1024
EPS = 1e-6

kernel_name = "hymba_diffattn_retnet_hmoe_block"


def rms_norm(x, g):
    xf = x.astype(jnp.float32)
    y = xf * lax.rsqrt(jnp.mean(xf * xf, axis=-1, keepdims=True) + EPS)
    return (y * g.astype(jnp.float32)).astype(x.dtype)


def t5_bucket(dist):
    max_exact = N_BUCKETS // 2
    d = jnp.maximum(dist, 0)
    log_ratio = jnp.log(jnp.maximum(d, 1).astype(jnp.float32) / max_exact) / math.log(MAX_DISTANCE / max_exact)
    large = max_exact + (log_ratio * (N_BUCKETS - max_exact)).astype(jnp.int32)
    large = jnp.minimum(large, N_BUCKETS - 1)
    return jnp.where(d < max_exact, d, large)


def rotary(x, pos):
    d = x.shape[-1]
    inv_freq = ROPE_BASE ** (-jnp.arange(0, d, 2, dtype=jnp.float32) / d)
    ang = pos.astype(jnp.float32)[:, None] * inv_freq[None, :]
    cos = jnp.cos(ang)[None, :, None, :].astype(x.dtype)
    sin = jnp.sin(ang)[None, :, None, :].astype(x.dtype)
    x1, x2 = x[..., : d // 2], x[..., d // 2:]
    return jnp.concatenate([x1 * cos - x2 * sin, x1 * sin + x2 * cos], axis=-1)


def diff_attention(q, k, v, rel_bias, lam):
    S = q.shape[1]
    scale = DIFF_HEAD_DIM ** -0.5
    outs = []
    for blk in range(S // Q_BLOCK):
        s0, s1 = blk * Q_BLOCK, (blk + 1) * Q_BLOCK
        qb = q[:, s0:s1]
        kb, vb = k[:, :s1], v[:, :s1]
        logits = jnp.einsum('bqhcd,bkhcd->bhcqk', qb, kb).astype(jnp.float32) * scale
        dist = jnp.arange(s0, s1)[:, None] - jnp.arange(s1)[None, :]
        bias = jnp.transpose(rel_bias[t5_bucket(dist)], (2, 0, 1)).astype(jnp.float32)
        logits = logits + bias[None, :, None]
        logits = jnp.where((dist >= 0)[None, None, None], logits, -jnp.inf)
        p = jax.nn.softmax(logits, axis=-1)
        attn = (p[:, :, 0] - lam * p[:, :, 1]).astype(v.dtype)
        outs.append(jnp.einsum('bhqk,bkhd->bqhd', attn, vb))
    return jnp.concatenate(outs, axis=1)


def retention(q, k, v):
    B, S, H, dk = q.shape
    dv = v.shape[-1]
    C = RET_CHUNK
    NC = S // C
    log_g = jnp.log(1.0 - jnp.exp2(-5.0 - jnp.arange(H, dtype=jnp.float32)))
    n = jnp.arange(C, dtype=jnp.float32)
    diff = n[:, None] - n[None, :]
    inner_decay = jnp.where(diff[None] >= 0, jnp.exp(jnp.maximum(diff, 0.0)[None] * log_g[:, None, None]), 0.0).astype(q.dtype)
    cross_decay = jnp.exp((n[None] + 1.0) * log_g[:, None]).astype(q.dtype)
    key_decay = jnp.exp((C - 1.0 - n[None]) * log_g[:, None]).astype(q.dtype)
    chunk_decay = jnp.exp(C * log_g).astype(q.dtype)

    def to_chunks(t):
        return jnp.transpose(t.reshape(B, NC, C, H, t.shape[-1]), (1, 0, 3, 2, 4))

    def step(state, inp):
        qc, kc, vc = inp
        scores = jnp.einsum('bhnd,bhmd->bhnm', qc, kc) * inner_decay[None]
        inner = jnp.einsum('bhnm,bhme->bhne', scores, vc)
        cross = jnp.einsum('bhnd,bhde->bhne', qc, state) * cross_decay[None, :, :, None]
        new_state = state * chunk_decay[None, :, None, None] + jnp.einsum('bhmd,bhme->bhde', kc * key_decay[None, :, :, None], vc)
        return new_state, inner + cross

    state0 = jnp.zeros((B, H, dk, dv), q.dtype)
    _, ys = lax.scan(step, state0, (to_chunks(q), to_chunks(k), to_chunks(v)))
    return jnp.transpose(ys, (1, 0, 3, 2, 4)).reshape(B, S, H, dv)


def hierarchical_moe(h, w_group_router, b_group, w_inner_router, b_inner, w_gate, w_up, w_down):
    N = h.shape[0]
    group_logits = (h @ w_group_router).astype(jnp.float32) + b_group.astype(jnp.float32)
    group_probs = jax.nn.softmax(group_logits, axis=-1)
    g_idx = jnp.argmax(group_logits, axis=-1)
    p_group = jnp.take_along_axis(group_probs, g_idx[:, None], axis=1)
    inner_all = jnp.einsum('nd,gde->nge', h, w_inner_router).astype(jnp.float32)
    inner = jnp.take_along_axis(inner_all, g_idx[:, None, None], axis=1)[:, 0] + b_inner[g_idx].astype(jnp.float32)
    top_vals, top_idx = lax.top_k(inner, TOP_K_INNER)
    gates = p_group * jax.nn.softmax(top_vals, axis=-1)
    expert_id = (g_idx[:, None] * EXPERTS_PER_GROUP + top_idx).reshape(-1)
    order = jnp.argsort(expert_id)
    token = order // TOP_K_INNER
    xs = h[token]
    sizes = jnp.bincount(expert_id, length=N_EXPERTS).astype(jnp.int32)
    a = lax.ragged_dot(xs, w_gate, sizes)
    b = lax.ragged_dot(xs, w_up, sizes)
    ys = lax.ragged_dot(jax.nn.silu(a) * b, w_down, sizes)
    ys = ys * gates.reshape(-1)[order][:, None].astype(ys.dtype)
    return jnp.zeros((N, h.shape[1]), ys.dtype).at[token].add(ys)


def setup_inputs(seed: int = 0) -> dict:
    key = jax.random.key(seed)
    ks = jax.random.split(key, 20)
    f32 = jnp.float32
    nrm = lambda k, shape, s: jax.random.normal(k, shape, f32) * s
    return {
        "x": nrm(ks[0], (BATCH, SEQ, D_MODEL), 1.0),
        "rel_bias": nrm(ks[1], (N_BUCKETS, N_DIFF_HEADS), 0.5),
        "attn_norm_g": 1.0 + nrm(ks[2], (DEPTH, D_MODEL), 0.02),
        "w_in": nrm(ks[3], (DEPTH, D_MODEL, IN_COLS), D_MODEL ** -0.5),
        "lambda_q1": nrm(ks[4], (DEPTH, DIFF_HEAD_DIM), 0.1),
        "lambda_k1": nrm(ks[5], (DEPTH, DIFF_HEAD_DIM), 0.1),
        "lambda_q2": nrm(ks[6], (DEPTH, DIFF_HEAD_DIM), 0.1),
        "lambda_k2": nrm(ks[7], (DEPTH, DIFF_HEAD_DIM), 0.1),
        "diff_subln_g": 1.0 + nrm(ks[8], (DEPTH, DIFF_V_DIM), 0.02),
        "ret_gn_g": 1.0 + nrm(ks[9], (DEPTH, RET_WIDTH), 0.02),
        "w_out": nrm(ks[10], (DEPTH, MIX_WIDTH, D_MODEL), MIX_WIDTH ** -0.5),
        "ffn_norm_g": 1.0 + nrm(ks[11], (DEPTH, D_MODEL), 0.02),
        "w_group_router": nrm(ks[12], (DEPTH, D_MODEL, N_GROUPS), D_MODEL ** -0.5),
        "b_group": nrm(ks[13], (DEPTH, N_GROUPS), 0.01),
        "w_inner_router": nrm(ks[14], (DEPTH, N_GROUPS, D_MODEL, EXPERTS_PER_GROUP), D_MODEL ** -0.5),
        "b_inner": nrm(ks[15], (DEPTH, N_GROUPS, EXPERTS_PER_GROUP), 0.01),
        "w_gate_exp": nrm(ks[16], (DEPTH, N_EXPERTS, D_MODEL, D_FF_EXPERT), D_MODEL ** -0.5),
        "w_up_exp": nrm(ks[17], (DEPTH, N_EXPERTS, D_MODEL, D_FF_EXPERT), D_MODEL ** -0.5),
        "w_down_exp": nrm(ks[18], (DEPTH, N_EXPERTS, D_FF_EXPERT, D_MODEL), D_FF_EXPERT ** -0.5),
        "final_g": 1.0 + nrm(ks[19], (D_MODEL,), 0.02),
    }


def reference(x, rel_bias, attn_norm_g, w_in, lambda_q1, lambda_k1, lambda_q2, lambda_k2,
              diff_subln_g, ret_gn_g, w_out, ffn_norm_g, w_group_router, b_group,
              w_inner_router, b_inner, w_gate_exp, w_up_exp, w_down_exp, final_g):
    B, S, D = x.shape
    pos = jnp.arange(S)
    for l in range(DEPTH):
        h = rms_norm(x, attn_norm_g[l])
        proj = h @ w_in[l]
        cuts = list(np.cumsum(COL_SPLITS)[:-1])
        dq, dk_, dv_, rq, rk, rv, rg = jnp.split(proj, cuts, axis=-1)

        lam_init = 0.8 - 0.6 * math.exp(-0.3 * l)
        lam = (jnp.exp(jnp.sum(lambda_q1[l] * lambda_k1[l]).astype(jnp.float32))
               - jnp.exp(jnp.sum(lambda_q2[l] * lambda_k2[l]).astype(jnp.float32)) + lam_init)
        dq = dq.reshape(B, S, N_DIFF_HEADS, 2, DIFF_HEAD_DIM)
        dk_ = dk_.reshape(B, S, N_DIFF_HEADS, 2, DIFF_HEAD_DIM)
        dv_ = dv_.reshape(B, S, N_DIFF_HEADS, DIFF_V_DIM)
        a_out = diff_attention(dq, dk_, dv_, rel_bias, lam)
        a_out = rms_norm(a_out, diff_subln_g[l]) * (1.0 - lam_init)
        a_out = a_out.reshape(B, S, DIFF_WIDTH)

        rq = rotary(rq.reshape(B, S, N_RET_HEADS, RET_QK_DIM), pos)
        rk = rotary(rk.reshape(B, S, N_RET_HEADS, RET_QK_DIM), pos) * (RET_QK_DIM ** -0.5)
        rv = rv.reshape(B, S, N_RET_HEADS, RET_V_DIM)
        r = retention(rq, rk, rv).astype(jnp.float32)
        mu = jnp.mean(r, axis=-1, keepdims=True)
        var = jnp.mean(jnp.square(r - mu), axis=-1, keepdims=True)
        r = ((r - mu) * lax.rsqrt(var + EPS)).reshape(B, S, RET_WIDTH) * ret_gn_g[l].astype(jnp.float32)
        r_out = (jax.nn.silu(rg) * r.astype(x.dtype))

        mixed = jnp.concatenate([a_out.astype(x.dtype), r_out], axis=-1)
        x = x + mixed @ w_out[l]

        h2 = rms_norm(x, ffn_norm_g[l]).reshape(B * S, D)
        y = hierarchical_moe(h2, w_group_router[l], b_group[l], w_inner_router[l], b_inner[l],
                             w_gate_exp[l], w_up_exp[l], w_down_exp[l])
        x = x + y.reshape(B, S, D).astype(x.dtype)
    return rms_norm(x, final_g)
```

```python
import math
from contextlib import ExitStack

import numpy as np
import ml_dtypes
import concourse.bass as bass
import concourse.mybir as mybir
from concourse.bass_utils import run_bass_kernel_spmd

F32 = mybir.dt.float32
BF16 = mybir.dt.bfloat16
I32 = mybir.dt.int32
AF = mybir.ActivationFunctionType
ALU = mybir.AluOpType
AX = mybir.AxisListType

S = 2048
D = 2048
NH = 8
CAP = 256
NE = 32
NSLOT = NE * CAP
FF = 1024
EPS = 1e-6
NBLK = 40
NEG = -30000.0


class Buf:
    def __init__(self, name):
        self.name = name
        self.w = None
        self.r = {}
        self.dsem = None
        self.dcnt = 0


class DSem:
    def __init__(self, sem):
        self.sem = sem
        self.cnt = 0


class Ctx:
    def share(self, bufs, name):
        ds = DSem(self.nc.alloc_semaphore("d_" + name))
        self.dbufs.append(ds)
        for b in bufs:
            b.dsem = ds

    def __init__(self, nc):
        self.nc = nc
        self.engs = {"pe": nc.tensor, "act": nc.scalar, "dve": nc.vector,
                     "pool": nc.gpsimd, "sp": nc.sync}
        self.sem = {e: nc.alloc_semaphore("s_" + e) for e in self.engs}
        self.cnt = {e: 0 for e in self.engs}
        self.seen = {e: {} for e in self.engs}
        self.dbufs = []

    def buf(self, name):
        return Buf(name)

    def _wait(self, e, ev):
        sem, val = ev
        key = id(sem)
        if e == "pe" and sem is self.sem["pe"]:
            return
        if self.seen[e].get(key, 0) >= val:
            return
        self.engs[e].wait_ge(sem, val)
        self.seen[e][key] = val

    def _deps(self, e, reads, writes, join=False):
        for b in reads:
            if b.w is not None:
                self._wait(e, b.w)
        for b in writes:
            if b.w is not None and not join:
                self._wait(e, b.w)
            for ev in b.r.values():
                self._wait(e, ev)

    def _mark(self, ev, reads, writes):
        for b in reads:
            b.r[id(ev[0])] = ev
        for b in writes:
            b.w = ev
            b.r = {}

    def op(self, e, reads, writes, fn):
        self._deps(e, reads, writes)
        inst = fn(self.engs[e])
        self.cnt[e] += 1
        inst.then_inc(self.sem[e], 1)
        self._mark((self.sem[e], self.cnt[e]), reads, writes)

    def dma(self, q, out, in_, reads, writes, join=False, fn=None, sem_buf=None, **kw):
        (wb0,) = writes
        self._deps(q, reads, writes, join=join)
        wb = sem_buf if sem_buf is not None else wb0
        if wb.dsem is None:
            wb.dsem = DSem(self.nc.alloc_semaphore("d_" + wb.name))
            self.dbufs.append(wb.dsem)
        if fn is None:
            inst = self.engs[q].dma_start(out=out, in_=in_, **kw)
        else:
            inst = fn(self.engs[q])
        wb.dsem.cnt += 1
        inst.then_inc(wb.dsem.sem, 16)
        self._mark((wb.dsem.sem, 16 * wb.dsem.cnt), reads, writes)

    def barrier(self):
        for e in self.engs:
            for e2 in self.engs:
                if e2 != e and self.cnt[e2] > 0:
                    self._wait(e, (self.sem[e2], self.cnt[e2]))
            for ds in self.dbufs:
                if ds.cnt:
                    self._wait(e, (ds.sem, 16 * ds.cnt))


def _t5_onehot():
    d = np.arange(-127, 256)
    dd = np.maximum(d, 0)
    lr = (np.log(np.maximum(dd, 1).astype(np.float32) / np.float32(16)) /
          np.float32(math.log(128 / 16))).astype(np.float32)
    large = 16 + (lr * np.float32(16)).astype(np.int32)
    large = np.minimum(large, 31)
    bucket = np.where(dd < 16, dd, large)
    oh = np.zeros((33, 383), np.float32)
    for j in range(383):
        if d[j] < 0:
            oh[32, j] = 1.0
        else:
            oh[bucket[j], j] = 1.0
    return oh


def _consts():
    c = {}
    c["ident_bf"] = np.eye(128, dtype=np.float32).astype(ml_dtypes.bfloat16)
    c["ident_f"] = np.eye(128, dtype=np.float32)
    tp = np.arange(128)
    c["lstrict"] = (tp[:, None] < tp[None, :]).astype(np.float32).astype(ml_dtypes.bfloat16)
    c["iota_e"] = np.tile(np.arange(32, dtype=np.float32)[None], (128, 1))
    c["oh"] = _t5_onehot()
    inv = (10000.0 ** (-np.arange(0, 64, 2, dtype=np.float32) / np.float32(64))).astype(np.float32)
    ang = np.arange(S, dtype=np.float32)[:, None] * inv[None, :]
    cos = np.cos(ang).astype(np.float32).T
    sin = np.sin(ang).astype(np.float32).T
    cos2 = np.zeros((128, S), np.float32)
    sin2 = np.zeros((128, S), np.float32)
    for p in range(128):
        w = p % 64
        cos2[p] = cos[w % 32]
        sin2[p] = -sin[w] if w < 32 else sin[w - 32]
    c["cos2"] = cos2
    c["sin2"] = sin2
    hh = np.arange(8, dtype=np.float32)
    log_g = np.log(np.float32(1.0) - np.exp2(np.float32(-5.0) - hh)).astype(np.float32)
    t = np.arange(S, dtype=np.float32)
    lg64 = log_g.astype(np.float64)
    tm = (np.arange(S) % 512).astype(np.float64)
    qd = np.exp(tm[None, :] * lg64[:, None])
    qdec = np.zeros((4, 128, S), np.float32)
    for hp in range(4):
        qdec[hp, 0:64] = qd[2 * hp][None, :]
        qdec[hp, 64:128] = qd[2 * hp + 1][None, :]
    c["qdec"] = qdec
    kk = np.arange(128, dtype=np.float64)
    mm = np.arange(-3, 16, dtype=np.float64)
    bcj = 0.125 * np.exp(-kk[:, None, None] * lg64[None, :, None] + 128.0 * mm[None, None, :] * lg64[None, :, None])
    c["bcj"] = bcj.astype(np.float32)
    c["mqk"] = (kk[:, None] <= kk[None, :]).astype(np.float32)
    return c


def _fm_cols():
    blocks = []
    for h in range(8):
        blocks.append(list(range(h * 128, (h + 1) * 128)))
    for h in range(8):
        blocks.append(list(range(1024 + h * 128, 1024 + (h + 1) * 128)))
    swap = [(d + 32) % 64 for d in range(64)]
    for hp in range(4):
        for base in (3072, 3584):
            a, b = [], []
            for sub in range(2):
                h = 2 * hp + sub
                a += [base + h * 64 + d for d in range(64)]
                b += [base + h * 64 + swap[d] for d in range(64)]
            blocks.append(a)
            blocks.append(b)
    for h in range(8):
        blocks.append(list(range(5120 + h * 128, 5120 + (h + 1) * 128)))
    assert len(blocks) == NBLK
    return blocks


def _layout_weights(inp):
    w_in = np.asarray(inp["w_in"][0], dtype=np.float32)
    blocks = _fm_cols()
    w4 = w_in.reshape(16, 128, 6144)
    wfm = np.empty((NBLK, 128, 16, 128), np.float32)
    for i, cols in enumerate(blocks):
        wfm[i] = w4[:, :, cols].transpose(1, 0, 2)
    vcols = list(range(2048, 3072)) + list(range(4096, 5120))
    wv = w4[:, :, vcols]
    wtm = np.ascontiguousarray(wv.reshape(16, 128, 4, 512).transpose(2, 1, 0, 3))
    wr = np.concatenate([np.asarray(inp["w_group_router"][0])] +
                        [np.asarray(inp["w_inner_router"][0][g]) for g in range(4)], axis=1)
    wr = np.ascontiguousarray(wr.reshape(16, 128, 36).transpose(1, 0, 2)).astype(np.float32)
    rb = np.concatenate([np.asarray(inp["b_group"][0]).reshape(1, 4),
                         np.asarray(inp["b_inner"][0]).reshape(1, 32)], axis=1).astype(np.float32)
    d = {
        "wfm": wfm.reshape(NBLK, 128, 2048),
        "wtm": wtm,
        "wout": np.ascontiguousarray(inp["w_out"][0], dtype=np.float32),
        "wg": np.ascontiguousarray(inp["w_gate_exp"][0], dtype=np.float32),
        "wu": np.ascontiguousarray(inp["w_up_exp"][0], dtype=np.float32),
        "wd": np.ascontiguousarray(inp["w_down_exp"][0], dtype=np.float32),
        "wr": wr,
        "rbias": rb,
        "rel_bias": np.ascontiguousarray(inp["rel_bias"], dtype=np.float32),
        "g_attn": np.asarray(inp["attn_norm_g"][0], np.float32).reshape(1, D),
        "g_ffn": np.asarray(inp["ffn_norm_g"][0], np.float32).reshape(1, D),
        "g_fin": np.asarray(inp["final_g"], np.float32).reshape(1, D),
        "lq1": np.asarray(inp["lambda_q1"], np.float32).reshape(1, 64),
        "lk1": np.asarray(inp["lambda_k1"], np.float32).reshape(1, 64),
        "lq2": np.asarray(inp["lambda_q2"], np.float32).reshape(1, 64),
        "lk2": np.asarray(inp["lambda_k2"], np.float32).reshape(1, 64),
        "gsub": np.ascontiguousarray(np.asarray(inp["diff_subln_g"][0], np.float32).reshape(128, 1)),
        "gng": np.ascontiguousarray(np.asarray(inp["ret_gn_g"][0], np.float32).reshape(8, 128).T),
    }
    return d


IN_SPECS = [
    ("x", [S, D], F32), ("wfm", [NBLK, 128, 2048], F32), ("wtm", [4, 128, 16, 512], F32),
    ("wout", [2048, 2048], F32), ("wg", [NE, D, FF], F32), ("wu", [NE, D, FF], F32),
    ("wd", [NE, FF, D], F32), ("wr", [128, 16, 36], F32), ("rbias", [1, 36], F32),
    ("rel_bias", [32, 8], F32), ("g_attn", [1, D], F32), ("g_ffn", [1, D], F32),
    ("g_fin", [1, D], F32), ("lq1", [1, 64], F32), ("lk1", [1, 64], F32), ("lq2", [1, 64], F32),
    ("lk2", [1, 64], F32), ("gsub", [128, 1], F32), ("gng", [128, 8], F32),
    ("ident_bf", [128, 128], BF16), ("ident_f", [128, 128], F32), ("lstrict", [128, 128], BF16),
    ("iota_e", [128, 32], F32), ("oh", [33, 383], F32), ("cos2", [128, S], F32),
    ("sin2", [128, S], F32), ("qdec", [4, 128, S], F32), ("bcj", [128, 8, 19], F32), ("mqk", [128, 128], F32),
]


def build(stop_after=None, debug=False):
    nc = bass.Bass("TRN2", target_bir_lowering=False)
    cx = Ctx(nc)
    A = {}
    for name, shape, dt in IN_SPECS:
        A[name] = nc.dram_tensor(name, shape, dt, kind="ExternalInput").ap()
    out_d = nc.dram_tensor("out", [S, D], F32, kind="ExternalOutput").ap()
    dk = "ExternalOutput" if debug else "Internal"
    QK = nc.dram_tensor("QK", [NBLK, 128, S], BF16, kind=dk).ap()
    VT = nc.dram_tensor("VT", [S, 2048], BF16, kind=dk).ap()
    X1 = nc.dram_tensor("X1", [S, D], F32, kind=dk).ap()
    MTD = nc.dram_tensor("MTD", [128, 16, S], BF16, kind=dk).ap() if debug else None
    XS = nc.dram_tensor("XS", [NSLOT, D], BF16, kind="Internal").ap()
    YS = nc.dram_tensor("YS", [NSLOT, D], BF16, kind="Internal").ap()
    fscr_t = nc.dram_tensor("FSCR", [8 * 128 * 383], F32, kind="Internal")
    RD = nc.dram_tensor("RD", [S, 8], F32, kind=dk).ap() if debug else None

    bX = cx.buf("x_in")
    bOut = cx.buf("out")
    bQK = [cx.buf("QK%d" % i) for i in range(NBLK)]
    bVT = cx.buf("VT")
    bX1 = cx.buf("X1")
    bXS = cx.buf("XS")
    bYS = cx.buf("YS")
    bFS = cx.buf("FSCR")
    bW = cx.buf("wdram")

    es0 = ExitStack()

    def sb(es, name, shape, dt=F32):
        return es.enter_context(nc.sbuf_tensor("sb_" + name, shape, dt)).ap()

    ps_all = nc.alloc_psum_tensor("ps_all", [128, 8, 512], F32).ap()
    pb = [ps_all[:, i, :] for i in range(8)]
    bpb = [cx.buf("pb%d" % i) for i in range(8)]

    ident_bf = sb(es0, "ident_bf", [128, 128], BF16)
    ident_f = sb(es0, "ident_f", [128, 128], F32)
    ones_bf = sb(es0, "ones_bf", [128, 128], BF16)
    ones_f = sb(es0, "ones_f", [128, 128], F32)
    eps_t = sb(es0, "eps_t", [128, 1], F32)
    bC = cx.buf("consts")
    cx.dma("sp", ident_bf[:, :], A["ident_bf"][:, :], [bW], [bC])
    cx.dma("sp", ident_f[:, :], A["ident_f"][:, :], [bW], [bC], join=True)
    cx.op("dve", [], [bC], lambda e: e.memset(ones_bf[:, :], 1.0))
    cx.op("dve", [], [bC], lambda e: e.memset(ones_f[:, :], 1.0 / 128))
    cx.op("dve", [], [bC], lambda e: e.memset(eps_t[:, :], EPS))
    gates = sb(es0, "gates", [128, 16, 2], F32)
    rows_i = sb(es0, "rows_i", [128, 16, 2], I32)
    bGates = cx.buf("gates")
    bRows = cx.buf("rows")

    esA = ExitStack()
    HT = sb(esA, "HT", [128, 16, S], BF16)
    bHT = [cx.buf("HTlo"), cx.buf("HThi")]
    MT = HT
    bMT = cx.buf("MT")
    BT = sb(esA, "BT", [128, 8, 256], F32)
    cfar = sb(esA, "cfar", [128, 8], F32)
    nlam = sb(esA, "nlam", [128, 1], F32)
    g08 = sb(esA, "g08", [128, 1], F32)
    gng = sb(esA, "gng", [128, 8], F32)
    bBT = cx.buf("BT")
    bLam = cx.buf("lam")
    bSm = cx.buf("smallA")
    cx.dma("sp", g08[:, :], A["gsub"][:, :], [bW], [bSm])
    cx.dma("sp", gng[:, :], A["gng"][:, :], [bW], [bSm], join=True)
    cx.op("dve", [bSm], [bSm], lambda e: e.tensor_scalar(out=g08[:, :], in0=g08[:, :], scalar1=0.8, scalar2=None, op0=ALU.mult))

    esP0 = ExitStack()
    es = esP0
    if True:
        lv = sb(es, "lv", [128, 4, 64], F32)
        bl = cx.buf("lv")
        for i, nm in enumerate(["lq1", "lk1", "lq2", "lk2"]):
            cx.dma("sp", lv[:, i, :], A[nm].to_broadcast([128, 64]), [bW], [bl], join=(i > 0))
        pr = sb(es, "lpr", [128, 2, 64], F32)
        sm = sb(es, "lsm", [128, 4], F32)
        cx.op("dve", [bl], [bl], lambda e: e.tensor_tensor(out=pr[:, 0, :], in0=lv[:, 0, :], in1=lv[:, 1, :], op=ALU.mult))
        cx.op("dve", [bl], [bl], lambda e: e.tensor_tensor(out=pr[:, 1, :], in0=lv[:, 2, :], in1=lv[:, 3, :], op=ALU.mult))
        cx.op("dve", [bl], [bl], lambda e: e.reduce_sum(out=sm[:, 0:2], in_=pr[:, :, :], axis=AX.X))
        cx.op("act", [bl], [bl], lambda e: e.activation(out=sm[:, 2:4], in_=sm[:, 0:2], func=AF.Exp))
        cx.op("dve", [bl], [bLam], lambda e: e.tensor_tensor(out=nlam[:, :], in0=sm[:, 3:4], in1=sm[:, 2:3], op=ALU.subtract))
        cx.op("dve", [bLam], [bLam], lambda e: e.tensor_scalar(out=nlam[:, :], in0=nlam[:, :], scalar1=-0.2, scalar2=None, op0=ALU.add))
        rbe = sb(es, "rbe", [33, 8], F32)
        rbx = sb(es, "rbx", [33, 8, 128], F32)
        oh = sb(es, "oh", [33, 383], F32)
        ft = [sb(es, "ft%d" % i, [128, 383], F32) for i in range(8)]
        brb = cx.buf("rbe")
        cx.op("dve", [], [brb], lambda e: e.memset(rbe[:, :], NEG))
        cx.dma("sp", rbe[0:32, :], A["rel_bias"][:, :], [bW], [brb])
        cx.dma("sp", oh[:, :], A["oh"][:, :], [bW], [brb], join=True)
        cx.op("dve", [brb], [brb], lambda e: e.tensor_copy(out=rbx[:, :, :], in_=rbe[:, :].unsqueeze(2).to_broadcast([33, 8, 128])))
        bft = [cx.buf("ft%d" % i) for i in range(8)]
        fs_ap = fscr_t.ap().rearrange("(h p j) -> h p j", h=8, p=128)
        for h in range(8):
            cx.op("pe", [brb], [bpb[h % 2]], lambda e: e.matmul(pb[h % 2][:, 0:383], lhsT=rbx[:, h, :], rhs=oh[:, :], start=True, stop=True))
            if h % 2 == 0:
                cx.op("act", [bpb[h % 2]], [bft[h]], lambda e: e.activation(out=ft[h][:, :], in_=pb[h % 2][:, 0:383], func=AF.Copy))
            else:
                cx.op("dve", [bpb[h % 2]], [bft[h]], lambda e: e.tensor_copy(out=ft[h][:, :], in_=pb[h % 2][:, 0:383]))
            cx.op("dve", [bft[h]], [bBT], lambda e: e.tensor_copy(out=cfar[:, h:h + 1], in_=ft[h][:, 382:383]))
            cx.dma("pool", fs_ap[h], ft[h][:, :], [bft[h]], [bFS])
            toe = bass.AP(tensor=fscr_t, offset=h * 128 * 383 + 127, ap=[[382, 128], [1, 256]])
            cx.dma("pool", BT[:, h, :], toe, [bFS], [bBT])

    with ExitStack() as es:
        xt = [sb(es, "xt%d" % i, [128, D], F32) for i in range(3)]
        hb = [sb(es, "hb%d" % i, [128, D], BF16) for i in range(3)]
        junk = sb(es, "junk", [128, D], BF16)
        grep = sb(es, "grep", [128, D], F32)
        stt = [sb(es, "stt%d" % i, [128, 4], F32) for i in range(3)]
        bxt = [cx.buf("xt%d" % i) for i in range(3)]
        bhb = [cx.buf("hb%d" % i) for i in range(3)]
        bjunk = cx.buf("junk")
        bgrep = cx.buf("grep")
        bst = [cx.buf("stt%d" % i) for i in range(3)]
        cx.dma("act", grep[:, :], A["g_attn"].to_broadcast([128, D]), [bW], [bgrep])
        def p1_front(t):
            s = t % 3
            cx.dma("sp", xt[s][:, :], A["x"][t * 128:(t + 1) * 128, :], [bX], [bxt[s]])
            cx.op("act", [bxt[s]], [bjunk, bst[s]], lambda e: e.activation(out=junk[:, :], in_=xt[s][:, :], func=AF.Square, accum_out=stt[s][:, 0:1]))
            cx.op("act", [bst[s], bC], [bst[s]], lambda e: e.activation(out=stt[s][:, 1:2], in_=stt[s][:, 0:1], func=AF.Ln, scale=1.0 / D, bias=eps_t[:, 0:1]))
            cx.op("act", [bst[s]], [bst[s]], lambda e: e.activation(out=stt[s][:, 2:3], in_=stt[s][:, 1:2], func=AF.Exp, scale=-0.5))
            cx.op("dve", [bxt[s], bst[s], bgrep], [bhb[s]], lambda e: e.scalar_tensor_tensor(out=hb[s][:, :], in0=xt[s][:, :], scalar=stt[s][:, 2:3], in1=grep[:, :], op0=ALU.mult, op1=ALU.mult))

        def p1_back(t):
            s = t % 3
            for half in range(2):
                bk = 2 * (t % 2) + half
                pbf = pb[bk].bitcast(BF16)
                for k in range(8):
                    kc = half * 8 + k
                    cx.op("pe", [bhb[s], bC], [bpb[bk]], lambda e: e.transpose(pbf[:, k * 128:(k + 1) * 128], hb[s][:, kc * 128:(kc + 1) * 128], ident_bf[:, :]))
                dst = HT[:, half * 8:(half + 1) * 8, t * 128:(t + 1) * 128]
                srcv = pbf[:, :].rearrange("p (k c) -> p k c", k=8)
                if half == 0:
                    cx.op("act", [bpb[bk]], [bHT[0]], lambda e: e.activation(out=dst, in_=srcv, func=AF.Copy))
                else:
                    cx.op("dve", [bpb[bk]], [bHT[1]], lambda e: e.tensor_copy(out=dst, in_=srcv))

        p1_front(0)
        for t in range(16):
            if t + 1 < 16:
                p1_front(t + 1)
            p1_back(t)
        cx.barrier()
    esP0.close()

    with ExitStack() as es:
        zt = sb(es, "zt", [128, 4, D], BF16)
        bzt = cx.buf("zt")
        cx.op("pool", [], [bzt], lambda e: e.memset(zt[:, :, :], 0.0))
        XSz = XS.rearrange("(c p r) d -> c p r d", p=128, r=4)
        for c in range(NSLOT // 512):
            cx.dma("act", XSz[c], zt[:, :, :], [bzt], [bXS], join=True)
        wsl = [sb(es, "wsl%d" % i, [128, 2048], BF16) for i in range(4)]
        stg = [sb(es, "stg%d" % i, [128, S], BF16) for i in range(2)]
        bws = [cx.buf("wsl%d" % i) for i in range(4)]
        bstg = [cx.buf("stg%d" % i) for i in range(2)]
        n = 0
        for blk in range(NBLK):
            ws = wsl[blk % 4]
            cx.dma("pool", ws[:, :], A["wfm"][blk], [bW], [bws[blk % 4]])
            for tc in range(4):
                bk = n % 4
                for kc in range(16):
                    cx.op("pe", [bws[blk % 4]] + bHT, [bpb[bk]], lambda e: e.matmul(pb[bk][:, :], lhsT=ws[:, kc * 128:(kc + 1) * 128], rhs=HT[:, kc, tc * 512:(tc + 1) * 512], start=(kc == 0), stop=(kc == 15)))
                dst = stg[blk % 2][:, tc * 512:(tc + 1) * 512]
                if n % 2 == 0:
                    cx.op("act", [bpb[bk]], [bstg[blk % 2]], lambda e: e.activation(out=dst, in_=pb[bk][:, :], func=AF.Copy))
                else:
                    cx.op("dve", [bpb[bk]], [bstg[blk % 2]], lambda e: e.tensor_copy(out=dst, in_=pb[bk][:, :]))
                n += 1
            cx.dma("sp", QK[blk], stg[blk % 2][:, :], [bstg[blk % 2]], [bQK[blk]], sem_buf=bstg[blk % 2])
        wtb = [sb(es, "wtb%d" % i, [128, 16, 512], BF16) for i in range(2)]
        st2 = [sb(es, "st2%d" % i, [128, 512], BF16) for i in range(2)]
        bwt = [cx.buf("wtb%d" % i) for i in range(2)]
        bst2 = [cx.buf("st2%d" % i) for i in range(2)]
        n = 0
        for cb in range(4):
            wt = wtb[cb % 2]
            for hf in range(2):
                cx.dma("pool", wt[:, hf * 8:(hf + 1) * 8, :], A["wtm"][cb, :, hf * 8:(hf + 1) * 8, :], [bW], [bwt[cb % 2]], join=(hf > 0))
            for t in range(16):
                bk = 4 + n % 4
                for kc in range(16):
                    cx.op("pe", [bwt[cb % 2]] + bHT, [bpb[bk]], lambda e: e.matmul(pb[bk][:, :], lhsT=HT[:, kc, t * 128:(t + 1) * 128], rhs=wt[:, kc, :], start=(kc == 0), stop=(kc == 15)))
                if n % 2 == 0:
                    cx.op("act", [bpb[bk]], [bst2[n % 2]], lambda e: e.activation(out=st2[n % 2][:, :], in_=pb[bk][:, :], func=AF.Copy))
                else:
                    cx.op("dve", [bpb[bk]], [bst2[n % 2]], lambda e: e.tensor_copy(out=st2[n % 2][:, :], in_=pb[bk][:, :]))
                cx.dma("act", VT[t * 128:(t + 1) * 128, cb * 512:(cb + 1) * 512], st2[n % 2][:, :], [bst2[n % 2]], [bVT], join=True, sem_buf=bst2[n % 2])
                n += 1
        cx.barrier()

    if stop_after == "P2":
        return _finish(nc, cx, es0, esA, out_d, bOut)

    with ExitStack() as es:
        qT = [sb(es, "qT%d" % i, [128, S], BF16) for i in range(2)]
        kz = [[sb(es, "kz%d_%d" % (i, c), [128, S], BF16) for c in range(2)] for i in range(2)]
        vt = [sb(es, "vt%d" % i, [128, 16, 128], BF16) for i in range(2)]
        PT = [sb(es, "PT%d" % i, [128, 2, 256], BF16) for i in range(4)]
        tmpn = [sb(es, "tmpn%d" % i, [128, 2, 256], F32) for i in range(2)]
        fa = [sb(es, "fa%d" % i, [128, 2, 256], F32) for i in range(2)]
        fb = [sb(es, "fb%d" % i, [128, 256], F32) for i in range(2)]
        fc = [sb(es, "fc%d" % i, [128, 256], F32) for i in range(2)]
        fd = [sb(es, "fd%d" % i, [128, 256], F32) for i in range(2)]
        bq = [cx.buf("q%d" % i) for i in range(2)]
        bk_ = [cx.buf("k%d" % i) for i in range(2)]
        bv = [cx.buf("v%d" % i) for i in range(2)]
        bPT = [cx.buf("PT%d" % i) for i in range(4)]
        btn = [cx.buf("tmpn%d" % i) for i in range(2)]
        bfa = [cx.buf("fa%d" % i) for i in range(2)]
        bfb = [cx.buf("fb%d" % i) for i in range(2)]
        bfc = [cx.buf("fc%d" % i) for i in range(2)]
        bfd = [cx.buf("fd%d" % i) for i in range(2)]
        VTv = VT.rearrange("(t p) c -> p t c", p=128)
        p3 = [pb[i].rearrange("p (c q) -> p c q", c=2) for i in range(8)]
        bslot = [cx.buf("sslot%d" % i) for i in range(4)]
        st = {"s": 0, "near": 0, "pend": None, "pt": 0}

        for i in range(2):
            cx.op("pool", [], [bk_[i]], lambda e: e.memset(kz[i][0][64:128, :], 0.0))
            cx.op("pool", [], [bk_[i]], lambda e: e.memset(kz[i][1][0:64, :], 0.0))

        def load_head(h):
            s = h % 2
            cx.dma("sp", qT[s][:, :], QK[h], [bQK[h]], [bq[s]])
            cx.dma("sp", kz[s][0][0:64, :], QK[8 + h][0:64, :], [bQK[8 + h]], [bk_[s]])
            cx.dma("sp", kz[s][1][64:128, :], QK[8 + h][64:128, :], [bQK[8 + h]], [bk_[s]], join=True)
            cx.dma("act", vt[s][:, :, :], VTv[:, :, h * 128:(h + 1) * 128], [bVT], [bv[s]])

        load_head(0)
        chn = 0
        for h in range(NH):
            s = h % 2
            if h + 1 < NH:
                load_head(h + 1)
            for ch in range(8):
                q0 = ch * 256
                nj = 2 * ch + 2
                par = chn % 2
                ot, su = p3[4 + par], p3[6 + par]
                bot_, bsu = bpb[4 + par], bpb[6 + par]
                sbank = {}

                def s_mm(j):
                    co = max(0, j * 128 - q0)
                    slot = st["s"] % 4
                    st["s"] += 1
                    sbank[j] = slot
                    for c in range(2):
                        cx.op("pe", [bq[s], bk_[s]], [bpb[slot]], lambda e: e.matmul(
                            p3[slot][:, c, co:256], lhsT=kz[s][c][:, j * 128:(j + 1) * 128],
                            rhs=qT[s][:, q0 + co:q0 + 256], start=True, stop=True))

                LA = 3
                for jj in range(min(LA, nj)):
                    s_mm(jj)
                for j in range(nj):
                    if j + LA < nj:
                        s_mm(j + LA)
                    co = max(0, j * 128 - q0)
                    ne = min(j * 128 + 256, q0 + 256) - q0
                    fs = max(ne, co)
                    slot = sbank[j]
                    pt = PT[st["pt"] % 4]
                    bpt = bPT[st["pt"] % 4]
                    st["pt"] += 1
                    sview = p3[slot]
                    if ne > co:
                        w = ne - co
                        b0 = q0 + co - j * 128
                        tn = tmpn[st["near"] % 2]
                        btn_ = btn[st["near"] % 2]
                        st["near"] += 1
                        cx.op("dve", [bpb[slot], bBT], [btn_], lambda e: e.scalar_tensor_tensor(
                            out=tn[:, :, 0:w], in0=sview[:, :, co:ne], scalar=0.125,
                            in1=BT[:, h, b0:b0 + w].unsqueeze(1).to_broadcast([128, 2, w]), op0=ALU.mult, op1=ALU.add))
                        cx.op("act", [btn_], [bpt], lambda e: e.activation(out=pt[:, :, co:ne], in_=tn[:, :, 0:w], func=AF.Exp))
                    if fs < 256:
                        cx.op("act", [bpb[slot], bBT], [bpt], lambda e: e.activation(
                            out=pt[:, :, fs:256], in_=sview[:, :, fs:256], func=AF.Exp, scale=0.125, bias=cfar[:, h:h + 1]))
                    if co == 0:
                        ptf = pt[:, :, :].rearrange("p c q -> p (c q)")
                        cx.op("pe", [bv[s], bpt], [bot_], lambda e: e.matmul(
                            pb[4 + par][:, :], lhsT=vt[s][:, j, :], rhs=ptf, start=(j == 0), stop=(j == nj - 1)))
                        cx.op("pe", [bC, bpt], [bsu], lambda e: e.matmul(
                            pb[6 + par][:, :], lhsT=ones_bf[:, :], rhs=ptf, start=(j == 0), stop=(j == nj - 1)))
                    else:
                        for c in range(2):
                            cx.op("pe", [bv[s], bpt], [bot_], lambda e: e.matmul(
                                ot[:, c, co:256], lhsT=vt[s][:, j, :], rhs=pt[:, c, co:256], start=(j == 0), stop=(j == nj - 1 and c == 1)))
                            cx.op("pe", [bC, bpt], [bsu], lambda e: e.matmul(
                                su[:, c, co:256], lhsT=ones_bf[:, :], rhs=pt[:, c, co:256], start=(j == 0), stop=(j == nj - 1 and c == 1)))
                    if j == min(1, nj - 1) and st["pend"] is not None:
                        st["pend"]()
                        st["pend"] = None
                fa_, fb_, fc_, fd_ = fa[par], fb[par], fc[par], fd[par]
                cx.op("dve", [bsu], [bfa[par]], lambda e: e.reciprocal(out=fa_[:, :, :], in_=su[:, :, :]))
                cx.op("dve", [bot_, bfa[par]], [bfa[par]], lambda e: e.tensor_tensor(out=fa_[:, :, :], in0=ot[:, :, :], in1=fa_[:, :, :], op=ALU.mult))
                cx.op("dve", [bfa[par], bLam], [bfb[par]], lambda e: e.scalar_tensor_tensor(
                    out=fb_[:, :], in0=fa_[:, 1, :], scalar=nlam[:, 0:1], in1=fa_[:, 0, :], op0=ALU.mult, op1=ALU.add))
                cx.op("dve", [bfb[par]], [bfc[par]], lambda e: e.tensor_tensor(out=fc_[:, :], in0=fb_[:, :], in1=fb_[:, :], op=ALU.mult))

                def fin2(h=h, q0=q0, par=par, fb_=fb_, fc_=fc_, fd_=fd_):
                    msv = pb[6 + par][:, 0:256]
                    cx.op("pe", [bfc[par], bC], [bpb[6 + par]], lambda e: e.matmul(msv, lhsT=ones_f[:, :], rhs=fc_[:, :], start=True, stop=True))
                    cx.op("act", [bpb[6 + par], bC], [bfd[par]], lambda e: e.activation(out=fd_[:, :], in_=msv, func=AF.Ln, bias=eps_t[:, 0:1]))
                    cx.op("act", [bfd[par]], [bfd[par]], lambda e: e.activation(out=fd_[:, :], in_=fd_[:, :], func=AF.Exp, scale=-0.5))
                    cx.op("dve", [bfb[par], bfd[par], bSm], [bMT], lambda e: e.scalar_tensor_tensor(
                        out=MT[:, h, q0:q0 + 256], in0=fb_[:, :], scalar=g08[:, 0:1], in1=fd_[:, :], op0=ALU.mult, op1=ALU.mult))

                st["pend"] = fin2
                chn += 1
        st["pend"]()
        st["pend"] = None
        cx.barrier()

    if stop_after == "P3":
        if debug:
            cx.dma("sp", MTD[:, :, :], MT[:, :, :], [bMT], [bOut])
        return _finish(nc, cx, es0, esA, out_d, bOut)

    with ExitStack() as es:
        ab = [sb(es, "ab%d" % i, [128, S], BF16) for i in range(4)]
        cos2 = sb(es, "cos2", [128, S], F32)
        sin2 = sb(es, "sin2", [128, S], F32)
        qdec = sb(es, "qdec", [128, S], F32)
        tA = sb(es, "tA", [128, S], F32)
        tB = sb(es, "tB", [128, S], F32)
        qr2 = [sb(es, "qr%d" % i, [128, S], BF16) for i in range(2)]
        kr2 = [sb(es, "kr%d" % i, [128, S], BF16) for i in range(2)]
        vt4 = [sb(es, "rvt%d" % i, [128, 16, 128], BF16) for i in range(4)]
        rg4 = [sb(es, "rg%d" % i, [128, S], BF16) for i in range(4)]
        bcj = sb(es, "bcj", [128, 8, 19], F32)
        mqk = sb(es, "mqk", [128, 128], F32)
        PT = [sb(es, "rPT%d" % i, [128, 512], BF16) for i in range(4)]
        fa = [sb(es, "rfa%d" % i, [128, 512], F32) for i in range(2)]
        fb = [sb(es, "rfb%d" % i, [128, 512], F32) for i in range(2)]
        fc = [sb(es, "rfc%d" % i, [128, 512], F32) for i in range(2)]
        fd = [sb(es, "rfd%d" % i, [128, 512], F32) for i in range(2)]
        bab = [cx.buf("ab%d" % i) for i in range(4)]
        brope = cx.buf("rope")
        bqd = cx.buf("qdec")
        btA, btB = cx.buf("tA"), cx.buf("tB")
        bqr2 = [cx.buf("qr%d" % i) for i in range(2)]
        bkr2 = [cx.buf("kr%d" % i) for i in range(2)]
        bv4 = [cx.buf("rv%d" % i) for i in range(4)]
        brg4 = [cx.buf("rg%d" % i) for i in range(4)]
        bPT = [cx.buf("rPT%d" % i) for i in range(4)]
        bfa = [cx.buf("rfa%d" % i) for i in range(2)]
        bfb = [cx.buf("rfb%d" % i) for i in range(2)]
        bfc = [cx.buf("rfc%d" % i) for i in range(2)]
        bfd = [cx.buf("rfd%d" % i) for i in range(2)]
        cx.dma("sp", cos2[:, :], A["cos2"][:, :], [bW], [brope])
        cx.dma("sp", sin2[:, :], A["sin2"][:, :], [bW], [brope], join=True)
        cx.dma("sp", bcj[:, :, :], A["bcj"][:, :, :], [bW], [brope], join=True)
        cx.dma("sp", mqk[:, :], A["mqk"][:, :], [bW], [brope], join=True)
        VTv = VT.rearrange("(t p) c -> p t c", p=128)
        st = {"s": 0, "alt": 0, "pend": None}
        chn = 0
        def prep_pair(hp, eng="pool"):
            par = hp % 2
            for i in range(4):
                cx.dma("sp", ab[i][:, :], QK[16 + hp * 4 + i], [bQK[16 + hp * 4 + i]], [bab[i]])
            cx.dma("sp", qdec[:, :], A["qdec"][hp], [bW], [bqd])
            for sub in range(2):
                h = 2 * hp + sub
                ix = 2 * par + sub
                cx.dma("act", vt4[ix][:, :, :], VTv[:, :, 1024 + h * 128:1024 + (h + 1) * 128], [bVT], [bv4[ix]])
                cx.dma("act", rg4[ix][:, :], QK[32 + h], [bQK[32 + h]], [brg4[ix]])
                cx.op("act", [brg4[ix]], [brg4[ix]], lambda e: e.activation(out=rg4[ix][:, :], in_=rg4[ix][:, :], func=AF.Silu))
            qr, kr = qr2[par], kr2[par]
            cx.op(eng, [bab[0], brope], [btA], lambda e: e.tensor_tensor(out=tA[:, :], in0=ab[0][:, :], in1=cos2[:, :], op=ALU.mult))
            cx.op(eng, [bab[1], brope], [btB], lambda e: e.tensor_tensor(out=tB[:, :], in0=ab[1][:, :], in1=sin2[:, :], op=ALU.mult))
            cx.op(eng, [btA, btB], [btA], lambda e: e.tensor_tensor(out=tA[:, :], in0=tA[:, :], in1=tB[:, :], op=ALU.add))
            cx.op(eng, [btA, bqd], [bqr2[par]], lambda e: e.tensor_tensor(out=qr[:, :], in0=tA[:, :], in1=qdec[:, :], op=ALU.mult))
            cx.op(eng, [bab[2], brope], [btA], lambda e: e.tensor_tensor(out=tA[:, :], in0=ab[2][:, :], in1=cos2[:, :], op=ALU.mult))
            cx.op(eng, [bab[3], brope], [btB], lambda e: e.tensor_tensor(out=tB[:, :], in0=ab[3][:, :], in1=sin2[:, :], op=ALU.mult))
            cx.op(eng, [btA, btB], [bkr2[par]], lambda e: e.tensor_tensor(out=kr[:, :], in0=tA[:, :], in1=tB[:, :], op=ALU.add))

        prep_pair(0, "dve")
        for hp in range(4):
            if hp + 1 < 4:
                prep_pair(hp + 1)
            qr, kr = qr2[hp % 2], kr2[hp % 2]
            bqr, bkr = bqr2[hp % 2], bkr2[hp % 2]
            vt = [vt4[2 * (hp % 2)], vt4[2 * (hp % 2) + 1]]
            rg = [rg4[2 * (hp % 2)], rg4[2 * (hp % 2) + 1]]
            bv = [bv4[2 * (hp % 2)], bv4[2 * (hp % 2) + 1]]
            brg = [brg4[2 * (hp % 2)], brg4[2 * (hp % 2) + 1]]
            for sub in range(2):
                h = 2 * hp + sub
                s = sub
                p0 = sub * 64
                for qc in range(4):
                    q0 = qc * 512
                    nj = 4 * qc + 4
                    par = chn % 2
                    bot_ = bpb[4 + par]
                    otb = pb[4 + par]
                    sbank = {}

                    def s_mm(j):
                        co = max(0, j * 128 - q0)
                        bank = st["s"] % 4
                        st["s"] += 1
                        sbank[j] = bank
                        cx.op("pe", [bqr, bkr], [bpb[bank]], lambda e: e.matmul(
                            pb[bank][:, co:512], lhsT=kr[p0:p0 + 64, j * 128:(j + 1) * 128],
                            rhs=qr[p0:p0 + 64, q0 + co:q0 + 512], start=True, stop=True))

                    LA = 3
                    for jj in range(min(LA, nj)):
                        s_mm(jj)
                    for j in range(nj):
                        if j + LA < nj:
                            s_mm(j + LA)
                        co = max(0, j * 128 - q0)
                        bank = sbank[j]
                        pt = PT[bank]
                        bpt = bPT[bank]
                        mi = (q0 - 128 * j) // 128 + 3
                        sc = bcj[:, h, mi:mi + 1]
                        fs = co
                        if j * 128 >= q0:
                            cx.op("dve", [bpb[bank], brope], [bpt], lambda e: e.scalar_tensor_tensor(
                                out=pt[:, co:co + 128], in0=pb[bank][:, co:co + 128], scalar=sc, in1=mqk[:, :], op0=ALU.mult, op1=ALU.mult))
                            fs = co + 128
                        if fs < 512:
                            if st["alt"] % 2 == 0:
                                cx.op("act", [bpb[bank], brope], [bpt], lambda e: e.activation(out=pt[:, fs:512], in_=pb[bank][:, fs:512], func=AF.Copy, scale=sc))
                            else:
                                cx.op("dve", [bpb[bank], brope], [bpt], lambda e: e.tensor_scalar(out=pt[:, fs:512], in0=pb[bank][:, fs:512], scalar1=sc, scalar2=None, op0=ALU.mult))
                            st["alt"] += 1
                        cx.op("pe", [bv[s], bpt], [bot_], lambda e: e.matmul(
                            otb[:, co:512], lhsT=vt[s][:, j, :], rhs=pt[:, co:512], start=(j == 0), stop=(j == nj - 1)))
                        if j == min(1, nj - 1) and st["pend"] is not None:
                            st["pend"]()
                            st["pend"] = None
                    fa_, fb_, fc_, fd_ = fa[par], fb[par], fc[par], fd[par]
                    cx.op("act", [bot_], [bfa[par]], lambda e: e.activation(out=fa_[:, :], in_=otb[:, :], func=AF.Copy))
                    cx.op("dve", [bfa[par]], [bfb[par]], lambda e: e.tensor_tensor(out=fb_[:, :], in0=fa_[:, :], in1=fa_[:, :], op=ALU.mult))

                    def fin2(h=h, s=s, q0=q0, par=par, fa_=fa_, fb_=fb_, fc_=fc_, fd_=fd_):
                        cx.op("pe", [bfa[par], bC], [bpb[6]], lambda e: e.matmul(pb[6][:, :], lhsT=ones_f[:, :], rhs=fa_[:, :], start=True, stop=True))
                        cx.op("pe", [bfb[par], bC], [bpb[7]], lambda e: e.matmul(pb[7][:, :], lhsT=ones_f[:, :], rhs=fb_[:, :], start=True, stop=True))
                        cx.op("act", [bpb[6]], [bfc[par]], lambda e: e.activation(out=fc_[:, :], in_=pb[6][:, :], func=AF.Copy))
                        cx.op("dve", [bfc[par]], [bfd[par]], lambda e: e.tensor_tensor(out=fd_[:, :], in0=fc_[:, :], in1=fc_[:, :], op=ALU.mult))
                        cx.op("dve", [bpb[7], bfd[par]], [bfd[par]], lambda e: e.tensor_tensor(out=fd_[:, :], in0=pb[7][:, :], in1=fd_[:, :], op=ALU.subtract))
                        cx.op("dve", [bfd[par]], [bfd[par]], lambda e: e.tensor_scalar(out=fd_[:, :], in0=fd_[:, :], scalar1=0.0, scalar2=None, op0=ALU.max))
                        cx.op("act", [bfd[par], bC], [bfd[par]], lambda e: e.activation(out=fd_[:, :], in_=fd_[:, :], func=AF.Ln, bias=eps_t[:, 0:1]))
                        cx.op("act", [bfd[par]], [bfd[par]], lambda e: e.activation(out=fd_[:, :], in_=fd_[:, :], func=AF.Exp, scale=-0.5))
                        cx.op("dve", [bfa[par], bfc[par]], [bfa[par]], lambda e: e.tensor_tensor(out=fa_[:, :], in0=fa_[:, :], in1=fc_[:, :], op=ALU.subtract))
                        cx.op("dve", [bfa[par], bfd[par], bSm], [bfa[par]], lambda e: e.scalar_tensor_tensor(
                            out=fa_[:, :], in0=fa_[:, :], scalar=gng[:, h:h + 1], in1=fd_[:, :], op0=ALU.mult, op1=ALU.mult))
                        cx.op("dve", [bfa[par], brg[s]], [bMT], lambda e: e.tensor_tensor(
                            out=MT[:, 8 + h, q0:q0 + 512], in0=fa_[:, :], in1=rg[s][:, q0:q0 + 512], op=ALU.mult))

                    st["pend"] = fin2
                    chn += 1
            if st["pend"] is not None:
                st["pend"]()
                st["pend"] = None
        cx.barrier()

    if stop_after == "P4":
        if debug:
            cx.dma("sp", MTD[:, :, :], MT[:, :, :], [bMT], [bOut])
        return _finish(nc, cx, es0, esA, out_d, bOut)

    with ExitStack() as es:
        wob = [sb(es, "wob%d" % i, [128, 16, 512], BF16) for i in range(2)]
        xs_t = [sb(es, "xs_t%d" % i, [128, 512], F32) for i in range(2)]
        x1p = [sb(es, "x1p%d" % i, [128, 512], F32) for i in range(2)]
        bwo = [cx.buf("wob%d" % i) for i in range(2)]
        bxs = [cx.buf("xs_t%d" % i) for i in range(2)]
        bx1p = [cx.buf("x1p%d" % i) for i in range(2)]
        wov = A["wout"].rearrange("(kc p) f -> p kc f", p=128)
        n = 0
        for dc in range(4):
            wo = wob[dc % 2]
            for hf in range(2):
                cx.dma("pool", wo[:, hf * 8:(hf + 1) * 8, :], wov[:, hf * 8:(hf + 1) * 8, dc * 512:(dc + 1) * 512], [bW], [bwo[dc % 2]], join=(hf > 0))
            for t in range(16):
                bk = n % 4
                s = n % 2
                cx.dma("sp", xs_t[s][:, :], A["x"][t * 128:(t + 1) * 128, dc * 512:(dc + 1) * 512], [bX], [bxs[s]])
                for kc in range(16):
                    cx.op("pe", [bwo[dc % 2], bMT], [bpb[bk]], lambda e: e.matmul(pb[bk][:, :], lhsT=MT[:, kc, t * 128:(t + 1) * 128], rhs=wo[:, kc, :], start=(kc == 0), stop=(kc == 15)))
                cx.op("dve", [bpb[bk], bxs[s]], [bx1p[s]], lambda e: e.tensor_tensor(out=x1p[s][:, :], in0=pb[bk][:, :], in1=xs_t[s][:, :], op=ALU.add))
                cx.dma("act", X1[t * 128:(t + 1) * 128, dc * 512:(dc + 1) * 512], x1p[s][:, :], [bx1p[s]], [bX1], join=True, sem_buf=bx1p[s])
                n += 1
        cx.barrier()
    esA.close()

    if stop_after == "P5":
        return _finish(nc, cx, es0, None, out_d, bOut)

    _phase_b(nc, cx, A, sb, pb, bpb, bW, bC, X1, bX1, XS, bXS, YS, bYS, out_d, bOut,
             ident_f, ident_bf, ones_bf, eps_t, gates, rows_i, bGates, bRows, RD, debug, stop_after)
    return _finish(nc, cx, es0, None, out_d, bOut)


def _finish(nc, cx, es0, esA, out_d, bOut):
    cx.barrier()
    return nc


def _phase_b(nc, cx, A, sb, pb, bpb, bW, bC, X1, bX1, XS, bXS, YS, bYS, out_d, bOut,
             ident_f, ident_bf, ones_bf, eps_t, gates, rows_i, bGates, bRows, RD, debug, stop_after):
    BIG = 20000.0
    bnd_reg = nc.gpsimd.alloc_register("bnd")
    nc.gpsimd.reg_mov(bnd_reg, NSLOT - 1)
    with ExitStack() as es:
        x1t = [sb(es, "x1t%d" % i, [128, D], F32) for i in range(2)]
        h2f = [sb(es, "h2f%d" % i, [128, D], F32) for i in range(2)]
        h2b = sb(es, "h2b", [128, 16, D], BF16)
        junk = sb(es, "junk6", [128, D], BF16)
        h2T = [sb(es, "h2T%d" % i, [128, 16, 128], F32) for i in range(2)]
        wr = sb(es, "wr", [128, 16, 36], F32)
        gffn = sb(es, "gffn", [128, D], F32)
        rbias = sb(es, "rbias", [128, 36], F32)
        Mall = sb(es, "Mall", [128, 16, 32], BF16)
        lstrict = sb(es, "lstrict", [128, 128], BF16)
        iota_e = sb(es, "iota_e", [128, 32], F32)
        LG = sb(es, "LG", [128, 16, 36], F32)
        stA = sb(es, "stA", [128, 16, 4], F32)
        gmax = sb(es, "gmax", [128, 16], F32)
        t4 = sb(es, "t4", [128, 16, 4], F32)
        gmk = sb(es, "gmk", [128, 16, 4], F32)
        sc16 = sb(es, "sc16", [128, 12, 16], F32)
        mi = sb(es, "mi", [128, 16, 32], F32)
        mi2 = sb(es, "mi2", [128, 16, 32], F32)
        M1 = sb(es, "M1", [128, 16, 32], F32)
        M2 = sb(es, "M2", [128, 16, 32], F32)
        tt = sb(es, "tt6", [128, 16, 32], F32)
        csa = sb(es, "csa", [128, 16, 32], F32)
        pre = sb(es, "pre", [128, 16, 32], F32)
        cnt = sb(es, "cnt", [128, 16, 32], F32)
        rowf = sb(es, "rowf", [128, 16, 2], F32)
        bx1t = [cx.buf("x1t%d" % i) for i in range(2)]
        bh2f = [cx.buf("h2f%d" % i) for i in range(2)]
        bh2b = [cx.buf("h2b%d" % i) for i in range(16)]
        bjunk = cx.buf("junk6")
        bh2T = [cx.buf("h2T%d" % i) for i in range(2)]
        bK = cx.buf("p6const")
        bLG = cx.buf("LG")
        bstA = cx.buf("stA")
        bs = cx.buf("p6small")
        cx.dma("sp", wr[:, :, :], A["wr"][:, :, :], [bW], [bK])
        cx.dma("sp", gffn[:, :], A["g_ffn"].to_broadcast([128, D]), [bW], [bK], join=True)
        cx.dma("sp", rbias[:, :], A["rbias"].to_broadcast([128, 36]), [bW], [bK], join=True)
        cx.dma("sp", lstrict[:, :], A["lstrict"][:, :], [bW], [bK], join=True)
        cx.dma("sp", iota_e[:, :], A["iota_e"][:, :], [bW], [bK], join=True)
        def p6_front(t):
            s = t % 2
            cx.dma("sp", x1t[s][:, :], X1[t * 128:(t + 1) * 128, :], [bX1], [bx1t[s]])
            cx.op("act", [bx1t[s]], [bjunk, bstA], lambda e: e.activation(out=junk[:, :], in_=x1t[s][:, :], func=AF.Square, accum_out=stA[:, t, 0:1]))
            cx.op("act", [bstA, bC], [bstA], lambda e: e.activation(out=stA[:, t, 1:2], in_=stA[:, t, 0:1], func=AF.Ln, scale=1.0 / D, bias=eps_t[:, 0:1]))
            cx.op("act", [bstA], [bstA], lambda e: e.activation(out=stA[:, t, 2:3], in_=stA[:, t, 1:2], func=AF.Exp, scale=-0.5))
            cx.op("dve", [bx1t[s], bstA, bK], [bh2f[s]], lambda e: e.scalar_tensor_tensor(out=h2f[s][:, :], in0=x1t[s][:, :], scalar=stA[:, t, 2:3], in1=gffn[:, :], op0=ALU.mult, op1=ALU.mult))
            cx.op("act", [bh2f[s]], [bh2b[t]], lambda e: e.activation(out=h2b[:, t, :], in_=h2f[s][:, :], func=AF.Copy))

        def p6_back(t):
            s = t % 2
            for g4 in range(4):
                bk = 4 * s + g4
                for k in range(4):
                    kc = g4 * 4 + k
                    cx.op("pe", [bh2f[s], bC], [bpb[bk]], lambda e: e.transpose(pb[bk][:, k * 128:(k + 1) * 128], h2f[s][:, kc * 128:(kc + 1) * 128], ident_f[:, :]))
                dst = h2T[s][:, g4 * 4:(g4 + 1) * 4, :]
                srcv = pb[bk].rearrange("p (k c) -> p k c", k=4)
                if g4 % 2 == 0:
                    cx.op("act", [bpb[bk]], [bh2T[s]], lambda e: e.activation(out=dst, in_=srcv, func=AF.Copy))
                else:
                    cx.op("dve", [bpb[bk]], [bh2T[s]], lambda e: e.tensor_copy(out=dst, in_=srcv))
            bk = 4 * s
            for kc in range(16):
                cx.op("pe", [bh2T[s], bK], [bpb[bk]], lambda e: e.matmul(pb[bk][:, 0:36], lhsT=h2T[s][:, kc, :], rhs=wr[:, kc, :], start=(kc == 0), stop=(kc == 15)))
            cx.op("dve", [bpb[bk], bK], [bLG], lambda e: e.tensor_tensor(out=LG[:, t, :], in0=pb[bk][:, 0:36], in1=rbias[:, :], op=ALU.add))

        p6_front(0)
        for t in range(16):
            if t + 1 < 16:
                p6_front(t + 1)
            p6_back(t)
        lg4 = LG[:, :, 0:4]
        lgin = LG[:, :, 4:36].rearrange("p t (g e) -> p t g e", g=4)
        S_ = lambda i: sc16[:, i, :]
        bc4 = lambda ap: ap.unsqueeze(2).to_broadcast([128, 16, 4])
        bc32 = lambda ap: ap.unsqueeze(2).to_broadcast([128, 16, 32])

        def dve(fn, rd=(), wr_=None):
            cx.op("dve", [bs] + list(rd), [bs if wr_ is None else wr_], fn)

        def act(fn):
            cx.op("act", [bs], [bs], fn)

        dve(lambda e: e.tensor_reduce(out=gmax[:, :], in_=lg4, axis=AX.X, op=ALU.max), rd=[bLG])
        dve(lambda e: e.tensor_tensor(out=t4[:, :, :], in0=lg4, in1=bc4(gmax[:, :]), op=ALU.subtract), rd=[bLG])
        act(lambda e: e.activation(out=t4[:, :, :], in_=t4[:, :, :], func=AF.Exp))
        dve(lambda e: e.reduce_sum(out=S_(0), in_=t4[:, :, :], axis=AX.X))
        act(lambda e: e.activation(out=S_(1), in_=S_(0), func=AF.Ln))
        act(lambda e: e.activation(out=S_(2), in_=S_(1), func=AF.Exp, scale=-1.0))
        dve(lambda e: e.tensor_tensor(out=gmk[:, :, :], in0=lg4, in1=bc4(gmax[:, :]), op=ALU.is_equal), rd=[bLG])
        dve(lambda e: e.tensor_scalar(out=gmk[:, :, :], in0=gmk[:, :, :], scalar1=-1.0, scalar2=1e9, op0=ALU.add, op1=ALU.mult))
        dve(lambda e: e.tensor_tensor(out=mi[:, :, :].rearrange("p t (g e) -> p t g e", g=4), in0=lgin,
                                      in1=gmk[:, :, :].unsqueeze(3).to_broadcast([128, 16, 4, 8]), op=ALU.add), rd=[bLG])
        dve(lambda e: e.tensor_reduce(out=S_(3), in_=mi[:, :, :], axis=AX.X, op=ALU.max))
        dve(lambda e: e.tensor_tensor(out=M1[:, :, :], in0=mi[:, :, :], in1=bc32(S_(3)), op=ALU.is_equal))
        dve(lambda e: e.scalar_tensor_tensor(out=mi2[:, :, :], in0=M1[:, :, :], scalar=-1e9, in1=mi[:, :, :], op0=ALU.mult, op1=ALU.add))
        dve(lambda e: e.tensor_reduce(out=S_(4), in_=mi2[:, :, :], axis=AX.X, op=ALU.max))
        dve(lambda e: e.tensor_tensor(out=M2[:, :, :], in0=mi2[:, :, :], in1=bc32(S_(4)), op=ALU.is_equal))
        dve(lambda e: e.tensor_tensor(out=S_(5), in0=S_(4), in1=S_(3), op=ALU.subtract))
        act(lambda e: e.activation(out=S_(5), in_=S_(5), func=AF.Exp))
        dve(lambda e: e.tensor_scalar(out=S_(6), in0=S_(5), scalar1=1.0, scalar2=None, op0=ALU.add))
        act(lambda e: e.activation(out=S_(6), in_=S_(6), func=AF.Ln))
        act(lambda e: e.activation(out=S_(6), in_=S_(6), func=AF.Exp, scale=-1.0))
        dve(lambda e: e.tensor_tensor(out=S_(7), in0=S_(5), in1=S_(6), op=ALU.mult))
        dve(lambda e: e.tensor_tensor(out=S_(6), in0=S_(6), in1=S_(2), op=ALU.mult))
        dve(lambda e: e.tensor_tensor(out=S_(7), in0=S_(7), in1=S_(2), op=ALU.mult))
        bMall = cx.buf("Mall")
        dve(lambda e: e.tensor_tensor(out=Mall[:, :, :], in0=M1[:, :, :], in1=M2[:, :, :], op=ALU.add), wr_=bMall)
        mflat = Mall[:, :, :].rearrange("p t e -> p (t e)")
        cx.op("pe", [bMall, bK], [bpb[0]], lambda e: e.matmul(pb[0][:, :], lhsT=lstrict[:, :], rhs=mflat, start=True, stop=True))
        cx.op("pe", [bMall, bC], [bpb[1]], lambda e: e.matmul(pb[1][:, :], lhsT=ones_bf[:, :], rhs=mflat, start=True, stop=True))
        dve(lambda e: e.tensor_copy(out=csa[:, :, :].rearrange("p t e -> p (t e)"), in_=pb[1][:, :]), rd=[bpb[1]])
        dve(lambda e: e.memset(pre[:, 0, :], 0.0))
        for t in range(1, 16):
            dve(lambda e: e.tensor_tensor(out=pre[:, t, :], in0=pre[:, t - 1, :], in1=csa[:, t - 1, :], op=ALU.add))
        dve(lambda e: e.tensor_tensor(out=cnt[:, :, :].rearrange("p t e -> p (t e)"), in0=pb[0][:, :], in1=pre[:, :, :].rearrange("p t e -> p (t e)"), op=ALU.add), rd=[bpb[0]])
        for k, Mk in ((0, M1), (1, M2)):
            dve(lambda e: e.tensor_tensor(out=tt[:, :, :], in0=Mk[:, :, :], in1=cnt[:, :, :], op=ALU.mult))
            dve(lambda e: e.reduce_sum(out=S_(8), in_=tt[:, :, :], axis=AX.X))
            dve(lambda e: e.tensor_tensor(out=tt[:, :, :], in0=Mk[:, :, :], in1=iota_e[:, :].unsqueeze(1).to_broadcast([128, 16, 32]), op=ALU.mult), rd=[bK])
            dve(lambda e: e.reduce_sum(out=S_(9), in_=tt[:, :, :], axis=AX.X))
            dve(lambda e: e.scalar_tensor_tensor(out=S_(10), in0=S_(9), scalar=float(CAP), in1=S_(8), op0=ALU.mult, op1=ALU.add))
            dve(lambda e: e.tensor_scalar(out=S_(11), in0=S_(8), scalar1=float(CAP), scalar2=None, op0=ALU.is_lt))
            dve(lambda e: e.scalar_tensor_tensor(out=S_(10), in0=S_(10), scalar=-BIG, in1=S_(11), op0=ALU.add, op1=ALU.mult))
            dve(lambda e: e.tensor_scalar(out=rowf[:, :, k], in0=S_(10), scalar1=BIG, scalar2=None, op0=ALU.add))
            dve(lambda e: e.tensor_tensor(out=gates[:, :, k], in0=S_(6 + k), in1=S_(11), op=ALU.mult), wr_=bGates)
        dve(lambda e: e.tensor_copy(out=rows_i[:, :, :], in_=rowf[:, :, :]), wr_=bRows)
        for t in range(16):
            for k in range(2):
                cx.dma("pool", None, None, [bh2b[t], bRows], [bXS], join=True, sem_buf=bh2b[t], fn=lambda e: e.indirect_dma_start(
                    out=XS[:, :], out_offset=bass.IndirectOffsetOnAxis(ap=rows_i[:, t, k:k + 1], axis=0),
                    in_=h2b[:, t, :], in_offset=None, bounds_check=bnd_reg, oob_is_err=False))
        cx.barrier()

    if stop_after == "P6":
        return

    with ExitStack() as es:
        NST = CAP // 128
        xtok = [sb(es, "xtok%d" % i, [128, NST, D], BF16) for i in range(2)]
        xsT = [sb(es, "xsT%d" % i, [128, 16, CAP], BF16) for i in range(2)]
        slab = [sb(es, "slab%d" % i, [128, 8, FF], BF16) for i in range(4)]
        wdb = [sb(es, "wdb%d" % i, [128, 8, D], BF16) for i in range(2)]
        sg = sb(es, "sg", [128, 8, CAP], F32)
        hT = [sb(es, "hT%d" % i, [128, 8, CAP], BF16) for i in range(2)]
        yst = [sb(es, "yst%d" % i, [128, D], BF16) for i in range(2)]
        bxtok = [cx.buf("xtok%d" % i) for i in range(2)]
        bxsT = [cx.buf("xsT%d" % i) for i in range(2)]
        bslab = [cx.buf("slab%d" % i) for i in range(4)]
        bwdb = [cx.buf("wdb%d" % i) for i in range(2)]
        bsg = [cx.buf("sg%d" % i) for i in range(4)]
        bhT = [cx.buf("hT%d" % i) for i in range(2)]
        byst = [cx.buf("yst%d" % i) for i in range(2)]
        ucount = 0
        ntr = 0
        ndn = 0
        for ex in range(NE):
            s = ex % 2
            cx.dma("sp", xtok[s][:, :, :], XS[ex * CAP:(ex + 1) * CAP, :].rearrange("(st p) d -> p st d", p=128), [bXS], [bxtok[s]])
            for st_ in range(NST):
                for half in range(2):
                    bk = 6 + ntr % 2
                    pbf = pb[bk].bitcast(BF16)
                    for k in range(8):
                        kc = half * 8 + k
                        cx.op("pe", [bxtok[s], bC], [bpb[bk]], lambda e: e.transpose(pbf[:, k * 128:(k + 1) * 128], xtok[s][:, st_, kc * 128:(kc + 1) * 128], ident_bf[:, :]))
                    dst = xsT[s][:, half * 8:(half + 1) * 8, st_ * 128:(st_ + 1) * 128]
                    src = pbf[:, :].rearrange("p (k c) -> p k c", k=8)
                    if ntr % 2 == 0:
                        cx.op("act", [bpb[bk]], [bxsT[s]], lambda e: e.activation(out=dst, in_=src, func=AF.Copy))
                    else:
                        cx.op("dve", [bpb[bk]], [bxsT[s]], lambda e: e.tensor_copy(out=dst, in_=src))
                    ntr += 1
            for (wname, isgate) in (("wg", True), ("wu", False)):
                sls = [slab[ucount % 4], slab[(ucount + 1) % 4]]
                bsls = [bslab[ucount % 4], bslab[(ucount + 1) % 4]]
                ucount += 2
                wv = A[wname][ex].rearrange("(kc p) f -> p kc f", p=128)
                for kh in range(2):
                    for hf in range(2):
                        cx.dma("pool", sls[kh][:, hf * 4:(hf + 1) * 4, :], wv[:, kh * 8 + hf * 4:kh * 8 + (hf + 1) * 4, :], [bW], [bsls[kh]], join=(hf > 0))
                for f in range(8):
                    bk = f // 2
                    c0 = (f % 2) * CAP
                    for kc in range(16):
                        sl = sls[kc // 8]
                        cx.op("pe", [bsls[kc // 8], bxsT[s]], [bpb[bk]], lambda e: e.matmul(pb[bk][:, c0:c0 + CAP], lhsT=sl[:, kc % 8, f * 128:(f + 1) * 128], rhs=xsT[s][:, kc, :], start=(kc == 0), stop=(kc == 15)))
                    if f % 2 == 1:
                        sgv = sg[:, f - 1:f + 1, :].rearrange("p a c -> p (a c)")
                        if isgate:
                            cx.op("act", [bpb[bk]], [bsg[bk]], lambda e: e.activation(out=sgv, in_=pb[bk][:, :], func=AF.Silu))
                        else:
                            dst = hT[s][:, f - 1:f + 1, :].rearrange("p a c -> p (a c)")
                            cx.op("dve", [bpb[bk], bsg[bk]], [bhT[s]], lambda e: e.tensor_tensor(out=dst, in0=pb[bk][:, :], in1=sgv, op=ALU.mult))
            wdv = A["wd"][ex].rearrange("(kc p) f -> p kc f", p=128)
            for hf in range(4):
                cx.dma("pool", wdb[s][:, hf * 2:(hf + 1) * 2, :], wdv[:, hf * 2:(hf + 1) * 2, :], [bW], [bwdb[s]], join=(hf > 0))
            for st_ in range(NST):
                ys_ = yst[st_ % 2]
                for dc in range(4):
                    bk = 4 + ndn % 2
                    for kc in range(8):
                        cx.op("pe", [bhT[s], bwdb[s]], [bpb[bk]], lambda e: e.matmul(pb[bk][:, :], lhsT=hT[s][:, kc, st_ * 128:(st_ + 1) * 128], rhs=wdb[s][:, kc, dc * 512:(dc + 1) * 512], start=(kc == 0), stop=(kc == 7)))
                    if ndn % 2 == 0:
                        cx.op("act", [bpb[bk]], [byst[st_ % 2]], lambda e: e.activation(out=ys_[:, dc * 512:(dc + 1) * 512], in_=pb[bk][:, :], func=AF.Copy))
                    else:
                        cx.op("dve", [bpb[bk]], [byst[st_ % 2]], lambda e: e.tensor_copy(out=ys_[:, dc * 512:(dc + 1) * 512], in_=pb[bk][:, :]))
                    ndn += 1
                r0 = ex * CAP + st_ * 128
                cx.dma("sp", YS[r0:r0 + 128, :], ys_[:, :], [byst[st_ % 2]], [bYS], join=True, sem_buf=byst[st_ % 2])
        cx.barrier()

    with ExitStack() as es:
        R8 = 3
        y1 = [sb(es, "y1_%d" % i, [128, D], BF16) for i in range(R8)]
        y2 = [sb(es, "y2_%d" % i, [128, D], BF16) for i in range(R8)]
        x1t = [sb(es, "x8t%d" % i, [128, D], F32) for i in range(R8)]
        acc = [sb(es, "acc8_%d" % i, [128, D], F32) for i in range(2)]
        gfin = sb(es, "gfin", [128, D], F32)
        junk = sb(es, "junk8", [128, D], BF16)
        ot = [sb(es, "ot%d" % i, [128, D], F32) for i in range(2)]
        sm = [sb(es, "sm8_%d" % i, [128, 4], F32) for i in range(2)]
        by1 = [cx.buf("y1_%d" % i) for i in range(R8)]
        by2 = [cx.buf("y2_%d" % i) for i in range(R8)]
        bx = [cx.buf("x8t%d" % i) for i in range(R8)]
        bacc = [cx.buf("acc8_%d" % i) for i in range(2)]
        bg = cx.buf("gfin")
        bj = cx.buf("junk8")
        bot = [cx.buf("ot%d" % i) for i in range(2)]
        bs = [cx.buf("sm8_%d" % i) for i in range(2)]
        cx.dma("sp", gfin[:, :], A["g_fin"].to_broadcast([128, D]), [bW], [bg])
        for i in range(R8):
            cx.op("dve", [], [by1[i]], lambda e: e.memset(y1[i][:, :], 0.0))
            cx.op("pool", [], [by2[i]], lambda e: e.memset(y2[i][:, :], 0.0))
        def fetch(t):
            s = t % R8
            for (yy, byy, k) in ((y1, by1, 0), (y2, by2, 1)):
                cx.dma("pool", None, None, [bYS, bRows], [byy[s]], fn=lambda e: e.indirect_dma_start(
                    out=yy[s][:, :], out_offset=None, in_=YS[:, :],
                    in_offset=bass.IndirectOffsetOnAxis(ap=rows_i[:, t, k:k + 1], axis=0),
                    bounds_check=bnd_reg, oob_is_err=False))
            cx.dma("sp", x1t[s][:, :], X1[t * 128:(t + 1) * 128, :], [bX1], [bx[s]])

        def p8_front(t):
            s = t % R8
            a = t % 2
            cx.op("dve", [by1[s], bx[s], bGates], [bacc[a]], lambda e: e.scalar_tensor_tensor(out=acc[a][:, :], in0=y1[s][:, :], scalar=gates[:, t, 0:1], in1=x1t[s][:, :], op0=ALU.mult, op1=ALU.add))
            cx.op("dve", [by2[s], bGates, bacc[a]], [bacc[a]], lambda e: e.scalar_tensor_tensor(out=acc[a][:, :], in0=y2[s][:, :], scalar=gates[:, t, 1:2], in1=acc[a][:, :], op0=ALU.mult, op1=ALU.add))
            if t + R8 < 16:
                fetch(t + R8)
            cx.op("act", [bacc[a]], [bj, bs[a]], lambda e: e.activation(out=junk[:, :], in_=acc[a][:, :], func=AF.Square, accum_out=sm[a][:, 0:1]))
            cx.op("act", [bs[a], bC], [bs[a]], lambda e: e.activation(out=sm[a][:, 1:2], in_=sm[a][:, 0:1], func=AF.Ln, scale=1.0 / D, bias=eps_t[:, 0:1]))
            cx.op("act", [bs[a]], [bs[a]], lambda e: e.activation(out=sm[a][:, 2:3], in_=sm[a][:, 1:2], func=AF.Exp, scale=-0.5))

        def p8_back(t):
            a = t % 2
            cx.op("dve", [bacc[a], bs[a], bg], [bot[a]], lambda e: e.scalar_tensor_tensor(out=ot[a][:, :], in0=acc[a][:, :], scalar=sm[a][:, 2:3], in1=gfin[:, :], op0=ALU.mult, op1=ALU.mult))
            cx.dma("sp", out_d[t * 128:(t + 1) * 128, :], ot[a][:, :], [bot[a]], [bOut], join=True, sem_buf=bot[a])

        for t in range(R8):
            fetch(t)
        p8_front(0)
        for t in range(16):
            if t + 1 < 16:
                p8_front(t + 1)
            p8_back(t)


def kernel(**inputs):
    wl = _layout_weights(inputs)
    cs = _consts()
    base = dict(wl)
    base.update(cs)
    x = np.asarray(inputs["x"], dtype=np.float32)
    nb = x.shape[0]
    nc = build()
    in_maps = []
    for b in range(nb):
        m = dict(base)
        m["x"] = np.ascontiguousarray(x[b])
        in_maps.append(m)
    res = run_bass_kernel_spmd(nc, in_maps, core_ids=list(range(nb)))
    return np.stack([np.asarray(r["out"], dtype=np.float32) for r in res.results], axis=0)
```

```python
import math
from contextlib import ExitStack

import numpy as np
import ml_dtypes
import concourse.bass as bass
import concourse.mybir as mybir
from concourse.bass_utils import run_bass_kernel_spmd

F32 = mybir.dt.float32
BF16 = mybir.dt.bfloat16
I32 = mybir.dt.int32
AF = mybir.ActivationFunctionType
ALU = mybir.AluOpType
AX = mybir.AxisListType

S = 2048
D = 2048
NH = 8
CAP = 256
NE = 32
NSLOT = NE * CAP
FF = 1024
EPS = 1e-6
NBLK = 40
NEG = -30000.0


class Buf:
    def __init__(self, name):
        self.name = name
        self.w = None
        self.r = {}
        self.dsem = None
        self.dcnt = 0


class DSem:
    def __init__(self, sem):
        self.sem = sem
        self.cnt = 0


class Ctx:
    def share(self, bufs, name):
        ds = DSem(self.nc.alloc_semaphore("d_" + name))
        self.dbufs.append(ds)
        for b in bufs:
            b.dsem = ds

    def __init__(self, nc):
        self.nc = nc
        self.engs = {"pe": nc.tensor, "act": nc.scalar, "dve": nc.vector,
                     "pool": nc.gpsimd, "sp": nc.sync}
        self.sem = {e: nc.alloc_semaphore("s_" + e) for e in self.engs}
        self.cnt = {e: 0 for e in self.engs}
        self.seen = {e: {} for e in self.engs}
        self.dbufs = []

    def buf(self, name):
        return Buf(name)

    def _wait(self, e, ev):
        sem, val = ev
        key = id(sem)
        if e == "pe" and sem is self.sem["pe"]:
            return
        if self.seen[e].get(key, 0) >= val:
            return
        self.engs[e].wait_ge(sem, val)
        self.seen[e][key] = val

    def _deps(self, e, reads, writes, join=False):
        for b in reads:
            if b.w is not None:
                self._wait(e, b.w)
        for b in writes:
            if b.w is not None and not join:
                self._wait(e, b.w)
            for ev in b.r.values():
                self._wait(e, ev)

    def _mark(self, ev, reads, writes):
        for b in reads:
            b.r[id(ev[0])] = ev
        for b in writes:
            b.w = ev
            b.r = {}

    def op(self, e, reads, writes, fn):
        self._deps(e, reads, writes)
        inst = fn(self.engs[e])
        self.cnt[e] += 1
        inst.then_inc(self.sem[e], 1)
        self._mark((self.sem[e], self.cnt[e]), reads, writes)

    def dma(self, q, out, in_, reads, writes, join=False, fn=None, sem_buf=None, **kw):
        (wb0,) = writes
        self._deps(q, reads, writes, join=join)
        wb = sem_buf if sem_buf is not None else wb0
        if wb.dsem is None:
            wb.dsem = DSem(self.nc.alloc_semaphore("d_" + wb.name))
            self.dbufs.append(wb.dsem)
        if fn is None:
            inst = self.engs[q].dma_start(out=out, in_=in_, **kw)
        else:
            inst = fn(self.engs[q])
        wb.dsem.cnt += 1
        inst.then_inc(wb.dsem.sem, 16)
        self._mark((wb.dsem.sem, 16 * wb.dsem.cnt), reads, writes)

    def barrier(self):
        for e in self.engs:
            for e2 in self.engs:
                if e2 != e and self.cnt[e2] > 0:
                    self._wait(e, (self.sem[e2], self.cnt[e2]))
            for ds in self.dbufs:
                if ds.cnt:
                    self._wait(e, (ds.sem, 16 * ds.cnt))


def _t5_onehot():
    d = np.arange(-127, 256)
    dd = np.maximum(d, 0)
    lr = (np.log(np.maximum(dd, 1).astype(np.float32) / np.float32(16)) /
          np.float32(math.log(128 / 16))).astype(np.float32)
    large = 16 + (lr * np.float32(16)).astype(np.int32)
    large = np.minimum(large, 31)
    bucket = np.where(dd < 16, dd, large)
    oh = np.zeros((33, 383), np.float32)
    for j in range(383):
        if d[j] < 0:
            oh[32, j] = 1.0
        else:
            oh[bucket[j], j] = 1.0
    return oh


def _consts():
    c = {}
    c["ident_bf"] = np.eye(128, dtype=np.float32).astype(ml_dtypes.bfloat16)
    c["ident_f"] = np.eye(128, dtype=np.float32)
    tp = np.arange(128)
    c["lstrict"] = (tp[:, None] < tp[None, :]).astype(np.float32).astype(ml_dtypes.bfloat16)
    c["iota_e"] = np.tile(np.arange(32, dtype=np.float32)[None], (128, 1))
    c["oh"] = _t5_onehot()
    inv = (10000.0 ** (-np.arange(0, 64, 2, dtype=np.float32) / np.float32(64))).astype(np.float32)
    ang = np.arange(S, dtype=np.float32)[:, None] * inv[None, :]
    cos = np.cos(ang).astype(np.float32).T
    sin = np.sin(ang).astype(np.float32).T
    cos2 = np.zeros((128, S), np.float32)
    sin2 = np.zeros((128, S), np.float32)
    for p in range(128):
        w = p % 64
        cos2[p] = cos[w % 32]
        sin2[p] = -sin[w] if w < 32 else sin[w - 32]
    c["cos2"] = cos2
    c["sin2"] = sin2
    hh = np.arange(8, dtype=np.float32)
    log_g = np.log(np.float32(1.0) - np.exp2(np.float32(-5.0) - hh)).astype(np.float32)
    t = np.arange(S, dtype=np.float32)
    lg64 = log_g.astype(np.float64)
    tm = (np.arange(S) % 512).astype(np.float64)
    qd = np.exp(tm[None, :] * lg64[:, None])
    qdec = np.zeros((4, 128, S), np.float32)
    for hp in range(4):
        qdec[hp, 0:64] = qd[2 * hp][None, :]
        qdec[hp, 64:128] = qd[2 * hp + 1][None, :]
    c["qdec"] = qdec
    kk = np.arange(128, dtype=np.float64)
    mm = np.arange(-3, 16, dtype=np.float64)
    bcj = 0.125 * np.exp(-kk[:, None, None] * lg64[None, :, None] + 128.0 * mm[None, None, :] * lg64[None, :, None])
    c["bcj"] = bcj.astype(np.float32)
    c["mqk"] = (kk[:, None] <= kk[None, :]).astype(np.float32)
    return c


def _fm_cols():
    blocks = []
    for h in range(8):
        blocks.append(list(range(h * 128, (h + 1) * 128)))
    for h in range(8):
        blocks.append(list(range(1024 + h * 128, 1024 + (h + 1) * 128)))
    swap = [(d + 32) % 64 for d in range(64)]
    for hp in range(4):
        for base in (3072, 3584):
            a, b = [], []
            for sub in range(2):
                h = 2 * hp + sub
                a += [base + h * 64 + d for d in range(64)]
                b += [base + h * 64 + swap[d] for d in range(64)]
            blocks.append(a)
            blocks.append(b)
    for h in range(8):
        blocks.append(list(range(5120 + h * 128, 5120 + (h + 1) * 128)))
    assert len(blocks) == NBLK
    return blocks


def _layout_weights(inp):
    w_in = np.asarray(inp["w_in"][0], dtype=np.float32)
    blocks = _fm_cols()
    w4 = w_in.reshape(16, 128, 6144)
    wfm = np.empty((NBLK, 128, 16, 128), np.float32)
    for i, cols in enumerate(blocks):
        wfm[i] = w4[:, :, cols].transpose(1, 0, 2)
    vcols = list(range(2048, 3072)) + list(range(4096, 5120))
    wv = w4[:, :, vcols]
    wtm = np.ascontiguousarray(wv.reshape(16, 128, 4, 512).transpose(2, 1, 0, 3))
    wr = np.concatenate([np.asarray(inp["w_group_router"][0])] +
                        [np.asarray(inp["w_inner_router"][0][g]) for g in range(4)], axis=1)
    wr = np.ascontiguousarray(wr.reshape(16, 128, 36).transpose(1, 0, 2)).astype(np.float32)
    rb = np.concatenate([np.asarray(inp["b_group"][0]).reshape(1, 4),
                         np.asarray(inp["b_inner"][0]).reshape(1, 32)], axis=1).astype(np.float32)
    d = {
        "wfm": wfm.reshape(NBLK, 128, 2048),
        "wtm": wtm,
        "wout": np.ascontiguousarray(inp["w_out"][0], dtype=np.float32),
        "wg": np.ascontiguousarray(inp["w_gate_exp"][0], dtype=np.float32),
        "wu": np.ascontiguousarray(inp["w_up_exp"][0], dtype=np.float32),
        "wd": np.ascontiguousarray(inp["w_down_exp"][0], dtype=np.float32),
        "wr": wr,
        "rbias": rb,
        "rel_bias": np.ascontiguousarray(inp["rel_bias"], dtype=np.float32),
        "g_attn": np.asarray(inp["attn_norm_g"][0], np.float32).reshape(1, D),
        "g_ffn": np.asarray(inp["ffn_norm_g"][0], np.float32).reshape(1, D),
        "g_fin": np.asarray(inp["final_g"], np.float32).reshape(1, D),
        "lq1": np.asarray(inp["lambda_q1"], np.float32).reshape(1, 64),
        "lk1": np.asarray(inp["lambda_k1"], np.float32).reshape(1, 64),
        "lq2": np.asarray(inp["lambda_q2"], np.float32).reshape(1, 64),
        "lk2": np.asarray(inp["lambda_k2"], np.float32).reshape(1, 64),
        "gsub": np.ascontiguousarray(np.asarray(inp["diff_subln_g"][0], np.float32).reshape(128, 1)),
        "gng": np.ascontiguousarray(np.asarray(inp["ret_gn_g"][0], np.float32).reshape(8, 128).T),
    }
    return d


IN_SPECS = [
    ("x", [S, D], F32), ("wfm", [NBLK, 128, 2048], F32), ("wtm", [4, 128, 16, 512], F32),
    ("wout", [2048, 2048], F32), ("wg", [NE, D, FF], F32), ("wu", [NE, D, FF], F32),
    ("wd", [NE, FF, D], F32), ("wr", [128, 16, 36], F32), ("rbias", [1, 36], F32),
    ("rel_bias", [32, 8], F32), ("g_attn", [1, D], F32), ("g_ffn", [1, D], F32),
    ("g_fin", [1, D], F32), ("lq1", [1, 64], F32), ("lk1", [1, 64], F32), ("lq2", [1, 64], F32),
    ("lk2", [1, 64], F32), ("gsub", [128, 1], F32), ("gng", [128, 8], F32),
    ("ident_bf", [128, 128], BF16), ("ident_f", [128, 128], F32), ("lstrict", [128, 128], BF16),
    ("iota_e", [128, 32], F32), ("oh", [33, 383], F32), ("cos2", [128, S], F32),
    ("sin2", [128, S], F32), ("qdec", [4, 128, S], F32), ("bcj", [128, 8, 19], F32), ("mqk", [128, 128], F32),
]


def build(stop_after=None, debug=False):
    nc = bass.Bass("TRN2", target_bir_lowering=False)
    cx = Ctx(nc)
    A = {}
    for name, shape, dt in IN_SPECS:
        A[name] = nc.dram_tensor(name, shape, dt, kind="ExternalInput").ap()
    out_d = nc.dram_tensor("out", [S, D], F32, kind="ExternalOutput").ap()
    dk = "ExternalOutput" if debug else "Internal"
    QK = nc.dram_tensor("QK", [NBLK, 128, S], BF16, kind=dk).ap()
    VT = nc.dram_tensor("VT", [S, 2048], BF16, kind=dk).ap()
    X1 = nc.dram_tensor("X1", [S, D], F32, kind=dk).ap()
    MTD = nc.dram_tensor("MTD", [128, 16, S], BF16, kind=dk).ap() if debug else None
    XS = nc.dram_tensor("XS", [NSLOT, D], BF16, kind="Internal").ap()
    YS = nc.dram_tensor("YS", [NSLOT, D], BF16, kind="Internal").ap()
    fscr_t = nc.dram_tensor("FSCR", [8 * 128 * 383], F32, kind="Internal")
    RD = nc.dram_tensor("RD", [S, 8], F32, kind=dk).ap() if debug else None

    bX = cx.buf("x_in")
    bOut = cx.buf("out")
    bQK = [cx.buf("QK%d" % i) for i in range(NBLK)]
    bVT = cx.buf("VT")
    bX1 = cx.buf("X1")
    bXS = cx.buf("XS")
    bYS = cx.buf("YS")
    bFS = cx.buf("FSCR")
    bW = cx.buf("wdram")

    es0 = ExitStack()

    def sb(es, name, shape, dt=F32):
        return es.enter_context(nc.sbuf_tensor("sb_" + name, shape, dt)).ap()

    ps_all = nc.alloc_psum_tensor("ps_all", [128, 8, 512], F32).ap()
    pb = [ps_all[:, i, :] for i in range(8)]
    bpb = [cx.buf("pb%d" % i) for i in range(8)]

    ident_bf = sb(es0, "ident_bf", [128, 128], BF16)
    ident_f = sb(es0, "ident_f", [128, 128], F32)
    ones_bf = sb(es0, "ones_bf", [128, 128], BF16)
    ones_f = sb(es0, "ones_f", [128, 128], F32)
    eps_t = sb(es0, "eps_t", [128, 1], F32)
    bC = cx.buf("consts")
    cx.dma("sp", ident_bf[:, :], A["ident_bf"][:, :], [bW], [bC])
    cx.dma("sp", ident_f[:, :], A["ident_f"][:, :], [bW], [bC], join=True)
    cx.op("dve", [], [bC], lambda e: e.memset(ones_bf[:, :], 1.0))
    cx.op("dve", [], [bC], lambda e: e.memset(ones_f[:, :], 1.0 / 128))
    cx.op("dve", [], [bC], lambda e: e.memset(eps_t[:, :], EPS))
    gates = sb(es0, "gates", [128, 16, 2], F32)
    rows_i = sb(es0, "rows_i", [128, 16, 2], I32)
    bGates = cx.buf("gates")
    bRows = cx.buf("rows")

    esA = ExitStack()
    HT = sb(esA, "HT", [128, 16, S], BF16)
    bHT = [cx.buf("HTlo"), cx.buf("HThi")]
    MT = HT
    bMT = cx.buf("MT")
    BT = sb(esA, "BT", [128, 8, 256], F32)
    cfar = sb(esA, "cfar", [128, 8], F32)
    nlam = sb(esA, "nlam", [128, 1], F32)
    g08 = sb(esA, "g08", [128, 1], F32)
    gng = sb(esA, "gng", [128, 8], F32)
    bBT = cx.buf("BT")
    bLam = cx.buf("lam")
    bSm = cx.buf("smallA")
    cx.dma("sp", g08[:, :], A["gsub"][:, :], [bW], [bSm])
    cx.dma("sp", gng[:, :], A["gng"][:, :], [bW], [bSm], join=True)
    cx.op("dve", [bSm], [bSm], lambda e: e.tensor_scalar(out=g08[:, :], in0=g08[:, :], scalar1=0.8, scalar2=None, op0=ALU.mult))

    esP0 = ExitStack()
    es = esP0
    if True:
        lv = sb(es, "lv", [128, 4, 64], F32)
        bl = cx.buf("lv")
        for i, nm in enumerate(["lq1", "lk1", "lq2", "lk2"]):
            cx.dma("sp", lv[:, i, :], A[nm].to_broadcast([128, 64]), [bW], [bl], join=(i > 0))
        pr = sb(es, "lpr", [128, 2, 64], F32)
        sm = sb(es, "lsm", [128, 4], F32)
        cx.op("dve", [bl], [bl], lambda e: e.tensor_tensor(out=pr[:, 0, :], in0=lv[:, 0, :], in1=lv[:, 1, :], op=ALU.mult))
        cx.op("dve", [bl], [bl], lambda e: e.tensor_tensor(out=pr[:, 1, :], in0=lv[:, 2, :], in1=lv[:, 3, :], op=ALU.mult))
        cx.op("dve", [bl], [bl], lambda e: e.reduce_sum(out=sm[:, 0:2], in_=pr[:, :, :], axis=AX.X))
        cx.op("act", [bl], [bl], lambda e: e.activation(out=sm[:, 2:4], in_=sm[:, 0:2], func=AF.Exp))
        cx.op("dve", [bl], [bLam], lambda e: e.tensor_tensor(out=nlam[:, :], in0=sm[:, 3:4], in1=sm[:, 2:3], op=ALU.subtract))
        cx.op("dve", [bLam], [bLam], lambda e: e.tensor_scalar(out=nlam[:, :], in0=nlam[:, :], scalar1=-0.2, scalar2=None, op0=ALU.add))
        rbe = sb(es, "rbe", [33, 8], F32)
        rbx = sb(es, "rbx", [33, 8, 128], F32)
        oh = sb(es, "oh", [33, 383], F32)
        ft = [sb(es, "ft%d" % i, [128, 383], F32) for i in range(8)]
        brb = cx.buf("rbe")
        cx.op("dve", [], [brb], lambda e: e.memset(rbe[:, :], NEG))
        cx.dma("sp", rbe[0:32, :], A["rel_bias"][:, :], [bW], [brb])
        cx.dma("sp", oh[:, :], A["oh"][:, :], [bW], [brb], join=True)
        cx.op("dve", [brb], [brb], lambda e: e.tensor_copy(out=rbx[:, :, :], in_=rbe[:, :].unsqueeze(2).to_broadcast([33, 8, 128])))
        bft = [cx.buf("ft%d" % i) for i in range(8)]
        fs_ap = fscr_t.ap().rearrange("(h p j) -> h p j", h=8, p=128)
        for h in range(8):
            cx.op("pe", [brb], [bpb[h % 2]], lambda e: e.matmul(pb[h % 2][:, 0:383], lhsT=rbx[:, h, :], rhs=oh[:, :], start=True, stop=True))
            if h % 2 == 0:
                cx.op("act", [bpb[h % 2]], [bft[h]], lambda e: e.activation(out=ft[h][:, :], in_=pb[h % 2][:, 0:383], func=AF.Copy))
            else:
                cx.op("dve", [bpb[h % 2]], [bft[h]], lambda e: e.tensor_copy(out=ft[h][:, :], in_=pb[h % 2][:, 0:383]))
            cx.op("dve", [bft[h]], [bBT], lambda e: e.tensor_copy(out=cfar[:, h:h + 1], in_=ft[h][:, 382:383]))
            cx.dma("pool", fs_ap[h], ft[h][:, :], [bft[h]], [bFS])
            toe = bass.AP(tensor=fscr_t, offset=h * 128 * 383 + 127, ap=[[382, 128], [1, 256]])
            cx.dma("pool", BT[:, h, :], toe, [bFS], [bBT])

    with ExitStack() as es:
        xt = [sb(es, "xt%d" % i, [128, D], F32) for i in range(3)]
        hb = [sb(es, "hb%d" % i, [128, D], BF16) for i in range(3)]
        junk = sb(es, "junk", [128, D], BF16)
        grep = sb(es, "grep", [128, D], F32)
        stt = [sb(es, "stt%d" % i, [128, 4], F32) for i in range(3)]
        bxt = [cx.buf("xt%d" % i) for i in range(3)]
        bhb = [cx.buf("hb%d" % i) for i in range(3)]
        bjunk = cx.buf("junk")
        bgrep = cx.buf("grep")
        bst = [cx.buf("stt%d" % i) for i in range(3)]
        cx.dma("act", grep[:, :], A["g_attn"].to_broadcast([128, D]), [bW], [bgrep])
        def p1_front(t):
            s = t % 3
            cx.dma("sp", xt[s][:, :], A["x"][t * 128:(t + 1) * 128, :], [bX], [bxt[s]])
            cx.op("act", [bxt[s]], [bjunk, bst[s]], lambda e: e.activation(out=junk[:, :], in_=xt[s][:, :], func=AF.Square, accum_out=stt[s][:, 0:1]))
            cx.op("act", [bst[s], bC], [bst[s]], lambda e: e.activation(out=stt[s][:, 1:2], in_=stt[s][:, 0:1], func=AF.Ln, scale=1.0 / D, bias=eps_t[:, 0:1]))
            cx.op("act", [bst[s]], [bst[s]], lambda e: e.activation(out=stt[s][:, 2:3], in_=stt[s][:, 1:2], func=AF.Exp, scale=-0.5))
            cx.op("dve", [bxt[s], bst[s], bgrep], [bhb[s]], lambda e: e.scalar_tensor_tensor(out=hb[s][:, :], in0=xt[s][:, :], scalar=stt[s][:, 2:3], in1=grep[:, :], op0=ALU.mult, op1=ALU.mult))

        def p1_back(t):
            s = t % 3
            for half in range(2):
                bk = 2 * (t % 2) + half
                pbf = pb[bk].bitcast(BF16)
                for k in range(8):
                    kc = half * 8 + k
                    cx.op("pe", [bhb[s], bC], [bpb[bk]], lambda e: e.transpose(pbf[:, k * 128:(k + 1) * 128], hb[s][:, kc * 128:(kc + 1) * 128], ident_bf[:, :]))
                dst = HT[:, half * 8:(half + 1) * 8, t * 128:(t + 1) * 128]
                srcv = pbf[:, :].rearrange("p (k c) -> p k c", k=8)
                if half == 0:
                    cx.op("act", [bpb[bk]], [bHT[0]], lambda e: e.activation(out=dst, in_=srcv, func=AF.Copy))
                else:
                    cx.op("dve", [bpb[bk]], [bHT[1]], lambda e: e.tensor_copy(out=dst, in_=srcv))

        p1_front(0)
        for t in range(16):
            if t + 1 < 16:
                p1_front(t + 1)
            p1_back(t)
        cx.barrier()
    esP0.close()

    with ExitStack() as es:
        zt = sb(es, "zt", [128, 4, D], BF16)
        bzt = cx.buf("zt")
        cx.op("pool", [], [bzt], lambda e: e.memset(zt[:, :, :], 0.0))
        XSz = XS.rearrange("(c p r) d -> c p r d", p=128, r=4)
        for c in range(NSLOT // 512):
            cx.dma("act", XSz[c], zt[:, :, :], [bzt], [bXS], join=True)
        wsl = [sb(es, "wsl%d" % i, [128, 2048], BF16) for i in range(4)]
        stg = [sb(es, "stg%d" % i, [128, S], BF16) for i in range(2)]
        bws = [cx.buf("wsl%d" % i) for i in range(4)]
        bstg = [cx.buf("stg%d" % i) for i in range(2)]
        n = 0
        for blk in range(NBLK):
            ws = wsl[blk % 4]
            cx.dma("pool", ws[:, :], A["wfm"][blk], [bW], [bws[blk % 4]])
            for tc in range(4):
                bk = n % 4
                for kc in range(16):
                    cx.op("pe", [bws[blk % 4]] + bHT, [bpb[bk]], lambda e: e.matmul(pb[bk][:, :], lhsT=ws[:, kc * 128:(kc + 1) * 128], rhs=HT[:, kc, tc * 512:(tc + 1) * 512], start=(kc == 0), stop=(kc == 15)))
                dst = stg[blk % 2][:, tc * 512:(tc + 1) * 512]
                if n % 2 == 0:
                    cx.op("act", [bpb[bk]], [bstg[blk % 2]], lambda e: e.activation(out=dst, in_=pb[bk][:, :], func=AF.Copy))
                else:
                    cx.op("dve", [bpb[bk]], [bstg[blk % 2]], lambda e: e.tensor_copy(out=dst, in_=pb[bk][:, :]))
                n += 1
            cx.dma("sp", QK[blk], stg[blk % 2][:, :], [bstg[blk % 2]], [bQK[blk]], sem_buf=bstg[blk % 2])
        wtb = [sb(es, "wtb%d" % i, [128, 16, 512], BF16) for i in range(2)]
        st2 = [sb(es, "st2%d" % i, [128, 512], BF16) for i in range(2)]
        bwt = [cx.buf("wtb%d" % i) for i in range(2)]
        bst2 = [cx.buf("st2%d" % i) for i in range(2)]
        n = 0
        for cb in range(4):
            wt = wtb[cb % 2]
            for hf in range(2):
                cx.dma("pool", wt[:, hf * 8:(hf + 1) * 8, :], A["wtm"][cb, :, hf * 8:(hf + 1) * 8, :], [bW], [bwt[cb % 2]], join=(hf > 0))
            for t in range(16):
                bk = 4 + n % 4
                for kc in range(16):
                    cx.op("pe", [bwt[cb % 2]] + bHT, [bpb[bk]], lambda e: e.matmul(pb[bk][:, :], lhsT=HT[:, kc, t * 128:(t + 1) * 128], rhs=wt[:, kc, :], start=(kc == 0), stop=(kc == 15)))
                if n % 2 == 0:
                    cx.op("act", [bpb[bk]], [bst2[n % 2]], lambda e: e.activation(out=st2[n % 2][:, :], in_=pb[bk][:, :], func=AF.Copy))
                else:
                    cx.op("dve", [bpb[bk]], [bst2[n % 2]], lambda e: e.tensor_copy(out=st2[n % 2][:, :], in_=pb[bk][:, :]))
                cx.dma("act", VT[t * 128:(t + 1) * 128, cb * 512:(cb + 1) * 512], st2[n % 2][:, :], [bst2[n % 2]], [bVT], join=True, sem_buf=bst2[n % 2])
                n += 1
        cx.barrier()

    if stop_after == "P2":
        return _finish(nc, cx, es0, esA, out_d, bOut)

    with ExitStack() as es:
        VTv = VT.rearrange("(t p) c -> p t c", p=128)
        qr_all = [sb(es, "qr%d" % i, [128, S], BF16) for i in range(4)]
        kr_all = [sb(es, "kr%d" % i, [128, S], BF16) for i in range(4)]
        bqr_all = [cx.buf("qr%d" % i) for i in range(4)]
        bkr_all = [cx.buf("kr%d" % i) for i in range(4)]
        with ExitStack() as es2:
            ab = [sb(es2, "ab%d" % i, [128, S], BF16) for i in range(8)]
            cos2 = sb(es2, "cos2", [128, S], F32)
            sin2 = sb(es2, "sin2", [128, S], F32)
            qdec = [sb(es2, "qdec%d" % i, [128, S], F32) for i in range(2)]
            tA = [sb(es2, "tA%d" % i, [128, S], F32) for i in range(2)]
            tB = [sb(es2, "tB%d" % i, [128, S], F32) for i in range(2)]
            bab = [cx.buf("ab%d" % i) for i in range(8)]
            brope0 = cx.buf("rope0")
            bqd = [cx.buf("qdec%d" % i) for i in range(2)]
            btA = [cx.buf("tA%d" % i) for i in range(2)]
            btB = [cx.buf("tB%d" % i) for i in range(2)]
            cx.dma("sp", cos2[:, :], A["cos2"][:, :], [bW], [brope0])
            cx.dma("act", sin2[:, :], A["sin2"][:, :], [bW], [brope0], join=True)
            for hp in range(4):
                par = hp % 2
                eng = "dve" if par == 0 else "pool"
                for i in range(4):
                    cx.dma("sp" if i % 2 == 0 else "act", ab[4 * par + i][:, :], QK[16 + hp * 4 + i], [bQK[16 + hp * 4 + i]], [bab[4 * par + i]])
                cx.dma("sp", qdec[par][:, :], A["qdec"][hp], [bW], [bqd[par]])
                a_ = [ab[4 * par + i] for i in range(4)]
                ba_ = [bab[4 * par + i] for i in range(4)]
                cx.op(eng, [ba_[0], brope0], [btA[par]], lambda e: e.tensor_tensor(out=tA[par][:, :], in0=a_[0][:, :], in1=cos2[:, :], op=ALU.mult))
                cx.op(eng, [ba_[1], brope0], [btB[par]], lambda e: e.tensor_tensor(out=tB[par][:, :], in0=a_[1][:, :], in1=sin2[:, :], op=ALU.mult))
                cx.op(eng, [btA[par], btB[par]], [btA[par]], lambda e: e.tensor_tensor(out=tA[par][:, :], in0=tA[par][:, :], in1=tB[par][:, :], op=ALU.add))
                cx.op(eng, [btA[par], bqd[par]], [bqr_all[hp]], lambda e: e.tensor_tensor(out=qr_all[hp][:, :], in0=tA[par][:, :], in1=qdec[par][:, :], op=ALU.mult))
                cx.op(eng, [ba_[2], brope0], [btA[par]], lambda e: e.tensor_tensor(out=tA[par][:, :], in0=a_[2][:, :], in1=cos2[:, :], op=ALU.mult))
                cx.op(eng, [ba_[3], brope0], [btB[par]], lambda e: e.tensor_tensor(out=tB[par][:, :], in0=a_[3][:, :], in1=sin2[:, :], op=ALU.mult))
                cx.op(eng, [btA[par], btB[par]], [bkr_all[hp]], lambda e: e.tensor_tensor(out=kr_all[hp][:, :], in0=tA[par][:, :], in1=tB[par][:, :], op=ALU.add))
            cx.barrier()

        qT = [sb(es, "qT%d" % i, [128, S], BF16) for i in range(2)]
        kz = [[sb(es, "kz%d_%d" % (i, c), [128, S], BF16) for c in range(2)] for i in range(2)]
        vt3 = [sb(es, "vt%d" % i, [128, 16, 128], BF16) for i in range(2)]
        PT3 = [sb(es, "PT%d" % i, [128, 2, 256], BF16) for i in range(3)]
        tmpn = [sb(es, "tmpn%d" % i, [128, 2, 256], F32) for i in range(2)]
        fa3 = [sb(es, "fa%d" % i, [128, 2, 256], F32) for i in range(2)]
        fb3 = [sb(es, "fb%d" % i, [128, 256], F32) for i in range(2)]
        fc3 = [sb(es, "fc%d" % i, [128, 256], F32) for i in range(2)]
        fd3 = [sb(es, "fd%d" % i, [128, 256], F32) for i in range(2)]
        bq = [cx.buf("q%d" % i) for i in range(2)]
        bk_ = [cx.buf("k%d" % i) for i in range(2)]
        bv3 = [cx.buf("v%d" % i) for i in range(2)]
        bPT3 = [cx.buf("PT%d" % i) for i in range(3)]
        btn = [cx.buf("tmpn%d" % i) for i in range(2)]
        bfa3 = [cx.buf("fa%d" % i) for i in range(2)]
        bfb3 = [cx.buf("fb%d" % i) for i in range(2)]
        bfc3 = [cx.buf("fc%d" % i) for i in range(2)]
        bfd3 = [cx.buf("fd%d" % i) for i in range(2)]
        p3 = [pb[i].rearrange("p (c q) -> p c q", c=2) for i in range(8)]
        vt4 = [sb(es, "rvt%d" % i, [128, 16, 128], BF16) for i in range(2)]
        rg4 = [sb(es, "rg%d" % i, [128, S], BF16) for i in range(2)]
        bcj = sb(es, "bcj", [128, 8, 19], F32)
        mqk = sb(es, "mqk", [128, 128], F32)
        PT4 = [sb(es, "rPT%d" % i, [128, 512], BF16) for i in range(3)]
        fa4 = [sb(es, "rfa%d" % i, [128, 512], F32) for i in range(2)]
        fb4 = [sb(es, "rfb%d" % i, [128, 512], F32) for i in range(2)]
        fc4 = [sb(es, "rfc%d" % i, [128, 512], F32) for i in range(2)]
        fd4 = [sb(es, "rfd%d" % i, [128, 512], F32) for i in range(2)]
        brope = cx.buf("rope")
        bv4 = [cx.buf("rv%d" % i) for i in range(2)]
        brg4 = [cx.buf("rg%d" % i) for i in range(2)]
        bPT4 = [cx.buf("rPT%d" % i) for i in range(3)]
        bfa4 = [cx.buf("rfa%d" % i) for i in range(2)]
        bfb4 = [cx.buf("rfb%d" % i) for i in range(2)]
        bfc4 = [cx.buf("rfc%d" % i) for i in range(2)]
        bfd4 = [cx.buf("rfd%d" % i) for i in range(2)]
        cx.dma("sp", bcj[:, :, :], A["bcj"][:, :, :], [bW], [brope])
        cx.dma("sp", mqk[:, :], A["mqk"][:, :], [bW], [brope], join=True)

        def gen3():
            st = {"s": 0, "near": 0, "pend": None, "pt": 0}
            for i in range(2):
                cx.op("pool", [], [bk_[i]], lambda e: e.memset(kz[i][0][64:128, :], 0.0))
                cx.op("pool", [], [bk_[i]], lambda e: e.memset(kz[i][1][0:64, :], 0.0))

            def load_head(h):
                s = h % 2
                cx.dma("sp", qT[s][:, :], QK[h], [bQK[h]], [bq[s]])
                cx.dma("sp", kz[s][0][0:64, :], QK[8 + h][0:64, :], [bQK[8 + h]], [bk_[s]])
                cx.dma("sp", kz[s][1][64:128, :], QK[8 + h][64:128, :], [bQK[8 + h]], [bk_[s]], join=True)
                cx.dma("sp", vt3[s][:, :, :], VTv[:, :, h * 128:(h + 1) * 128], [bVT], [bv3[s]])

            load_head(0)
            for h in range(NH):
                s = h % 2
                if h + 1 < NH:
                    load_head(h + 1)
                for ch in range(8):
                    q0 = ch * 256
                    nj = 2 * ch + 2
                    ot, su = p3[2], p3[3]
                    bot_, bsu = bpb[2], bpb[3]
                    sbank = {}

                    def s_mm(j):
                        co = max(0, j * 128 - q0)
                        slot = st["s"] % 2
                        st["s"] += 1
                        sbank[j] = slot
                        for c in range(2):
                            cx.op("pe", [bq[s], bk_[s]], [bpb[slot]], lambda e: e.matmul(
                                p3[slot][:, c, co:256], lhsT=kz[s][c][:, j * 128:(j + 1) * 128],
                                rhs=qT[s][:, q0 + co:q0 + 256], start=True, stop=True))

                    s_mm(0)
                    for j in range(nj):
                        if j + 1 < nj:
                            s_mm(j + 1)
                        co = max(0, j * 128 - q0)
                        ne = min(j * 128 + 256, q0 + 256) - q0
                        fs = max(ne, co)
                        slot = sbank[j]
                        pt = PT3[st["pt"] % 3]
                        bpt = bPT3[st["pt"] % 3]
                        st["pt"] += 1
                        sview = p3[slot]
                        if ne > co:
                            w = ne - co
                            b0 = q0 + co - j * 128
                            tn = tmpn[st["near"] % 2]
                            btn_ = btn[st["near"] % 2]
                            st["near"] += 1
                            cx.op("dve", [bpb[slot], bBT], [btn_], lambda e: e.scalar_tensor_tensor(
                                out=tn[:, :, 0:w], in0=sview[:, :, co:ne], scalar=0.125,
                                in1=BT[:, h, b0:b0 + w].unsqueeze(1).to_broadcast([128, 2, w]), op0=ALU.mult, op1=ALU.add))
                            cx.op("act", [btn_], [bpt], lambda e: e.activation(out=pt[:, :, co:ne], in_=tn[:, :, 0:w], func=AF.Exp))
                        if fs < 256:
                            cx.op("act", [bpb[slot], bBT], [bpt], lambda e: e.activation(
                                out=pt[:, :, fs:256], in_=sview[:, :, fs:256], func=AF.Exp, scale=0.125, bias=cfar[:, h:h + 1]))
                        if co == 0:
                            ptf = pt[:, :, :].rearrange("p c q -> p (c q)")
                            cx.op("pe", [bv3[s], bpt], [bot_], lambda e: e.matmul(
                                pb[2][:, :], lhsT=vt3[s][:, j, :], rhs=ptf, start=(j == 0), stop=(j == nj - 1)))
                            cx.op("pe", [bC, bpt], [bsu], lambda e: e.matmul(
                                pb[3][:, :], lhsT=ones_bf[:, :], rhs=ptf, start=(j == 0), stop=(j == nj - 1)))
                        else:
                            for c in range(2):
                                cx.op("pe", [bv3[s], bpt], [bot_], lambda e: e.matmul(
                                    ot[:, c, co:256], lhsT=vt3[s][:, j, :], rhs=pt[:, c, co:256], start=(j == 0), stop=(j == nj - 1 and c == 1)))
                                cx.op("pe", [bC, bpt], [bsu], lambda e: e.matmul(
                                    su[:, c, co:256], lhsT=ones_bf[:, :], rhs=pt[:, c, co:256], start=(j == 0), stop=(j == nj - 1 and c == 1)))
                        if j == 0 and st["pend"] is not None:
                            st["pend"]()
                            st["pend"] = None
                        yield
                    par = (h * 8 + ch) % 2
                    fa_, fb_, fc_, fd_ = fa3[par], fb3[par], fc3[par], fd3[par]
                    cx.op("act", [bsu], [bfa3[par]], lambda e: e.activation(out=fa_[:, :, :], in_=su[:, :, :], func=AF.Ln))
                    cx.op("act", [bfa3[par]], [bfa3[par]], lambda e: e.activation(out=fa_[:, :, :], in_=fa_[:, :, :], func=AF.Exp, scale=-1.0))
                    cx.op("dve", [bot_, bfa3[par]], [bfa3[par]], lambda e: e.tensor_tensor(out=fa_[:, :, :], in0=ot[:, :, :], in1=fa_[:, :, :], op=ALU.mult))
                    cx.op("dve", [bfa3[par], bLam], [bfb3[par]], lambda e: e.scalar_tensor_tensor(
                        out=fb_[:, :], in0=fa_[:, 1, :], scalar=nlam[:, 0:1], in1=fa_[:, 0, :], op0=ALU.mult, op1=ALU.add))
                    cx.op("dve", [bfb3[par]], [bfc3[par]], lambda e: e.tensor_tensor(out=fc_[:, :], in0=fb_[:, :], in1=fb_[:, :], op=ALU.mult))

                    def fin2(h=h, q0=q0, par=par, fb_=fb_, fc_=fc_, fd_=fd_):
                        slot = st["s"] % 2
                        msv = p3[slot][:, 0, :]
                        cx.op("pe", [bfc3[par], bC], [bpb[slot]], lambda e: e.matmul(msv, lhsT=ones_f[:, :], rhs=fc_[:, :], start=True, stop=True))
                        cx.op("act", [bpb[slot], bC], [bfd3[par]], lambda e: e.activation(out=fd_[:, :], in_=msv, func=AF.Ln, bias=eps_t[:, 0:1]))
                        cx.op("act", [bfd3[par]], [bfd3[par]], lambda e: e.activation(out=fd_[:, :], in_=fd_[:, :], func=AF.Exp, scale=-0.5))
                        cx.op("dve", [bfb3[par], bfd3[par], bSm], [bMT], lambda e: e.scalar_tensor_tensor(
                            out=MT[:, h, q0:q0 + 256], in0=fb_[:, :], scalar=g08[:, 0:1], in1=fd_[:, :], op0=ALU.mult, op1=ALU.mult))

                    st["pend"] = fin2
                    yield
            st["pend"]()

        def gen4():
            st = {"s": 0, "alt": 0, "pend": None, "pt": 0}

            def load_head(h):
                s = h % 2
                cx.dma("act", vt4[s][:, :, :], VTv[:, :, 1024 + h * 128:1024 + (h + 1) * 128], [bVT], [bv4[s]])
                cx.dma("act", rg4[s][:, :], QK[32 + h], [bQK[32 + h]], [brg4[s]])
                cx.op("act", [brg4[s]], [brg4[s]], lambda e: e.activation(out=rg4[s][:, :], in_=rg4[s][:, :], func=AF.Silu))

            load_head(0)
            for h in range(NH):
                hp, sub = h // 2, h % 2
                s = h % 2
                p0 = sub * 64
                qr, kr = qr_all[hp], kr_all[hp]
                bqr, bkr = bqr_all[hp], bkr_all[hp]
                for qc in range(4):
                    if qc == 1 and h + 1 < NH:
                        load_head(h + 1)
                    q0 = qc * 512
                    nj = 4 * qc + 4
                    sbank = {}

                    def s_mm(j):
                        co = max(0, j * 128 - q0)
                        bank = 4 + st["s"] % 2
                        st["s"] += 1
                        sbank[j] = bank
                        cx.op("pe", [bqr, bkr], [bpb[bank]], lambda e: e.matmul(
                            pb[bank][:, co:512], lhsT=kr[p0:p0 + 64, j * 128:(j + 1) * 128],
                            rhs=qr[p0:p0 + 64, q0 + co:q0 + 512], start=True, stop=True))

                    s_mm(0)
                    for j in range(nj):
                        if j + 1 < nj:
                            s_mm(j + 1)
                        co = max(0, j * 128 - q0)
                        bank = sbank[j]
                        pt = PT4[st["pt"] % 3]
                        bpt = bPT4[st["pt"] % 3]
                        st["pt"] += 1
                        mi = (q0 - 128 * j) // 128 + 3
                        sc = bcj[:, h, mi:mi + 1]
                        fs = co
                        if j * 128 >= q0:
                            cx.op("dve", [bpb[bank], brope], [bpt], lambda e: e.scalar_tensor_tensor(
                                out=pt[:, co:co + 128], in0=pb[bank][:, co:co + 128], scalar=sc, in1=mqk[:, :], op0=ALU.mult, op1=ALU.mult))
                            fs = co + 128
                        if fs < 512:
                            if st["alt"] % 3 == 0:
                                cx.op("act", [bpb[bank], brope], [bpt], lambda e: e.activation(out=pt[:, fs:512], in_=pb[bank][:, fs:512], func=AF.Copy, scale=sc))
                            else:
                                cx.op("dve", [bpb[bank], brope], [bpt], lambda e: e.tensor_scalar(out=pt[:, fs:512], in0=pb[bank][:, fs:512], scalar1=sc, scalar2=None, op0=ALU.mult))
                            st["alt"] += 1
                        cx.op("pe", [bv4[s], bpt], [bpb[6]], lambda e: e.matmul(
                            pb[6][:, co:512], lhsT=vt4[s][:, j, :], rhs=pt[:, co:512], start=(j == 0), stop=(j == nj - 1)))
                        if j == 0 and st["pend"] is not None:
                            st["pend"]()
                            st["pend"] = None
                        yield
                    par = (h * 4 + qc) % 2
                    fa_, fb_, fc_, fd_ = fa4[par], fb4[par], fc4[par], fd4[par]
                    cx.op("act", [bpb[6]], [bfa4[par]], lambda e: e.activation(out=fa_[:, :], in_=pb[6][:, :], func=AF.Copy))
                    cx.op("dve", [bfa4[par]], [bfb4[par]], lambda e: e.tensor_tensor(out=fb_[:, :], in0=fa_[:, :], in1=fa_[:, :], op=ALU.mult))

                    def fin2(h=h, s=s, q0=q0, par=par, fa_=fa_, fb_=fb_, fc_=fc_, fd_=fd_):
                        cx.op("pe", [bfa4[par], bC], [bpb[7]], lambda e: e.matmul(pb[7][:, :], lhsT=ones_f[:, :], rhs=fa_[:, :], start=True, stop=True))
                        cx.op("act", [bpb[7]], [bfc4[par]], lambda e: e.activation(out=fc_[:, :], in_=pb[7][:, :], func=AF.Copy))
                        cx.op("pe", [bfb4[par], bC], [bpb[7]], lambda e: e.matmul(pb[7][:, :], lhsT=ones_f[:, :], rhs=fb_[:, :], start=True, stop=True))
                        cx.op("dve", [bfc4[par]], [bfd4[par]], lambda e: e.tensor_tensor(out=fd_[:, :], in0=fc_[:, :], in1=fc_[:, :], op=ALU.mult))
                        cx.op("dve", [bpb[7], bfd4[par]], [bfd4[par]], lambda e: e.tensor_tensor(out=fd_[:, :], in0=pb[7][:, :], in1=fd_[:, :], op=ALU.subtract))
                        cx.op("dve", [bfd4[par]], [bfd4[par]], lambda e: e.tensor_scalar(out=fd_[:, :], in0=fd_[:, :], scalar1=0.0, scalar2=None, op0=ALU.max))
                        cx.op("act", [bfd4[par], bC], [bfd4[par]], lambda e: e.activation(out=fd_[:, :], in_=fd_[:, :], func=AF.Ln, bias=eps_t[:, 0:1]))
                        cx.op("act", [bfd4[par]], [bfd4[par]], lambda e: e.activation(out=fd_[:, :], in_=fd_[:, :], func=AF.Exp, scale=-0.5))
                        cx.op("dve", [bfa4[par], bfc4[par]], [bfa4[par]], lambda e: e.tensor_tensor(out=fa_[:, :], in0=fa_[:, :], in1=fc_[:, :], op=ALU.subtract))
                        cx.op("dve", [bfa4[par], bfd4[par], bSm], [bfa4[par]], lambda e: e.scalar_tensor_tensor(
                            out=fa_[:, :], in0=fa_[:, :], scalar=gng[:, h:h + 1], in1=fd_[:, :], op0=ALU.mult, op1=ALU.mult))
                        cx.op("dve", [bfa4[par], brg4[s]], [bMT], lambda e: e.tensor_tensor(
                            out=MT[:, 8 + h, q0:q0 + 512], in0=fa_[:, :], in1=rg4[s][:, q0:q0 + 512], op=ALU.mult))

                    st["pend"] = fin2
                    yield
            st["pend"]()

        g3, g4 = gen3(), gen4()
        alive3, alive4 = True, True
        while alive3 or alive4:
            for _ in range(2):
                if alive3:
                    try:
                        next(g3)
                    except StopIteration:
                        alive3 = False
            if alive4:
                try:
                    next(g4)
                except StopIteration:
                    alive4 = False
        cx.barrier()

    if stop_after == "P4":
        if debug:
            cx.dma("sp", MTD[:, :, :], MT[:, :, :], [bMT], [bOut])
        return _finish(nc, cx, es0, esA, out_d, bOut)

    with ExitStack() as es:
        wob = [sb(es, "wob%d" % i, [128, 16, 512], BF16) for i in range(2)]
        xs_t = [sb(es, "xs_t%d" % i, [128, 512], F32) for i in range(2)]
        x1p = [sb(es, "x1p%d" % i, [128, 512], F32) for i in range(2)]
        bwo = [cx.buf("wob%d" % i) for i in range(2)]
        bxs = [cx.buf("xs_t%d" % i) for i in range(2)]
        bx1p = [cx.buf("x1p%d" % i) for i in range(2)]
        wov = A["wout"].rearrange("(kc p) f -> p kc f", p=128)
        n = 0
        for dc in range(4):
            wo = wob[dc % 2]
            for hf in range(2):
                cx.dma("pool", wo[:, hf * 8:(hf + 1) * 8, :], wov[:, hf * 8:(hf + 1) * 8, dc * 512:(dc + 1) * 512], [bW], [bwo[dc % 2]], join=(hf > 0))
            for t in range(16):
                bk = n % 4
                s = n % 2
                cx.dma("sp", xs_t[s][:, :], A["x"][t * 128:(t + 1) * 128, dc * 512:(dc + 1) * 512], [bX], [bxs[s]])
                for kc in range(16):
                    cx.op("pe", [bwo[dc % 2], bMT], [bpb[bk]], lambda e: e.matmul(pb[bk][:, :], lhsT=MT[:, kc, t * 128:(t + 1) * 128], rhs=wo[:, kc, :], start=(kc == 0), stop=(kc == 15)))
                cx.op("dve", [bpb[bk], bxs[s]], [bx1p[s]], lambda e: e.tensor_tensor(out=x1p[s][:, :], in0=pb[bk][:, :], in1=xs_t[s][:, :], op=ALU.add))
                cx.dma("act", X1[t * 128:(t + 1) * 128, dc * 512:(dc + 1) * 512], x1p[s][:, :], [bx1p[s]], [bX1], join=True, sem_buf=bx1p[s])
                n += 1
        cx.barrier()
    esA.close()

    if stop_after == "P5":
        return _finish(nc, cx, es0, None, out_d, bOut)

    _phase_b(nc, cx, A, sb, pb, bpb, bW, bC, X1, bX1, XS, bXS, YS, bYS, out_d, bOut,
             ident_f, ident_bf, ones_bf, eps_t, gates, rows_i, bGates, bRows, RD, debug, stop_after)
    return _finish(nc, cx, es0, None, out_d, bOut)


def _finish(nc, cx, es0, esA, out_d, bOut):
    cx.barrier()
    return nc


def _phase_b(nc, cx, A, sb, pb, bpb, bW, bC, X1, bX1, XS, bXS, YS, bYS, out_d, bOut,
             ident_f, ident_bf, ones_bf, eps_t, gates, rows_i, bGates, bRows, RD, debug, stop_after):
    BIG = 20000.0
    bnd_reg = nc.gpsimd.alloc_register("bnd")
    nc.gpsimd.reg_mov(bnd_reg, NSLOT - 1)
    with ExitStack() as es:
        x1t = [sb(es, "x1t%d" % i, [128, D], F32) for i in range(2)]
        h2f = [sb(es, "h2f%d" % i, [128, D], F32) for i in range(2)]
        h2b = sb(es, "h2b", [128, 16, D], BF16)
        junk = sb(es, "junk6", [128, D], BF16)
        h2T = [sb(es, "h2T%d" % i, [128, 16, 128], F32) for i in range(2)]
        wr = sb(es, "wr", [128, 16, 36], F32)
        gffn = sb(es, "gffn", [128, D], F32)
        rbias = sb(es, "rbias", [128, 36], F32)
        Mall = sb(es, "Mall", [128, 16, 32], BF16)
        lstrict = sb(es, "lstrict", [128, 128], BF16)
        iota_e = sb(es, "iota_e", [128, 32], F32)
        LG = sb(es, "LG", [128, 16, 36], F32)
        stA = sb(es, "stA", [128, 16, 4], F32)
        gmax = sb(es, "gmax", [128, 16], F32)
        t4 = sb(es, "t4", [128, 16, 4], F32)
        gmk = sb(es, "gmk", [128, 16, 4], F32)
        sc16 = sb(es, "sc16", [128, 12, 16], F32)
        mi = sb(es, "mi", [128, 16, 32], F32)
        mi2 = sb(es, "mi2", [128, 16, 32], F32)
        M1 = sb(es, "M1", [128, 16, 32], F32)
        M2 = sb(es, "M2", [128, 16, 32], F32)
        tt = sb(es, "tt6", [128, 16, 32], F32)
        csa = sb(es, "csa", [128, 16, 32], F32)
        pre = sb(es, "pre", [128, 16, 32], F32)
        cnt = sb(es, "cnt", [128, 16, 32], F32)
        rowf = sb(es, "rowf", [128, 16, 2], F32)
        bx1t = [cx.buf("x1t%d" % i) for i in range(2)]
        bh2f = [cx.buf("h2f%d" % i) for i in range(2)]
        bh2b = [cx.buf("h2b%d" % i) for i in range(16)]
        bjunk = cx.buf("junk6")
        bh2T = [cx.buf("h2T%d" % i) for i in range(2)]
        bK = cx.buf("p6const")
        bLG = cx.buf("LG")
        bstA = cx.buf("stA")
        bs = cx.buf("p6small")
        cx.dma("sp", wr[:, :, :], A["wr"][:, :, :], [bW], [bK])
        cx.dma("sp", gffn[:, :], A["g_ffn"].to_broadcast([128, D]), [bW], [bK], join=True)
        cx.dma("sp", rbias[:, :], A["rbias"].to_broadcast([128, 36]), [bW], [bK], join=True)
        cx.dma("sp", lstrict[:, :], A["lstrict"][:, :], [bW], [bK], join=True)
        cx.dma("sp", iota_e[:, :], A["iota_e"][:, :], [bW], [bK], join=True)
        def p6_front(t):
            s = t % 2
            cx.dma("sp", x1t[s][:, :], X1[t * 128:(t + 1) * 128, :], [bX1], [bx1t[s]])
            cx.op("act", [bx1t[s]], [bjunk, bstA], lambda e: e.activation(out=junk[:, :], in_=x1t[s][:, :], func=AF.Square, accum_out=stA[:, t, 0:1]))
            cx.op("act", [bstA, bC], [bstA], lambda e: e.activation(out=stA[:, t, 1:2], in_=stA[:, t, 0:1], func=AF.Ln, scale=1.0 / D, bias=eps_t[:, 0:1]))
            cx.op("act", [bstA], [bstA], lambda e: e.activation(out=stA[:, t, 2:3], in_=stA[:, t, 1:2], func=AF.Exp, scale=-0.5))
            cx.op("dve", [bx1t[s], bstA, bK], [bh2f[s]], lambda e: e.scalar_tensor_tensor(out=h2f[s][:, :], in0=x1t[s][:, :], scalar=stA[:, t, 2:3], in1=gffn[:, :], op0=ALU.mult, op1=ALU.mult))
            cx.op("act", [bh2f[s]], [bh2b[t]], lambda e: e.activation(out=h2b[:, t, :], in_=h2f[s][:, :], func=AF.Copy))

        def p6_back(t):
            s = t % 2
            for g4 in range(4):
                bk = 4 * s + g4
                for k in range(4):
                    kc = g4 * 4 + k
                    cx.op("pe", [bh2f[s], bC], [bpb[bk]], lambda e: e.transpose(pb[bk][:, k * 128:(k + 1) * 128], h2f[s][:, kc * 128:(kc + 1) * 128], ident_f[:, :]))
                dst = h2T[s][:, g4 * 4:(g4 + 1) * 4, :]
                srcv = pb[bk].rearrange("p (k c) -> p k c", k=4)
                if g4 % 2 == 0:
                    cx.op("act", [bpb[bk]], [bh2T[s]], lambda e: e.activation(out=dst, in_=srcv, func=AF.Copy))
                else:
                    cx.op("dve", [bpb[bk]], [bh2T[s]], lambda e: e.tensor_copy(out=dst, in_=srcv))
            bk = 4 * s
            for kc in range(16):
                cx.op("pe", [bh2T[s], bK], [bpb[bk]], lambda e: e.matmul(pb[bk][:, 0:36], lhsT=h2T[s][:, kc, :], rhs=wr[:, kc, :], start=(kc == 0), stop=(kc == 15)))
            cx.op("dve", [bpb[bk], bK], [bLG], lambda e: e.tensor_tensor(out=LG[:, t, :], in0=pb[bk][:, 0:36], in1=rbias[:, :], op=ALU.add))

        p6_front(0)
        for t in range(16):
            if t + 1 < 16:
                p6_front(t + 1)
            p6_back(t)
        lg4 = LG[:, :, 0:4]
        lgin = LG[:, :, 4:36].rearrange("p t (g e) -> p t g e", g=4)
        S_ = lambda i: sc16[:, i, :]
        bc4 = lambda ap: ap.unsqueeze(2).to_broadcast([128, 16, 4])
        bc32 = lambda ap: ap.unsqueeze(2).to_broadcast([128, 16, 32])

        def dve(fn, rd=(), wr_=None):
            cx.op("dve", [bs] + list(rd), [bs if wr_ is None else wr_], fn)

        def act(fn):
            cx.op("act", [bs], [bs], fn)

        dve(lambda e: e.tensor_reduce(out=gmax[:, :], in_=lg4, axis=AX.X, op=ALU.max), rd=[bLG])
        dve(lambda e: e.tensor_tensor(out=t4[:, :, :], in0=lg4, in1=bc4(gmax[:, :]), op=ALU.subtract), rd=[bLG])
        act(lambda e: e.activation(out=t4[:, :, :], in_=t4[:, :, :], func=AF.Exp))
        dve(lambda e: e.reduce_sum(out=S_(0), in_=t4[:, :, :], axis=AX.X))
        act(lambda e: e.activation(out=S_(1), in_=S_(0), func=AF.Ln))
        act(lambda e: e.activation(out=S_(2), in_=S_(1), func=AF.Exp, scale=-1.0))
        dve(lambda e: e.tensor_tensor(out=gmk[:, :, :], in0=lg4, in1=bc4(gmax[:, :]), op=ALU.is_equal), rd=[bLG])
        dve(lambda e: e.tensor_scalar(out=gmk[:, :, :], in0=gmk[:, :, :], scalar1=-1.0, scalar2=1e9, op0=ALU.add, op1=ALU.mult))
        dve(lambda e: e.tensor_tensor(out=mi[:, :, :].rearrange("p t (g e) -> p t g e", g=4), in0=lgin,
                                      in1=gmk[:, :, :].unsqueeze(3).to_broadcast([128, 16, 4, 8]), op=ALU.add), rd=[bLG])
        dve(lambda e: e.tensor_reduce(out=S_(3), in_=mi[:, :, :], axis=AX.X, op=ALU.max))
        dve(lambda e: e.tensor_tensor(out=M1[:, :, :], in0=mi[:, :, :], in1=bc32(S_(3)), op=ALU.is_equal))
        dve(lambda e: e.scalar_tensor_tensor(out=mi2[:, :, :], in0=M1[:, :, :], scalar=-1e9, in1=mi[:, :, :], op0=ALU.mult, op1=ALU.add))
        dve(lambda e: e.tensor_reduce(out=S_(4), in_=mi2[:, :, :], axis=AX.X, op=ALU.max))
        dve(lambda e: e.tensor_tensor(out=M2[:, :, :], in0=mi2[:, :, :], in1=bc32(S_(4)), op=ALU.is_equal))
        dve(lambda e: e.tensor_tensor(out=S_(5), in0=S_(4), in1=S_(3), op=ALU.subtract))
        act(lambda e: e.activation(out=S_(5), in_=S_(5), func=AF.Exp))
        dve(lambda e: e.tensor_scalar(out=S_(6), in0=S_(5), scalar1=1.0, scalar2=None, op0=ALU.add))
        act(lambda e: e.activation(out=S_(6), in_=S_(6), func=AF.Ln))
        act(lambda e: e.activation(out=S_(6), in_=S_(6), func=AF.Exp, scale=-1.0))
        dve(lambda e: e.tensor_tensor(out=S_(7), in0=S_(5), in1=S_(6), op=ALU.mult))
        dve(lambda e: e.tensor_tensor(out=S_(6), in0=S_(6), in1=S_(2), op=ALU.mult))
        dve(lambda e: e.tensor_tensor(out=S_(7), in0=S_(7), in1=S_(2), op=ALU.mult))
        bMall = cx.buf("Mall")
        dve(lambda e: e.tensor_tensor(out=Mall[:, :, :], in0=M1[:, :, :], in1=M2[:, :, :], op=ALU.add), wr_=bMall)
        mflat = Mall[:, :, :].rearrange("p t e -> p (t e)")
        cx.op("pe", [bMall, bK], [bpb[0]], lambda e: e.matmul(pb[0][:, :], lhsT=lstrict[:, :], rhs=mflat, start=True, stop=True))
        cx.op("pe", [bMall, bC], [bpb[1]], lambda e: e.matmul(pb[1][:, :], lhsT=ones_bf[:, :], rhs=mflat, start=True, stop=True))
        dve(lambda e: e.tensor_copy(out=csa[:, :, :].rearrange("p t e -> p (t e)"), in_=pb[1][:, :]), rd=[bpb[1]])
        dve(lambda e: e.memset(pre[:, 0, :], 0.0))
        for t in range(1, 16):
            dve(lambda e: e.tensor_tensor(out=pre[:, t, :], in0=pre[:, t - 1, :], in1=csa[:, t - 1, :], op=ALU.add))
        dve(lambda e: e.tensor_tensor(out=cnt[:, :, :].rearrange("p t e -> p (t e)"), in0=pb[0][:, :], in1=pre[:, :, :].rearrange("p t e -> p (t e)"), op=ALU.add), rd=[bpb[0]])
        for k, Mk in ((0, M1), (1, M2)):
            dve(lambda e: e.tensor_tensor(out=tt[:, :, :], in0=Mk[:, :, :], in1=cnt[:, :, :], op=ALU.mult))
            dve(lambda e: e.reduce_sum(out=S_(8), in_=tt[:, :, :], axis=AX.X))
            dve(lambda e: e.tensor_tensor(out=tt[:, :, :], in0=Mk[:, :, :], in1=iota_e[:, :].unsqueeze(1).to_broadcast([128, 16, 32]), op=ALU.mult), rd=[bK])
            dve(lambda e: e.reduce_sum(out=S_(9), in_=tt[:, :, :], axis=AX.X))
            dve(lambda e: e.scalar_tensor_tensor(out=S_(10), in0=S_(9), scalar=float(CAP), in1=S_(8), op0=ALU.mult, op1=ALU.add))
            dve(lambda e: e.tensor_scalar(out=S_(11), in0=S_(8), scalar1=float(CAP), scalar2=None, op0=ALU.is_lt))
            dve(lambda e: e.scalar_tensor_tensor(out=S_(10), in0=S_(10), scalar=-BIG, in1=S_(11), op0=ALU.add, op1=ALU.mult))
            dve(lambda e: e.tensor_scalar(out=rowf[:, :, k], in0=S_(10), scalar1=BIG, scalar2=None, op0=ALU.add))
            dve(lambda e: e.tensor_tensor(out=gates[:, :, k], in0=S_(6 + k), in1=S_(11), op=ALU.mult), wr_=bGates)
        dve(lambda e: e.tensor_copy(out=rows_i[:, :, :], in_=rowf[:, :, :]), wr_=bRows)
        for t in range(16):
            for k in range(2):
                cx.dma("pool", None, None, [bh2b[t], bRows], [bXS], join=True, sem_buf=bh2b[t], fn=lambda e: e.indirect_dma_start(
                    out=XS[:, :], out_offset=bass.IndirectOffsetOnAxis(ap=rows_i[:, t, k:k + 1], axis=0),
                    in_=h2b[:, t, :], in_offset=None, bounds_check=bnd_reg, oob_is_err=False))
        cx.barrier()

    if stop_after == "P6":
        return

    with ExitStack() as es:
        NST = CAP // 128
        xtok = [sb(es, "xtok%d" % i, [128, NST, D], BF16) for i in range(2)]
        xsT = [sb(es, "xsT%d" % i, [128, 16, CAP], BF16) for i in range(2)]
        slab = [sb(es, "slab%d" % i, [128, 8, FF], BF16) for i in range(4)]
        wdb = [sb(es, "wdb%d" % i, [128, 8, D], BF16) for i in range(2)]
        sg = sb(es, "sg", [128, 8, CAP], F32)
        hT = [sb(es, "hT%d" % i, [128, 8, CAP], BF16) for i in range(2)]
        yst = [sb(es, "yst%d" % i, [128, D], BF16) for i in range(2)]
        bxtok = [cx.buf("xtok%d" % i) for i in range(2)]
        bxsT = [cx.buf("xsT%d" % i) for i in range(2)]
        bslab = [cx.buf("slab%d" % i) for i in range(4)]
        bwdb = [cx.buf("wdb%d" % i) for i in range(2)]
        bsg = [cx.buf("sg%d" % i) for i in range(4)]
        bhT = [cx.buf("hT%d" % i) for i in range(2)]
        byst = [cx.buf("yst%d" % i) for i in range(2)]
        ucount = 0
        ntr = 0
        ndn = 0
        for ex in range(NE):
            s = ex % 2
            cx.dma("sp", xtok[s][:, :, :], XS[ex * CAP:(ex + 1) * CAP, :].rearrange("(st p) d -> p st d", p=128), [bXS], [bxtok[s]])
            for st_ in range(NST):
                for half in range(2):
                    bk = 6 + ntr % 2
                    pbf = pb[bk].bitcast(BF16)
                    for k in range(8):
                        kc = half * 8 + k
                        cx.op("pe", [bxtok[s], bC], [bpb[bk]], lambda e: e.transpose(pbf[:, k * 128:(k + 1) * 128], xtok[s][:, st_, kc * 128:(kc + 1) * 128], ident_bf[:, :]))
                    dst = xsT[s][:, half * 8:(half + 1) * 8, st_ * 128:(st_ + 1) * 128]
                    src = pbf[:, :].rearrange("p (k c) -> p k c", k=8)
                    if ntr % 2 == 0:
                        cx.op("act", [bpb[bk]], [bxsT[s]], lambda e: e.activation(out=dst, in_=src, func=AF.Copy))
                    else:
                        cx.op("dve", [bpb[bk]], [bxsT[s]], lambda e: e.tensor_copy(out=dst, in_=src))
                    ntr += 1
            for (wname, isgate) in (("wg", True), ("wu", False)):
                sls = [slab[ucount % 4], slab[(ucount + 1) % 4]]
                bsls = [bslab[ucount % 4], bslab[(ucount + 1) % 4]]
                ucount += 2
                wv = A[wname][ex].rearrange("(kc p) f -> p kc f", p=128)
                for kh in range(2):
                    for hf in range(2):
                        cx.dma("pool", sls[kh][:, hf * 4:(hf + 1) * 4, :], wv[:, kh * 8 + hf * 4:kh * 8 + (hf + 1) * 4, :], [bW], [bsls[kh]], join=(hf > 0))
                for f in range(8):
                    bk = f // 2
                    c0 = (f % 2) * CAP
                    for kc in range(16):
                        sl = sls[kc // 8]
                        cx.op("pe", [bsls[kc // 8], bxsT[s]], [bpb[bk]], lambda e: e.matmul(pb[bk][:, c0:c0 + CAP], lhsT=sl[:, kc % 8, f * 128:(f + 1) * 128], rhs=xsT[s][:, kc, :], start=(kc == 0), stop=(kc == 15)))
                    if f % 2 == 1:
                        sgv = sg[:, f - 1:f + 1, :].rearrange("p a c -> p (a c)")
                        if isgate:
                            cx.op("act", [bpb[bk]], [bsg[bk]], lambda e: e.activation(out=sgv, in_=pb[bk][:, :], func=AF.Silu))
                        else:
                            dst = hT[s][:, f - 1:f + 1, :].rearrange("p a c -> p (a c)")
                            cx.op("dve", [bpb[bk], bsg[bk]], [bhT[s]], lambda e: e.tensor_tensor(out=dst, in0=pb[bk][:, :], in1=sgv, op=ALU.mult))
            wdv = A["wd"][ex].rearrange("(kc p) f -> p kc f", p=128)
            for hf in range(4):
                cx.dma("pool", wdb[s][:, hf * 2:(hf + 1) * 2, :], wdv[:, hf * 2:(hf + 1) * 2, :], [bW], [bwdb[s]], join=(hf > 0))
            for st_ in range(NST):
                ys_ = yst[st_ % 2]
                for dc in range(4):
                    bk = 4 + ndn % 2
                    for kc in range(8):
                        cx.op("pe", [bhT[s], bwdb[s]], [bpb[bk]], lambda e: e.matmul(pb[bk][:, :], lhsT=hT[s][:, kc, st_ * 128:(st_ + 1) * 128], rhs=wdb[s][:, kc, dc * 512:(dc + 1) * 512], start=(kc == 0), stop=(kc == 7)))
                    if ndn % 2 == 0:
                        cx.op("act", [bpb[bk]], [byst[st_ % 2]], lambda e: e.activation(out=ys_[:, dc * 512:(dc + 1) * 512], in_=pb[bk][:, :], func=AF.Copy))
                    else:
                        cx.op("dve", [bpb[bk]], [byst[st_ % 2]], lambda e: e.tensor_copy(out=ys_[:, dc * 512:(dc + 1) * 512], in_=pb[bk][:, :]))
                    ndn += 1
                r0 = ex * CAP + st_ * 128
                cx.dma("sp", YS[r0:r0 + 128, :], ys_[:, :], [byst[st_ % 2]], [bYS], join=True, sem_buf=byst[st_ % 2])
        cx.barrier()

    with ExitStack() as es:
        R8 = 3
        y1 = [sb(es, "y1_%d" % i, [128, D], BF16) for i in range(R8)]
        y2 = [sb(es, "y2_%d" % i, [128, D], BF16) for i in range(R8)]
        x1t = [sb(es, "x8t%d" % i, [128, D], F32) for i in range(R8)]
        acc = [sb(es, "acc8_%d" % i, [128, D], F32) for i in range(2)]
        gfin = sb(es, "gfin", [128, D], F32)
        junk = sb(es, "junk8", [128, D], BF16)
        ot = [sb(es, "ot%d" % i, [128, D], F32) for i in range(2)]
        sm = [sb(es, "sm8_%d" % i, [128, 4], F32) for i in range(2)]
        by1 = [cx.buf("y1_%d" % i) for i in range(R8)]
        by2 = [cx.buf("y2_%d" % i) for i in range(R8)]
        bx = [cx.buf("x8t%d" % i) for i in range(R8)]
        bacc = [cx.buf("acc8_%d" % i) for i in range(2)]
        bg = cx.buf("gfin")
        bj = cx.buf("junk8")
        bot = [cx.buf("ot%d" % i) for i in range(2)]
        bs = [cx.buf("sm8_%d" % i) for i in range(2)]
        cx.dma("sp", gfin[:, :], A["g_fin"].to_broadcast([128, D]), [bW], [bg])
        for i in range(R8):
            cx.op("dve", [], [by1[i]], lambda e: e.memset(y1[i][:, :], 0.0))
            cx.op("pool", [], [by2[i]], lambda e: e.memset(y2[i][:, :], 0.0))
        def fetch(t):
            s = t % R8
            for (yy, byy, k) in ((y1, by1, 0), (y2, by2, 1)):
                cx.dma("pool", None, None, [bYS, bRows], [byy[s]], fn=lambda e: e.indirect_dma_start(
                    out=yy[s][:, :], out_offset=None, in_=YS[:, :],
                    in_offset=bass.IndirectOffsetOnAxis(ap=rows_i[:, t, k:k + 1], axis=0),
                    bounds_check=bnd_reg, oob_is_err=False))
            cx.dma("sp", x1t[s][:, :], X1[t * 128:(t + 1) * 128, :], [bX1], [bx[s]])

        def p8_front(t):
            s = t % R8
            a = t % 2
            cx.op("dve", [by1[s], bx[s], bGates], [bacc[a]], lambda e: e.scalar_tensor_tensor(out=acc[a][:, :], in0=y1[s][:, :], scalar=gates[:, t, 0:1], in1=x1t[s][:, :], op0=ALU.mult, op1=ALU.add))
            cx.op("dve", [by2[s], bGates, bacc[a]], [bacc[a]], lambda e: e.scalar_tensor_tensor(out=acc[a][:, :], in0=y2[s][:, :], scalar=gates[:, t, 1:2], in1=acc[a][:, :], op0=ALU.mult, op1=ALU.add))
            if t + R8 < 16:
                fetch(t + R8)
            cx.op("act", [bacc[a]], [bj, bs[a]], lambda e: e.activation(out=junk[:, :], in_=acc[a][:, :], func=AF.Square, accum_out=sm[a][:, 0:1]))
            cx.op("act", [bs[a], bC], [bs[a]], lambda e: e.activation(out=sm[a][:, 1:2], in_=sm[a][:, 0:1], func=AF.Ln, scale=1.0 / D, bias=eps_t[:, 0:1]))
            cx.op("act", [bs[a]], [bs[a]], lambda e: e.activation(out=sm[a][:, 2:3], in_=sm[a][:, 1:2], func=AF.Exp, scale=-0.5))

        def p8_back(t):
            a = t % 2
            cx.op("dve", [bacc[a], bs[a], bg], [bot[a]], lambda e: e.scalar_tensor_tensor(out=ot[a][:, :], in0=acc[a][:, :], scalar=sm[a][:, 2:3], in1=gfin[:, :], op0=ALU.mult, op1=ALU.mult))
            cx.dma("sp", out_d[t * 128:(t + 1) * 128, :], ot[a][:, :], [bot[a]], [bOut], join=True, sem_buf=bot[a])

        for t in range(R8):
            fetch(t)
        p8_front(0)
        for t in range(16):
            if t + 1 < 16:
                p8_front(t + 1)
            p8_back(t)


def kernel(**inputs):
    wl = _layout_weights(inputs)
    cs = _consts()
    base = dict(wl)
    base.update(cs)
    x = np.asarray(inputs["x"], dtype=np.float32)
    nb = x.shape[0]
    nc = build()
    in_maps = []
    for b in range(nb):
        m = dict(base)
        m["x"] = np.ascontiguousarray(x[b])
        in_maps.append(m)
    res = run_bass_kernel_spmd(nc, in_maps, core_ids=list(range(nb)))
    return np.stack([np.asarray(r["out"], dtype=np.float32) for r in res.results], axis=0)
```

```python
import math
from contextlib import ExitStack

import numpy as np
import ml_dtypes
import concourse.bass as bass
import concourse.mybir as mybir
from concourse.bass_utils import run_bass_kernel_spmd

F32 = mybir.dt.float32
BF16 = mybir.dt.bfloat16
I32 = mybir.dt.int32
AF = mybir.ActivationFunctionType
ALU = mybir.AluOpType
AX = mybir.AxisListType

S = 2048
D = 2048
NH = 8
CAP = 256
NE = 32
NSLOT = NE * CAP
FF = 1024
EPS = 1e-6
NBLK = 40
NEG = -30000.0


class Buf:
    def __init__(self, name):
        self.name = name
        self.w = None
        self.r = {}
        self.dsem = None
        self.dcnt = 0


class DSem:
    def __init__(self, sem):
        self.sem = sem
        self.cnt = 0


class Ctx:
    def share(self, bufs, name):
        ds = DSem(self.nc.alloc_semaphore("d_" + name))
        self.dbufs.append(ds)
        for b in bufs:
            b.dsem = ds

    def __init__(self, nc):
        self.nc = nc
        self.engs = {"pe": nc.tensor, "act": nc.scalar, "dve": nc.vector,
                     "pool": nc.gpsimd, "sp": nc.sync}
        self.sem = {e: nc.alloc_semaphore("s_" + e) for e in self.engs}
        self.cnt = {e: 0 for e in self.engs}
        self.seen = {e: {} for e in self.engs}
        self.dbufs = []

    def buf(self, name):
        return Buf(name)

    def _wait(self, e, ev):
        sem, val = ev
        key = id(sem)
        if e == "pe" and sem is self.sem["pe"]:
            return
        if self.seen[e].get(key, 0) >= val:
            return
        self.engs[e].wait_ge(sem, val)
        self.seen[e][key] = val

    def _deps(self, e, reads, writes, join=False):
        for b in reads:
            if b.w is not None:
                self._wait(e, b.w)
        for b in writes:
            if b.w is not None and not join:
                self._wait(e, b.w)
            for ev in b.r.values():
                self._wait(e, ev)

    def _mark(self, ev, reads, writes):
        for b in reads:
            b.r[id(ev[0])] = ev
        for b in writes:
            b.w = ev
            b.r = {}

    def op(self, e, reads, writes, fn):
        self._deps(e, reads, writes)
        inst = fn(self.engs[e])
        self.cnt[e] += 1
        inst.then_inc(self.sem[e], 1)
        self._mark((self.sem[e], self.cnt[e]), reads, writes)

    def dma(self, q, out, in_, reads, writes, join=False, fn=None, sem_buf=None, **kw):
        (wb0,) = writes
        self._deps(q, reads, writes, join=join)
        wb = sem_buf if sem_buf is not None else wb0
        if wb.dsem is None:
            wb.dsem = DSem(self.nc.alloc_semaphore("d_" + wb.name))
            self.dbufs.append(wb.dsem)
        if fn is None:
            inst = self.engs[q].dma_start(out=out, in_=in_, **kw)
        else:
            inst = fn(self.engs[q])
        wb.dsem.cnt += 1
        inst.then_inc(wb.dsem.sem, 16)
        self._mark((wb.dsem.sem, 16 * wb.dsem.cnt), reads, writes)

    def barrier(self):
        for e in self.engs:
            for e2 in self.engs:
                if e2 != e and self.cnt[e2] > 0:
                    self._wait(e, (self.sem[e2], self.cnt[e2]))
            for ds in self.dbufs:
                if ds.cnt:
                    self._wait(e, (ds.sem, 16 * ds.cnt))


def _t5_onehot():
    d = np.arange(-127, 256)
    dd = np.maximum(d, 0)
    lr = (np.log(np.maximum(dd, 1).astype(np.float32) / np.float32(16)) /
          np.float32(math.log(128 / 16))).astype(np.float32)
    large = 16 + (lr * np.float32(16)).astype(np.int32)
    large = np.minimum(large, 31)
    bucket = np.where(dd < 16, dd, large)
    oh = np.zeros((33, 383), np.float32)
    for j in range(383):
        if d[j] < 0:
            oh[32, j] = 1.0
        else:
            oh[bucket[j], j] = 1.0
    return oh


def _consts():
    c = {}
    c["ident_bf"] = np.eye(128, dtype=np.float32).astype(ml_dtypes.bfloat16)
    c["ident_f"] = np.eye(128, dtype=np.float32)
    tp = np.arange(128)
    c["lstrict"] = (tp[:, None] < tp[None, :]).astype(np.float32).astype(ml_dtypes.bfloat16)
    c["iota_e"] = np.tile(np.arange(32, dtype=np.float32)[None], (128, 1))
    c["oh"] = _t5_onehot()
    inv = (10000.0 ** (-np.arange(0, 64, 2, dtype=np.float32) / np.float32(64))).astype(np.float32)
    ang = np.arange(S, dtype=np.float32)[:, None] * inv[None, :]
    cos = np.cos(ang).astype(np.float32).T
    sin = np.sin(ang).astype(np.float32).T
    cos2 = np.zeros((128, S), np.float32)
    sin2 = np.zeros((128, S), np.float32)
    for p in range(128):
        w = p % 64
        cos2[p] = cos[w % 32]
        sin2[p] = -sin[w] if w < 32 else sin[w - 32]
    c["cos2"] = cos2
    c["sin2"] = sin2
    hh = np.arange(8, dtype=np.float32)
    log_g = np.log(np.float32(1.0) - np.exp2(np.float32(-5.0) - hh)).astype(np.float32)
    t = np.arange(S, dtype=np.float32)
    lg64 = log_g.astype(np.float64)
    tm = (np.arange(S) % 512).astype(np.float64)
    qd = np.exp(tm[None, :] * lg64[:, None])
    qdec = np.zeros((4, 128, S), np.float32)
    for hp in range(4):
        qdec[hp, 0:64] = qd[2 * hp][None, :]
        qdec[hp, 64:128] = qd[2 * hp + 1][None, :]
    c["qdec"] = qdec
    kk = np.arange(128, dtype=np.float64)
    mm = np.arange(-3, 16, dtype=np.float64)
    bcj = 0.125 * np.exp(-kk[:, None, None] * lg64[None, :, None] + 128.0 * mm[None, None, :] * lg64[None, :, None])
    c["bcj"] = bcj.astype(np.float32)
    c["mqk"] = (kk[:, None] <= kk[None, :]).astype(np.float32)
    return c


def _fm_cols():
    blocks = []
    for h in range(8):
        blocks.append(list(range(h * 128, (h + 1) * 128)))
    for h in range(8):
        blocks.append(list(range(1024 + h * 128, 1024 + (h + 1) * 128)))
    swap = [(d + 32) % 64 for d in range(64)]
    for hp in range(4):
        for base in (3072, 3584):
            a, b = [], []
            for sub in range(2):
                h = 2 * hp + sub
                a += [base + h * 64 + d for d in range(64)]
                b += [base + h * 64 + swap[d] for d in range(64)]
            blocks.append(a)
            blocks.append(b)
    for h in range(8):
        blocks.append(list(range(5120 + h * 128, 5120 + (h + 1) * 128)))
    assert len(blocks) == NBLK
    return blocks


def _layout_weights(inp):
    w_in = np.asarray(inp["w_in"][0], dtype=np.float32)
    blocks = _fm_cols()
    w4 = w_in.reshape(16, 128, 6144)
    wfm = np.empty((NBLK, 128, 16, 128), np.float32)
    for i, cols in enumerate(blocks):
        wfm[i] = w4[:, :, cols].transpose(1, 0, 2)
    vcols = list(range(2048, 3072)) + list(range(4096, 5120))
    wv = w4[:, :, vcols]
    wtm = np.ascontiguousarray(wv.reshape(16, 128, 4, 512).transpose(2, 1, 0, 3))
    wr = np.concatenate([np.asarray(inp["w_group_router"][0])] +
                        [np.asarray(inp["w_inner_router"][0][g]) for g in range(4)], axis=1)
    wr = np.ascontiguousarray(wr.reshape(16, 128, 36).transpose(1, 0, 2)).astype(np.float32)
    rb = np.concatenate([np.asarray(inp["b_group"][0]).reshape(1, 4),
                         np.asarray(inp["b_inner"][0]).reshape(1, 32)], axis=1).astype(np.float32)
    d = {
        "wfm": wfm.reshape(NBLK, 128, 2048),
        "wtm": wtm,
        "wout": np.ascontiguousarray(inp["w_out"][0], dtype=np.float32),
        "wg": np.ascontiguousarray(inp["w_gate_exp"][0], dtype=np.float32),
        "wu": np.ascontiguousarray(inp["w_up_exp"][0], dtype=np.float32),
        "wd": np.ascontiguousarray(inp["w_down_exp"][0], dtype=np.float32),
        "wr": wr,
        "rbias": rb,
        "rel_bias": np.ascontiguousarray(inp["rel_bias"], dtype=np.float32),
        "g_attn": np.asarray(inp["attn_norm_g"][0], np.float32).reshape(1, D),
        "g_ffn": np.asarray(inp["ffn_norm_g"][0], np.float32).reshape(1, D),
        "g_fin": np.asarray(inp["final_g"], np.float32).reshape(1, D),
        "lq1": np.asarray(inp["lambda_q1"], np.float32).reshape(1, 64),
        "lk1": np.asarray(inp["lambda_k1"], np.float32).reshape(1, 64),
        "lq2": np.asarray(inp["lambda_q2"], np.float32).reshape(1, 64),
        "lk2": np.asarray(inp["lambda_k2"], np.float32).reshape(1, 64),
        "gsub": np.ascontiguousarray(np.asarray(inp["diff_subln_g"][0], np.float32).reshape(128, 1)),
        "gng": np.ascontiguousarray(np.asarray(inp["ret_gn_g"][0], np.float32).reshape(8, 128).T),
    }
    return d


IN_SPECS = [
    ("x", [S, D], F32), ("wfm", [NBLK, 128, 2048], F32), ("wtm", [4, 128, 16, 512], F32),
    ("wout", [2048, 2048], F32), ("wg", [NE, D, FF], F32), ("wu", [NE, D, FF], F32),
    ("wd", [NE, FF, D], F32), ("wr", [128, 16, 36], F32), ("rbias", [1, 36], F32),
    ("rel_bias", [32, 8], F32), ("g_attn", [1, D], F32), ("g_ffn", [1, D], F32),
    ("g_fin", [1, D], F32), ("lq1", [1, 64], F32), ("lk1", [1, 64], F32), ("lq2", [1, 64], F32),
    ("lk2", [1, 64], F32), ("gsub", [128, 1], F32), ("gng", [128, 8], F32),
    ("ident_bf", [128, 128], BF16), ("ident_f", [128, 128], F32), ("lstrict", [128, 128], BF16),
    ("iota_e", [128, 32], F32), ("oh", [33, 383], F32), ("cos2", [128, S], F32),
    ("sin2", [128, S], F32), ("qdec", [4, 128, S], F32), ("bcj", [128, 8, 19], F32), ("mqk", [128, 128], F32),
]


def build(stop_after=None, debug=False):
    nc = bass.Bass("TRN2", target_bir_lowering=False)
    cx = Ctx(nc)
    A = {}
    for name, shape, dt in IN_SPECS:
        A[name] = nc.dram_tensor(name, shape, dt, kind="ExternalInput").ap()
    out_d = nc.dram_tensor("out", [S, D], F32, kind="ExternalOutput").ap()
    dk = "ExternalOutput" if debug else "Internal"
    QK = nc.dram_tensor("QK", [NBLK, 128, S], BF16, kind=dk).ap()
    VT = nc.dram_tensor("VT", [S, 2048], BF16, kind=dk).ap()
    X1 = nc.dram_tensor("X1", [S, D], F32, kind=dk).ap()
    MTD = nc.dram_tensor("MTD", [128, 16, S], BF16, kind=dk).ap() if debug else None
    XS = nc.dram_tensor("XS", [NSLOT, D], BF16, kind="Internal").ap()
    YS = nc.dram_tensor("YS", [NSLOT, D], BF16, kind="Internal").ap()
    fscr_t = nc.dram_tensor("FSCR", [8 * 128 * 383], F32, kind="Internal")
    RD = nc.dram_tensor("RD", [S, 8], F32, kind=dk).ap() if debug else None

    bX = cx.buf("x_in")
    bOut = cx.buf("out")
    bQK = [cx.buf("QK%d" % i) for i in range(NBLK)]
    bVT = cx.buf("VT")
    bX1 = cx.buf("X1")
    bXS = cx.buf("XS")
    bYS = cx.buf("YS")
    bFS = cx.buf("FSCR")
    bW = cx.buf("wdram")

    es0 = ExitStack()

    def sb(es, name, shape, dt=F32):
        return es.enter_context(nc.sbuf_tensor("sb_" + name, shape, dt)).ap()

    ps_all = nc.alloc_psum_tensor("ps_all", [128, 8, 512], F32).ap()
    pb = [ps_all[:, i, :] for i in range(8)]
    bpb = [cx.buf("pb%d" % i) for i in range(8)]

    ident_bf = sb(es0, "ident_bf", [128, 128], BF16)
    ident_f = sb(es0, "ident_f", [128, 128], F32)
    ones_bf = sb(es0, "ones_bf", [128, 128], BF16)
    ones_f = sb(es0, "ones_f", [128, 128], F32)
    eps_t = sb(es0, "eps_t", [128, 1], F32)
    bC = cx.buf("consts")
    cx.dma("sp", ident_bf[:, :], A["ident_bf"][:, :], [bW], [bC])
    cx.dma("sp", ident_f[:, :], A["ident_f"][:, :], [bW], [bC], join=True)
    cx.op("dve", [], [bC], lambda e: e.memset(ones_bf[:, :], 1.0))
    cx.op("dve", [], [bC], lambda e: e.memset(ones_f[:, :], 1.0 / 128))
    cx.op("dve", [], [bC], lambda e: e.memset(eps_t[:, :], EPS))
    gates = sb(es0, "gates", [128, 16, 2], F32)
    rows_i = sb(es0, "rows_i", [128, 16, 2], I32)
    bGates = cx.buf("gates")
    bRows = cx.buf("rows")

    esA = ExitStack()
    HT = sb(esA, "HT", [128, 16, S], BF16)
    bHT = [cx.buf("HTlo"), cx.buf("HThi")]
    MT = HT
    bMT = cx.buf("MT")
    BT = sb(esA, "BT", [128, 8, 256], F32)
    cfar = sb(esA, "cfar", [128, 8], F32)
    nlam = sb(esA, "nlam", [128, 1], F32)
    g08 = sb(esA, "g08", [128, 1], F32)
    gng = sb(esA, "gng", [128, 8], F32)
    bBT = cx.buf("BT")
    bLam = cx.buf("lam")
    bSm = cx.buf("smallA")
    cx.dma("sp", g08[:, :], A["gsub"][:, :], [bW], [bSm])
    cx.dma("sp", gng[:, :], A["gng"][:, :], [bW], [bSm], join=True)
    cx.op("dve", [bSm], [bSm], lambda e: e.tensor_scalar(out=g08[:, :], in0=g08[:, :], scalar1=0.8, scalar2=None, op0=ALU.mult))

    esP0 = ExitStack()
    es = esP0
    if True:
        lv = sb(es, "lv", [128, 4, 64], F32)
        bl = cx.buf("lv")
        for i, nm in enumerate(["lq1", "lk1", "lq2", "lk2"]):
            cx.dma("sp", lv[:, i, :], A[nm].to_broadcast([128, 64]), [bW], [bl], join=(i > 0))
        pr = sb(es, "lpr", [128, 2, 64], F32)
        sm = sb(es, "lsm", [128, 4], F32)
        cx.op("dve", [bl], [bl], lambda e: e.tensor_tensor(out=pr[:, 0, :], in0=lv[:, 0, :], in1=lv[:, 1, :], op=ALU.mult))
        cx.op("dve", [bl], [bl], lambda e: e.tensor_tensor(out=pr[:, 1, :], in0=lv[:, 2, :], in1=lv[:, 3, :], op=ALU.mult))
        cx.op("dve", [bl], [bl], lambda e: e.reduce_sum(out=sm[:, 0:2], in_=pr[:, :, :], axis=AX.X))
        cx.op("act", [bl], [bl], lambda e: e.activation(out=sm[:, 2:4], in_=sm[:, 0:2], func=AF.Exp))
        cx.op("dve", [bl], [bLam], lambda e: e.tensor_tensor(out=nlam[:, :], in0=sm[:, 3:4], in1=sm[:, 2:3], op=ALU.subtract))
        cx.op("dve", [bLam], [bLam], lambda e: e.tensor_scalar(out=nlam[:, :], in0=nlam[:, :], scalar1=-0.2, scalar2=None, op0=ALU.add))
        rbe = sb(es, "rbe", [33, 8], F32)
        rbx = sb(es, "rbx", [33, 8, 128], F32)
        oh = sb(es, "oh", [33, 383], F32)
        ft = [sb(es, "ft%d" % i, [128, 383], F32) for i in range(8)]
        brb = cx.buf("rbe")
        cx.op("dve", [], [brb], lambda e: e.memset(rbe[:, :], NEG))
        cx.dma("sp", rbe[0:32, :], A["rel_bias"][:, :], [bW], [brb])
        cx.dma("sp", oh[:, :], A["oh"][:, :], [bW], [brb], join=True)
        cx.op("dve", [brb], [brb], lambda e: e.tensor_copy(out=rbx[:, :, :], in_=rbe[:, :].unsqueeze(2).to_broadcast([33, 8, 128])))
        bft = [cx.buf("ft%d" % i) for i in range(8)]
        fs_ap = fscr_t.ap().rearrange("(h p j) -> h p j", h=8, p=128)
        for h in range(8):
            cx.op("pe", [brb], [bpb[h % 2]], lambda e: e.matmul(pb[h % 2][:, 0:383], lhsT=rbx[:, h, :], rhs=oh[:, :], start=True, stop=True))
            if h % 2 == 0:
                cx.op("act", [bpb[h % 2]], [bft[h]], lambda e: e.activation(out=ft[h][:, :], in_=pb[h % 2][:, 0:383], func=AF.Copy))
            else:
                cx.op("dve", [bpb[h % 2]], [bft[h]], lambda e: e.tensor_copy(out=ft[h][:, :], in_=pb[h % 2][:, 0:383]))
            cx.op("dve", [bft[h]], [bBT], lambda e: e.tensor_copy(out=cfar[:, h:h + 1], in_=ft[h][:, 382:383]))
            cx.dma("pool", fs_ap[h], ft[h][:, :], [bft[h]], [bFS])
            toe = bass.AP(tensor=fscr_t, offset=h * 128 * 383 + 127, ap=[[382, 128], [1, 256]])
            cx.dma("pool", BT[:, h, :], toe, [bFS], [bBT])

    with ExitStack() as es:
        xt = [sb(es, "xt%d" % i, [128, D], F32) for i in range(3)]
        hb = [sb(es, "hb%d" % i, [128, D], BF16) for i in range(3)]
        junk = sb(es, "junk", [128, D], BF16)
        grep = sb(es, "grep", [128, D], F32)
        stt = [sb(es, "stt%d" % i, [128, 4], F32) for i in range(3)]
        bxt = [cx.buf("xt%d" % i) for i in range(3)]
        bhb = [cx.buf("hb%d" % i) for i in range(3)]
        bjunk = cx.buf("junk")
        bgrep = cx.buf("grep")
        bst = [cx.buf("stt%d" % i) for i in range(3)]
        cx.dma("act", grep[:, :], A["g_attn"].to_broadcast([128, D]), [bW], [bgrep])
        def p1_front(t):
            s = t % 3
            cx.dma("sp", xt[s][:, :], A["x"][t * 128:(t + 1) * 128, :], [bX], [bxt[s]])
            cx.op("act", [bxt[s]], [bjunk, bst[s]], lambda e: e.activation(out=junk[:, :], in_=xt[s][:, :], func=AF.Square, accum_out=stt[s][:, 0:1]))
            cx.op("act", [bst[s], bC], [bst[s]], lambda e: e.activation(out=stt[s][:, 1:2], in_=stt[s][:, 0:1], func=AF.Ln, scale=1.0 / D, bias=eps_t[:, 0:1]))
            cx.op("act", [bst[s]], [bst[s]], lambda e: e.activation(out=stt[s][:, 2:3], in_=stt[s][:, 1:2], func=AF.Exp, scale=-0.5))
            cx.op("dve", [bxt[s], bst[s], bgrep], [bhb[s]], lambda e: e.scalar_tensor_tensor(out=hb[s][:, :], in0=xt[s][:, :], scalar=stt[s][:, 2:3], in1=grep[:, :], op0=ALU.mult, op1=ALU.mult))

        def p1_back(t):
            s = t % 3
            for half in range(2):
                bk = 2 * (t % 2) + half
                pbf = pb[bk].bitcast(BF16)
                for k in range(8):
                    kc = half * 8 + k
                    cx.op("pe", [bhb[s], bC], [bpb[bk]], lambda e: e.transpose(pbf[:, k * 128:(k + 1) * 128], hb[s][:, kc * 128:(kc + 1) * 128], ident_bf[:, :]))
                dst = HT[:, half * 8:(half + 1) * 8, t * 128:(t + 1) * 128]
                srcv = pbf[:, :].rearrange("p (k c) -> p k c", k=8)
                if half == 0:
                    cx.op("act", [bpb[bk]], [bHT[0]], lambda e: e.activation(out=dst, in_=srcv, func=AF.Copy))
                else:
                    cx.op("dve", [bpb[bk]], [bHT[1]], lambda e: e.tensor_copy(out=dst, in_=srcv))

        p1_front(0)
        for t in range(16):
            if t + 1 < 16:
                p1_front(t + 1)
            p1_back(t)
        cx.barrier()
    esP0.close()

    with ExitStack() as es:
        zt = sb(es, "zt", [128, 4, D], BF16)
        bzt = cx.buf("zt")
        cx.op("pool", [], [bzt], lambda e: e.memset(zt[:, :, :], 0.0))
        XSz = XS.rearrange("(c p r) d -> c p r d", p=128, r=4)
        for c in range(NSLOT // 512):
            cx.dma("act", XSz[c], zt[:, :, :], [bzt], [bXS], join=True)
        wsl = [sb(es, "wsl%d" % i, [128, 2048], BF16) for i in range(4)]
        stg = [sb(es, "stg%d" % i, [128, S], BF16) for i in range(2)]
        bws = [cx.buf("wsl%d" % i) for i in range(4)]
        bstg = [cx.buf("stg%d" % i) for i in range(2)]
        n = 0
        for blk in range(NBLK):
            ws = wsl[blk % 4]
            cx.dma("pool", ws[:, :], A["wfm"][blk], [bW], [bws[blk % 4]])
            for tc in range(4):
                bk = n % 4
                for kc in range(16):
                    cx.op("pe", [bws[blk % 4]] + bHT, [bpb[bk]], lambda e: e.matmul(pb[bk][:, :], lhsT=ws[:, kc * 128:(kc + 1) * 128], rhs=HT[:, kc, tc * 512:(tc + 1) * 512], start=(kc == 0), stop=(kc == 15)))
                dst = stg[blk % 2][:, tc * 512:(tc + 1) * 512]
                if n % 2 == 0:
                    cx.op("act", [bpb[bk]], [bstg[blk % 2]], lambda e: e.activation(out=dst, in_=pb[bk][:, :], func=AF.Copy))
                else:
                    cx.op("dve", [bpb[bk]], [bstg[blk % 2]], lambda e: e.tensor_copy(out=dst, in_=pb[bk][:, :]))
                n += 1
            cx.dma("sp", QK[blk], stg[blk % 2][:, :], [bstg[blk % 2]], [bQK[blk]], sem_buf=bstg[blk % 2])
        wtb = [sb(es, "wtb%d" % i, [128, 16, 512], BF16) for i in range(2)]
        st2 = [sb(es, "st2%d" % i, [128, 512], BF16) for i in range(2)]
        bwt = [cx.buf("wtb%d" % i) for i in range(2)]
        bst2 = [cx.buf("st2%d" % i) for i in range(2)]
        n = 0
        for cb in range(4):
            wt = wtb[cb % 2]
            for hf in range(2):
                cx.dma("pool", wt[:, hf * 8:(hf + 1) * 8, :], A["wtm"][cb, :, hf * 8:(hf + 1) * 8, :], [bW], [bwt[cb % 2]], join=(hf > 0))
            for t in range(16):
                bk = 4 + n % 4
                for kc in range(16):
                    cx.op("pe", [bwt[cb % 2]] + bHT, [bpb[bk]], lambda e: e.matmul(pb[bk][:, :], lhsT=HT[:, kc, t * 128:(t + 1) * 128], rhs=wt[:, kc, :], start=(kc == 0), stop=(kc == 15)))
                if n % 2 == 0:
                    cx.op("act", [bpb[bk]], [bst2[n % 2]], lambda e: e.activation(out=st2[n % 2][:, :], in_=pb[bk][:, :], func=AF.Copy))
                else:
                    cx.op("dve", [bpb[bk]], [bst2[n % 2]], lambda e: e.tensor_copy(out=st2[n % 2][:, :], in_=pb[bk][:, :]))
                cx.dma("act", VT[t * 128:(t + 1) * 128, cb * 512:(cb + 1) * 512], st2[n % 2][:, :], [bst2[n % 2]], [bVT], join=True, sem_buf=bst2[n % 2])
                n += 1
        cx.barrier()

    if stop_after == "P2":
        return _finish(nc, cx, es0, esA, out_d, bOut)

    with ExitStack() as es:
        qT = [sb(es, "qT%d" % i, [128, S], BF16) for i in range(2)]
        kz = [[sb(es, "kz%d_%d" % (i, c), [128, S], BF16) for c in range(2)] for i in range(2)]
        vt = [sb(es, "vt%d" % i, [128, 16, 128], BF16) for i in range(2)]
        PT = [sb(es, "PT%d" % i, [128, 2, 256], BF16) for i in range(4)]
        tmpn = [sb(es, "tmpn%d" % i, [128, 2, 256], F32) for i in range(2)]
        fa = [sb(es, "fa%d" % i, [128, 2, 256], F32) for i in range(2)]
        fb = [sb(es, "fb%d" % i, [128, 256], F32) for i in range(2)]
        fc = [sb(es, "fc%d" % i, [128, 256], F32) for i in range(2)]
        fd = [sb(es, "fd%d" % i, [128, 256], F32) for i in range(2)]
        bq = [cx.buf("q%d" % i) for i in range(2)]
        bk_ = [cx.buf("k%d" % i) for i in range(2)]
        bv = [cx.buf("v%d" % i) for i in range(2)]
        bPT = [cx.buf("PT%d" % i) for i in range(4)]
        btn = [cx.buf("tmpn%d" % i) for i in range(2)]
        bfa = [cx.buf("fa%d" % i) for i in range(2)]
        bfb = [cx.buf("fb%d" % i) for i in range(2)]
        bfc = [cx.buf("fc%d" % i) for i in range(2)]
        bfd = [cx.buf("fd%d" % i) for i in range(2)]
        VTv = VT.rearrange("(t p) c -> p t c", p=128)
        p3 = [pb[i].rearrange("p (c q) -> p c q", c=2) for i in range(8)]
        bslot = [cx.buf("sslot%d" % i) for i in range(4)]
        st = {"s": 0, "near": 0, "pend": None, "pt": 0}

        for i in range(2):
            cx.op("pool", [], [bk_[i]], lambda e: e.memset(kz[i][0][64:128, :], 0.0))
            cx.op("pool", [], [bk_[i]], lambda e: e.memset(kz[i][1][0:64, :], 0.0))

        def load_head(h):
            s = h % 2
            cx.dma("sp", qT[s][:, :], QK[h], [bQK[h]], [bq[s]])
            cx.dma("sp", kz[s][0][0:64, :], QK[8 + h][0:64, :], [bQK[8 + h]], [bk_[s]])
            cx.dma("sp", kz[s][1][64:128, :], QK[8 + h][64:128, :], [bQK[8 + h]], [bk_[s]], join=True)
            cx.dma("act", vt[s][:, :, :], VTv[:, :, h * 128:(h + 1) * 128], [bVT], [bv[s]])

        load_head(0)
        chn = 0
        for h in range(NH):
            s = h % 2
            if h + 1 < NH:
                load_head(h + 1)
            for ch in range(8):
                q0 = ch * 256
                nj = 2 * ch + 2
                par = chn % 2
                ot, su = p3[4 + par], p3[6 + par]
                bot_, bsu = bpb[4 + par], bpb[6 + par]
                sbank = {}

                def s_mm(j):
                    co = max(0, j * 128 - q0)
                    slot = st["s"] % 4
                    st["s"] += 1
                    sbank[j] = slot
                    for c in range(2):
                        cx.op("pe", [bq[s], bk_[s]], [bpb[slot]], lambda e: e.matmul(
                            p3[slot][:, c, co:256], lhsT=kz[s][c][:, j * 128:(j + 1) * 128],
                            rhs=qT[s][:, q0 + co:q0 + 256], start=True, stop=True))

                LA = 3
                for jj in range(min(LA, nj)):
                    s_mm(jj)
                for j in range(nj):
                    if j + LA < nj:
                        s_mm(j + LA)
                    co = max(0, j * 128 - q0)
                    ne = min(j * 128 + 256, q0 + 256) - q0
                    fs = max(ne, co)
                    slot = sbank[j]
                    pt = PT[st["pt"] % 4]
                    bpt = bPT[st["pt"] % 4]
                    st["pt"] += 1
                    sview = p3[slot]
                    if ne > co:
                        w = ne - co
                        b0 = q0 + co - j * 128
                        tn = tmpn[st["near"] % 2]
                        btn_ = btn[st["near"] % 2]
                        st["near"] += 1
                        cx.op("dve", [bpb[slot], bBT], [btn_], lambda e: e.scalar_tensor_tensor(
                            out=tn[:, :, 0:w], in0=sview[:, :, co:ne], scalar=0.125,
                            in1=BT[:, h, b0:b0 + w].unsqueeze(1).to_broadcast([128, 2, w]), op0=ALU.mult, op1=ALU.add))
                        cx.op("act", [btn_], [bpt], lambda e: e.activation(out=pt[:, :, co:ne], in_=tn[:, :, 0:w], func=AF.Exp))
                    if fs < 256:
                        cx.op("act", [bpb[slot], bBT], [bpt], lambda e: e.activation(
                            out=pt[:, :, fs:256], in_=sview[:, :, fs:256], func=AF.Exp, scale=0.125, bias=cfar[:, h:h + 1]))
                    if co == 0:
                        ptf = pt[:, :, :].rearrange("p c q -> p (c q)")
                        cx.op("pe", [bv[s], bpt], [bot_], lambda e: e.matmul(
                            pb[4 + par][:, :], lhsT=vt[s][:, j, :], rhs=ptf, start=(j == 0), stop=(j == nj - 1)))
                        cx.op("pe", [bC, bpt], [bsu], lambda e: e.matmul(
                            pb[6 + par][:, :], lhsT=ones_bf[:, :], rhs=ptf, start=(j == 0), stop=(j == nj - 1)))
                    else:
                        for c in range(2):
                            cx.op("pe", [bv[s], bpt], [bot_], lambda e: e.matmul(
                                ot[:, c, co:256], lhsT=vt[s][:, j, :], rhs=pt[:, c, co:256], start=(j == 0), stop=(j == nj - 1 and c == 1)))
                            cx.op("pe", [bC, bpt], [bsu], lambda e: e.matmul(
                                su[:, c, co:256], lhsT=ones_bf[:, :], rhs=pt[:, c, co:256], start=(j == 0), stop=(j == nj - 1 and c == 1)))
                    if j == min(1, nj - 1) and st["pend"] is not None:
                        st["pend"]()
                        st["pend"] = None
                fa_, fb_, fc_, fd_ = fa[par], fb[par], fc[par], fd[par]
                cx.op("act", [bsu], [bfa[par]], lambda e: e.activation(out=fa_[:, :, :], in_=su[:, :, :], func=AF.Ln))
                cx.op("act", [bfa[par]], [bfa[par]], lambda e: e.activation(out=fa_[:, :, :], in_=fa_[:, :, :], func=AF.Exp, scale=-1.0))
                cx.op("dve", [bot_, bfa[par]], [bfa[par]], lambda e: e.tensor_tensor(out=fa_[:, :, :], in0=ot[:, :, :], in1=fa_[:, :, :], op=ALU.mult))
                cx.op("dve", [bfa[par], bLam], [bfb[par]], lambda e: e.scalar_tensor_tensor(
                    out=fb_[:, :], in0=fa_[:, 1, :], scalar=nlam[:, 0:1], in1=fa_[:, 0, :], op0=ALU.mult, op1=ALU.add))
                cx.op("dve", [bfb[par]], [bfc[par]], lambda e: e.tensor_tensor(out=fc_[:, :], in0=fb_[:, :], in1=fb_[:, :], op=ALU.mult))

                def fin2(h=h, q0=q0, par=par, fb_=fb_, fc_=fc_, fd_=fd_):
                    msv = pb[6 + par][:, 0:256]
                    cx.op("pe", [bfc[par], bC], [bpb[6 + par]], lambda e: e.matmul(msv, lhsT=ones_f[:, :], rhs=fc_[:, :], start=True, stop=True))
                    cx.op("act", [bpb[6 + par], bC], [bfd[par]], lambda e: e.activation(out=fd_[:, :], in_=msv, func=AF.Ln, bias=eps_t[:, 0:1]))
                    cx.op("act", [bfd[par]], [bfd[par]], lambda e: e.activation(out=fd_[:, :], in_=fd_[:, :], func=AF.Exp, scale=-0.5))
                    cx.op("dve", [bfb[par], bfd[par], bSm], [bMT], lambda e: e.scalar_tensor_tensor(
                        out=MT[:, h, q0:q0 + 256], in0=fb_[:, :], scalar=g08[:, 0:1], in1=fd_[:, :], op0=ALU.mult, op1=ALU.mult))

                st["pend"] = fin2
                chn += 1
        st["pend"]()
        st["pend"] = None
        cx.barrier()

    if stop_after == "P3":
        if debug:
            cx.dma("sp", MTD[:, :, :], MT[:, :, :], [bMT], [bOut])
        return _finish(nc, cx, es0, esA, out_d, bOut)

    with ExitStack() as es:
        ab = [sb(es, "ab%d" % i, [128, S], BF16) for i in range(4)]
        cos2 = sb(es, "cos2", [128, S], F32)
        sin2 = sb(es, "sin2", [128, S], F32)
        qdec = sb(es, "qdec", [128, S], F32)
        tA = sb(es, "tA", [128, S], F32)
        tB = sb(es, "tB", [128, S], F32)
        qr2 = [sb(es, "qr%d" % i, [128, S], BF16) for i in range(2)]
        kr2 = [sb(es, "kr%d" % i, [128, S], BF16) for i in range(2)]
        vt4 = [sb(es, "rvt%d" % i, [128, 16, 128], BF16) for i in range(4)]
        rg4 = [sb(es, "rg%d" % i, [128, S], BF16) for i in range(4)]
        bcj = sb(es, "bcj", [128, 8, 19], F32)
        mqk = sb(es, "mqk", [128, 128], F32)
        PT = [sb(es, "rPT%d" % i, [128, 512], BF16) for i in range(4)]
        fa = [sb(es, "rfa%d" % i, [128, 512], F32) for i in range(2)]
        fb = [sb(es, "rfb%d" % i, [128, 512], F32) for i in range(2)]
        fc = [sb(es, "rfc%d" % i, [128, 512], F32) for i in range(2)]
        fd = [sb(es, "rfd%d" % i, [128, 512], F32) for i in range(2)]
        bab = [cx.buf("ab%d" % i) for i in range(4)]
        brope = cx.buf("rope")
        bqd = cx.buf("qdec")
        btA, btB = cx.buf("tA"), cx.buf("tB")
        bqr2 = [cx.buf("qr%d" % i) for i in range(2)]
        bkr2 = [cx.buf("kr%d" % i) for i in range(2)]
        bv4 = [cx.buf("rv%d" % i) for i in range(4)]
        brg4 = [cx.buf("rg%d" % i) for i in range(4)]
        bPT = [cx.buf("rPT%d" % i) for i in range(4)]
        bfa = [cx.buf("rfa%d" % i) for i in range(2)]
        bfb = [cx.buf("rfb%d" % i) for i in range(2)]
        bfc = [cx.buf("rfc%d" % i) for i in range(2)]
        bfd = [cx.buf("rfd%d" % i) for i in range(2)]
        cx.dma("sp", cos2[:, :], A["cos2"][:, :], [bW], [brope])
        cx.dma("sp", sin2[:, :], A["sin2"][:, :], [bW], [brope], join=True)
        cx.dma("sp", bcj[:, :, :], A["bcj"][:, :, :], [bW], [brope], join=True)
        cx.dma("sp", mqk[:, :], A["mqk"][:, :], [bW], [brope], join=True)
        VTv = VT.rearrange("(t p) c -> p t c", p=128)
        st = {"s": 0, "alt": 0, "pend": None}
        chn = 0
        def prep_pair(hp, eng="pool"):
            par = hp % 2
            for i in range(4):
                cx.dma("sp", ab[i][:, :], QK[16 + hp * 4 + i], [bQK[16 + hp * 4 + i]], [bab[i]])
            cx.dma("sp", qdec[:, :], A["qdec"][hp], [bW], [bqd])
            for sub in range(2):
                h = 2 * hp + sub
                ix = 2 * par + sub
                cx.dma("act", vt4[ix][:, :, :], VTv[:, :, 1024 + h * 128:1024 + (h + 1) * 128], [bVT], [bv4[ix]])
                cx.dma("act", rg4[ix][:, :], QK[32 + h], [bQK[32 + h]], [brg4[ix]])
                cx.op("act", [brg4[ix]], [brg4[ix]], lambda e: e.activation(out=rg4[ix][:, :], in_=rg4[ix][:, :], func=AF.Silu))
            qr, kr = qr2[par], kr2[par]
            cx.op(eng, [bab[0], brope], [btA], lambda e: e.tensor_tensor(out=tA[:, :], in0=ab[0][:, :], in1=cos2[:, :], op=ALU.mult))
            cx.op(eng, [bab[1], brope], [btB], lambda e: e.tensor_tensor(out=tB[:, :], in0=ab[1][:, :], in1=sin2[:, :], op=ALU.mult))
            cx.op(eng, [btA, btB], [btA], lambda e: e.tensor_tensor(out=tA[:, :], in0=tA[:, :], in1=tB[:, :], op=ALU.add))
            cx.op(eng, [btA, bqd], [bqr2[par]], lambda e: e.tensor_tensor(out=qr[:, :], in0=tA[:, :], in1=qdec[:, :], op=ALU.mult))
            cx.op(eng, [bab[2], brope], [btA], lambda e: e.tensor_tensor(out=tA[:, :], in0=ab[2][:, :], in1=cos2[:, :], op=ALU.mult))
            cx.op(eng, [bab[3], brope], [btB], lambda e: e.tensor_tensor(out=tB[:, :], in0=ab[3][:, :], in1=sin2[:, :], op=ALU.mult))
            cx.op(eng, [btA, btB], [bkr2[par]], lambda e: e.tensor_tensor(out=kr[:, :], in0=tA[:, :], in1=tB[:, :], op=ALU.add))

        prep_pair(0, "dve")
        for hp in range(4):
            if hp + 1 < 4:
                prep_pair(hp + 1)
            qr, kr = qr2[hp % 2], kr2[hp % 2]
            bqr, bkr = bqr2[hp % 2], bkr2[hp % 2]
            vt = [vt4[2 * (hp % 2)], vt4[2 * (hp % 2) + 1]]
            rg = [rg4[2 * (hp % 2)], rg4[2 * (hp % 2) + 1]]
            bv = [bv4[2 * (hp % 2)], bv4[2 * (hp % 2) + 1]]
            brg = [brg4[2 * (hp % 2)], brg4[2 * (hp % 2) + 1]]
            for sub in range(2):
                h = 2 * hp + sub
                s = sub
                p0 = sub * 64
                for qc in range(4):
                    q0 = qc * 512
                    nj = 4 * qc + 4
                    par = chn % 2
                    bot_ = bpb[4 + par]
                    otb = pb[4 + par]
                    sbank = {}

                    def s_mm(j):
                        co = max(0, j * 128 - q0)
                        bank = st["s"] % 4
                        st["s"] += 1
                        sbank[j] = bank
                        cx.op("pe", [bqr, bkr], [bpb[bank]], lambda e: e.matmul(
                            pb[bank][:, co:512], lhsT=kr[p0:p0 + 64, j * 128:(j + 1) * 128],
                            rhs=qr[p0:p0 + 64, q0 + co:q0 + 512], start=True, stop=True))

                    LA = 3
                    for jj in range(min(LA, nj)):
                        s_mm(jj)
                    for j in range(nj):
                        if j + LA < nj:
                            s_mm(j + LA)
                        co = max(0, j * 128 - q0)
                        bank = sbank[j]
                        pt = PT[bank]
                        bpt = bPT[bank]
                        mi = (q0 - 128 * j) // 128 + 3
                        sc = bcj[:, h, mi:mi + 1]
                        fs = co
                        if j * 128 >= q0:
                            cx.op("dve", [bpb[bank], brope], [bpt], lambda e: e.scalar_tensor_tensor(
                                out=pt[:, co:co + 128], in0=pb[bank][:, co:co + 128], scalar=sc, in1=mqk[:, :], op0=ALU.mult, op1=ALU.mult))
                            fs = co + 128
                        if fs < 512:
                            if st["alt"] % 2 == 0:
                                cx.op("act", [bpb[bank], brope], [bpt], lambda e: e.activation(out=pt[:, fs:512], in_=pb[bank][:, fs:512], func=AF.Copy, scale=sc))
                            else:
                                cx.op("dve", [bpb[bank], brope], [bpt], lambda e: e.tensor_scalar(out=pt[:, fs:512], in0=pb[bank][:, fs:512], scalar1=sc, scalar2=None, op0=ALU.mult))
                            st["alt"] += 1
                        cx.op("pe", [bv[s], bpt], [bot_], lambda e: e.matmul(
                            otb[:, co:512], lhsT=vt[s][:, j, :], rhs=pt[:, co:512], start=(j == 0), stop=(j == nj - 1)))
                        if j == min(1, nj - 1) and st["pend"] is not None:
                            st["pend"]()
                            st["pend"] = None
                    fa_, fb_, fc_, fd_ = fa[par], fb[par], fc[par], fd[par]
                    cx.op("act", [bot_], [bfa[par]], lambda e: e.activation(out=fa_[:, :], in_=otb[:, :], func=AF.Copy))
                    cx.op("dve", [bfa[par]], [bfb[par]], lambda e: e.tensor_tensor(out=fb_[:, :], in0=fa_[:, :], in1=fa_[:, :], op=ALU.mult))

                    def fin2(h=h, s=s, q0=q0, par=par, fa_=fa_, fb_=fb_, fc_=fc_, fd_=fd_):
                        cx.op("pe", [bfa[par], bC], [bpb[6]], lambda e: e.matmul(pb[6][:, :], lhsT=ones_f[:, :], rhs=fa_[:, :], start=True, stop=True))
                        cx.op("pe", [bfb[par], bC], [bpb[7]], lambda e: e.matmul(pb[7][:, :], lhsT=ones_f[:, :], rhs=fb_[:, :], start=True, stop=True))
                        cx.op("act", [bpb[6]], [bfc[par]], lambda e: e.activation(out=fc_[:, :], in_=pb[6][:, :], func=AF.Copy))
                        cx.op("dve", [bfc[par]], [bfd[par]], lambda e: e.tensor_tensor(out=fd_[:, :], in0=fc_[:, :], in1=fc_[:, :], op=ALU.mult))
                        cx.op("dve", [bpb[7], bfd[par]], [bfd[par]], lambda e: e.tensor_tensor(out=fd_[:, :], in0=pb[7][:, :], in1=fd_[:, :], op=ALU.subtract))
                        cx.op("dve", [bfd[par]], [bfd[par]], lambda e: e.tensor_scalar(out=fd_[:, :], in0=fd_[:, :], scalar1=0.0, scalar2=None, op0=ALU.max))
                        cx.op("act", [bfd[par], bC], [bfd[par]], lambda e: e.activation(out=fd_[:, :], in_=fd_[:, :], func=AF.Ln, bias=eps_t[:, 0:1]))
                        cx.op("act", [bfd[par]], [bfd[par]], lambda e: e.activation(out=fd_[:, :], in_=fd_[:, :], func=AF.Exp, scale=-0.5))
                        cx.op("dve", [bfa[par], bfc[par]], [bfa[par]], lambda e: e.tensor_tensor(out=fa_[:, :], in0=fa_[:, :], in1=fc_[:, :], op=ALU.subtract))
                        cx.op("dve", [bfa[par], bfd[par], bSm], [bfa[par]], lambda e: e.scalar_tensor_tensor(
                            out=fa_[:, :], in0=fa_[:, :], scalar=gng[:, h:h + 1], in1=fd_[:, :], op0=ALU.mult, op1=ALU.mult))
                        cx.op("dve", [bfa[par], brg[s]], [bMT], lambda e: e.tensor_tensor(
                            out=MT[:, 8 + h, q0:q0 + 512], in0=fa_[:, :], in1=rg[s][:, q0:q0 + 512], op=ALU.mult))

                    st["pend"] = fin2
                    chn += 1
            if st["pend"] is not None:
                st["pend"]()
                st["pend"] = None
        cx.barrier()

    if stop_after == "P4":
        if debug:
            cx.dma("sp", MTD[:, :, :], MT[:, :, :], [bMT], [bOut])
        return _finish(nc, cx, es0, esA, out_d, bOut)

    with ExitStack() as es:
        wob = [sb(es, "wob%d" % i, [128, 16, 512], BF16) for i in range(2)]
        xs_t = [sb(es, "xs_t%d" % i, [128, 512], F32) for i in range(2)]
        x1p = [sb(es, "x1p%d" % i, [128, 512], F32) for i in range(2)]
        bwo = [cx.buf("wob%d" % i) for i in range(2)]
        bxs = [cx.buf("xs_t%d" % i) for i in range(2)]
        bx1p = [cx.buf("x1p%d" % i) for i in range(2)]
        wov = A["wout"].rearrange("(kc p) f -> p kc f", p=128)
        n = 0
        for dc in range(4):
            wo = wob[dc % 2]
            for hf in range(2):
                cx.dma("pool", wo[:, hf * 8:(hf + 1) * 8, :], wov[:, hf * 8:(hf + 1) * 8, dc * 512:(dc + 1) * 512], [bW], [bwo[dc % 2]], join=(hf > 0))
            for t in range(16):
                bk = n % 4
                s = n % 2
                cx.dma("sp", xs_t[s][:, :], A["x"][t * 128:(t + 1) * 128, dc * 512:(dc + 1) * 512], [bX], [bxs[s]])
                for kc in range(16):
                    cx.op("pe", [bwo[dc % 2], bMT], [bpb[bk]], lambda e: e.matmul(pb[bk][:, :], lhsT=MT[:, kc, t * 128:(t + 1) * 128], rhs=wo[:, kc, :], start=(kc == 0), stop=(kc == 15)))
                cx.op("dve", [bpb[bk], bxs[s]], [bx1p[s]], lambda e: e.tensor_tensor(out=x1p[s][:, :], in0=pb[bk][:, :], in1=xs_t[s][:, :], op=ALU.add))
                cx.dma("act", X1[t * 128:(t + 1) * 128, dc * 512:(dc + 1) * 512], x1p[s][:, :], [bx1p[s]], [bX1], join=True, sem_buf=bx1p[s])
                n += 1
        cx.barrier()
    esA.close()

    if stop_after == "P5":
        return _finish(nc, cx, es0, None, out_d, bOut)

    _phase_b(nc, cx, A, sb, pb, bpb, bW, bC, X1, bX1, XS, bXS, YS, bYS, out_d, bOut,
             ident_f, ident_bf, ones_bf, eps_t, gates, rows_i, bGates, bRows, RD, debug, stop_after)
    return _finish(nc, cx, es0, None, out_d, bOut)


def _finish(nc, cx, es0, esA, out_d, bOut):
    cx.barrier()
    return nc


def _phase_b(nc, cx, A, sb, pb, bpb, bW, bC, X1, bX1, XS, bXS, YS, bYS, out_d, bOut,
             ident_f, ident_bf, ones_bf, eps_t, gates, rows_i, bGates, bRows, RD, debug, stop_after):
    BIG = 20000.0
    bnd_reg = nc.gpsimd.alloc_register("bnd")
    nc.gpsimd.reg_mov(bnd_reg, NSLOT - 1)
    with ExitStack() as es:
        x1t = [sb(es, "x1t%d" % i, [128, D], F32) for i in range(2)]
        h2f = [sb(es, "h2f%d" % i, [128, D], F32) for i in range(2)]
        h2b = sb(es, "h2b", [128, 16, D], BF16)
        junk = sb(es, "junk6", [128, D], BF16)
        h2T = [sb(es, "h2T%d" % i, [128, 16, 128], F32) for i in range(2)]
        wr = sb(es, "wr", [128, 16, 36], F32)
        gffn = sb(es, "gffn", [128, D], F32)
        rbias = sb(es, "rbias", [128, 36], F32)
        Mall = sb(es, "Mall", [128, 16, 32], BF16)
        lstrict = sb(es, "lstrict", [128, 128], BF16)
        iota_e = sb(es, "iota_e", [128, 32], F32)
        LG = sb(es, "LG", [128, 16, 36], F32)
        stA = sb(es, "stA", [128, 16, 4], F32)
        gmax = sb(es, "gmax", [128, 16], F32)
        t4 = sb(es, "t4", [128, 16, 4], F32)
        gmk = sb(es, "gmk", [128, 16, 4], F32)
        sc16 = sb(es, "sc16", [128, 12, 16], F32)
        mi = sb(es, "mi", [128, 16, 32], F32)
        mi2 = sb(es, "mi2", [128, 16, 32], F32)
        M1 = sb(es, "M1", [128, 16, 32], F32)
        M2 = sb(es, "M2", [128, 16, 32], F32)
        tt = sb(es, "tt6", [128, 16, 32], F32)
        csa = sb(es, "csa", [128, 16, 32], F32)
        pre = sb(es, "pre", [128, 16, 32], F32)
        cnt = sb(es, "cnt", [128, 16, 32], F32)
        rowf = sb(es, "rowf", [128, 16, 2], F32)
        bx1t = [cx.buf("x1t%d" % i) for i in range(2)]
        bh2f = [cx.buf("h2f%d" % i) for i in range(2)]
        bh2b = [cx.buf("h2b%d" % i) for i in range(16)]
        bjunk = cx.buf("junk6")
        bh2T = [cx.buf("h2T%d" % i) for i in range(2)]
        bK = cx.buf("p6const")
        bLG = cx.buf("LG")
        bstA = cx.buf("stA")
        bs = cx.buf("p6small")
        cx.dma("sp", wr[:, :, :], A["wr"][:, :, :], [bW], [bK])
        cx.dma("sp", gffn[:, :], A["g_ffn"].to_broadcast([128, D]), [bW], [bK], join=True)
        cx.dma("sp", rbias[:, :], A["rbias"].to_broadcast([128, 36]), [bW], [bK], join=True)
        cx.dma("sp", lstrict[:, :], A["lstrict"][:, :], [bW], [bK], join=True)
        cx.dma("sp", iota_e[:, :], A["iota_e"][:, :], [bW], [bK], join=True)
        def p6_front(t):
            s = t % 2
            cx.dma("sp", x1t[s][:, :], X1[t * 128:(t + 1) * 128, :], [bX1], [bx1t[s]])
            cx.op("act", [bx1t[s]], [bjunk, bstA], lambda e: e.activation(out=junk[:, :], in_=x1t[s][:, :], func=AF.Square, accum_out=stA[:, t, 0:1]))
            cx.op("act", [bstA, bC], [bstA], lambda e: e.activation(out=stA[:, t, 1:2], in_=stA[:, t, 0:1], func=AF.Ln, scale=1.0 / D, bias=eps_t[:, 0:1]))
            cx.op("act", [bstA], [bstA], lambda e: e.activation(out=stA[:, t, 2:3], in_=stA[:, t, 1:2], func=AF.Exp, scale=-0.5))
            cx.op("dve", [bx1t[s], bstA, bK], [bh2f[s]], lambda e: e.scalar_tensor_tensor(out=h2f[s][:, :], in0=x1t[s][:, :], scalar=stA[:, t, 2:3], in1=gffn[:, :], op0=ALU.mult, op1=ALU.mult))
            cx.op("act", [bh2f[s]], [bh2b[t]], lambda e: e.activation(out=h2b[:, t, :], in_=h2f[s][:, :], func=AF.Copy))

        def p6_back(t):
            s = t % 2
            for g4 in range(4):
                bk = 4 * s + g4
                for k in range(4):
                    kc = g4 * 4 + k
                    cx.op("pe", [bh2f[s], bC], [bpb[bk]], lambda e: e.transpose(pb[bk][:, k * 128:(k + 1) * 128], h2f[s][:, kc * 128:(kc + 1) * 128], ident_f[:, :]))
                dst = h2T[s][:, g4 * 4:(g4 + 1) * 4, :]
                srcv = pb[bk].rearrange("p (k c) -> p k c", k=4)
                if g4 % 2 == 0:
                    cx.op("act", [bpb[bk]], [bh2T[s]], lambda e: e.activation(out=dst, in_=srcv, func=AF.Copy))
                else:
                    cx.op("dve", [bpb[bk]], [bh2T[s]], lambda e: e.tensor_copy(out=dst, in_=srcv))
            bk = 4 * s
            for kc in range(16):
                cx.op("pe", [bh2T[s], bK], [bpb[bk]], lambda e: e.matmul(pb[bk][:, 0:36], lhsT=h2T[s][:, kc, :], rhs=wr[:, kc, :], start=(kc == 0), stop=(kc == 15)))
            cx.op("dve", [bpb[bk], bK], [bLG], lambda e: e.tensor_tensor(out=LG[:, t, :], in0=pb[bk][:, 0:36], in1=rbias[:, :], op=ALU.add))

        p6_front(0)
        for t in range(16):
            if t + 1 < 16:
                p6_front(t + 1)
            p6_back(t)
        lg4 = LG[:, :, 0:4]
        lgin = LG[:, :, 4:36].rearrange("p t (g e) -> p t g e", g=4)
        S_ = lambda i: sc16[:, i, :]
        bc4 = lambda ap: ap.unsqueeze(2).to_broadcast([128, 16, 4])
        bc32 = lambda ap: ap.unsqueeze(2).to_broadcast([128, 16, 32])

        def dve(fn, rd=(), wr_=None):
            cx.op("dve", [bs] + list(rd), [bs if wr_ is None else wr_], fn)

        def act(fn):
            cx.op("act", [bs], [bs], fn)

        dve(lambda e: e.tensor_reduce(out=gmax[:, :], in_=lg4, axis=AX.X, op=ALU.max), rd=[bLG])
        dve(lambda e: e.tensor_tensor(out=t4[:, :, :], in0=lg4, in1=bc4(gmax[:, :]), op=ALU.subtract), rd=[bLG])
        act(lambda e: e.activation(out=t4[:, :, :], in_=t4[:, :, :], func=AF.Exp))
        dve(lambda e: e.reduce_sum(out=S_(0), in_=t4[:, :, :], axis=AX.X))
        act(lambda e: e.activation(out=S_(1), in_=S_(0), func=AF.Ln))
        act(lambda e: e.activation(out=S_(2), in_=S_(1), func=AF.Exp, scale=-1.0))
        dve(lambda e: e.tensor_tensor(out=gmk[:, :, :], in0=lg4, in1=bc4(gmax[:, :]), op=ALU.is_equal), rd=[bLG])
        dve(lambda e: e.tensor_scalar(out=gmk[:, :, :], in0=gmk[:, :, :], scalar1=-1.0, scalar2=1e9, op0=ALU.add, op1=ALU.mult))
        dve(lambda e: e.tensor_tensor(out=mi[:, :, :].rearrange("p t (g e) -> p t g e", g=4), in0=lgin,
                                      in1=gmk[:, :, :].unsqueeze(3).to_broadcast([128, 16, 4, 8]), op=ALU.add), rd=[bLG])
        dve(lambda e: e.tensor_reduce(out=S_(3), in_=mi[:, :, :], axis=AX.X, op=ALU.max))
        dve(lambda e: e.tensor_tensor(out=M1[:, :, :], in0=mi[:, :, :], in1=bc32(S_(3)), op=ALU.is_equal))
        dve(lambda e: e.scalar_tensor_tensor(out=mi2[:, :, :], in0=M1[:, :, :], scalar=-1e9, in1=mi[:, :, :], op0=ALU.mult, op1=ALU.add))
        dve(lambda e: e.tensor_reduce(out=S_(4), in_=mi2[:, :, :], axis=AX.X, op=ALU.max))
        dve(lambda e: e.tensor_tensor(out=M2[:, :, :], in0=mi2[:, :, :], in1=bc32(S_(4)), op=ALU.is_equal))
        dve(lambda e: e.tensor_tensor(out=S_(5), in0=S_(4), in1=S_(3), op=ALU.subtract))
        act(lambda e: e.activation(out=S_(5), in_=S_(5), func=AF.Exp))
        dve(lambda e: e.tensor_scalar(out=S_(6), in0=S_(5), scalar1=1.0, scalar2=None, op0=ALU.add))
        act(lambda e: e.activation(out=S_(6), in_=S_(6), func=AF.Ln))
        act(lambda e: e.activation(out=S_(6), in_=S_(6), func=AF.Exp, scale=-1.0))
        dve(lambda e: e.tensor_tensor(out=S_(7), in0=S_(5), in1=S_(6), op=ALU.mult))
        dve(lambda e: e.tensor_tensor(out=S_(6), in0=S_(6), in1=S_(2), op=ALU.mult))
        dve(lambda e: e.tensor_tensor(out=S_(7), in0=S_(7), in1=S_(2), op=ALU.mult))
        bMall = cx.buf("Mall")
        dve(lambda e: e.tensor_tensor(out=Mall[:, :, :], in0=M1[:, :, :], in1=M2[:, :, :], op=ALU.add), wr_=bMall)
        mflat = Mall[:, :, :].rearrange("p t e -> p (t e)")
        cx.op("pe", [bMall, bK], [bpb[0]], lambda e: e.matmul(pb[0][:, :], lhsT=lstrict[:, :], rhs=mflat, start=True, stop=True))
        cx.op("pe", [bMall, bC], [bpb[1]], lambda e: e.matmul(pb[1][:, :], lhsT=ones_bf[:, :], rhs=mflat, start=True, stop=True))
        dve(lambda e: e.tensor_copy(out=csa[:, :, :].rearrange("p t e -> p (t e)"), in_=pb[1][:, :]), rd=[bpb[1]])
        dve(lambda e: e.memset(pre[:, 0, :], 0.0))
        for t in range(1, 16):
            dve(lambda e: e.tensor_tensor(out=pre[:, t, :], in0=pre[:, t - 1, :], in1=csa[:, t - 1, :], op=ALU.add))
        dve(lambda e: e.tensor_tensor(out=cnt[:, :, :].rearrange("p t e -> p (t e)"), in0=pb[0][:, :], in1=pre[:, :, :].rearrange("p t e -> p (t e)"), op=ALU.add), rd=[bpb[0]])
        for k, Mk in ((0, M1), (1, M2)):
            dve(lambda e: e.tensor_tensor(out=tt[:, :, :], in0=Mk[:, :, :], in1=cnt[:, :, :], op=ALU.mult))
            dve(lambda e: e.reduce_sum(out=S_(8), in_=tt[:, :, :], axis=AX.X))
            dve(lambda e: e.tensor_tensor(out=tt[:, :, :], in0=Mk[:, :, :], in1=iota_e[:, :].unsqueeze(1).to_broadcast([128, 16, 32]), op=ALU.mult), rd=[bK])
            dve(lambda e: e.reduce_sum(out=S_(9), in_=tt[:, :, :], axis=AX.X))
            dve(lambda e: e.scalar_tensor_tensor(out=S_(10), in0=S_(9), scalar=float(CAP), in1=S_(8), op0=ALU.mult, op1=ALU.add))
            dve(lambda e: e.tensor_scalar(out=S_(11), in0=S_(8), scalar1=float(CAP), scalar2=None, op0=ALU.is_lt))
            dve(lambda e: e.scalar_tensor_tensor(out=S_(10), in0=S_(10), scalar=-BIG, in1=S_(11), op0=ALU.add, op1=ALU.mult))
            dve(lambda e: e.tensor_scalar(out=rowf[:, :, k], in0=S_(10), scalar1=BIG, scalar2=None, op0=ALU.add))
            dve(lambda e: e.tensor_tensor(out=gates[:, :, k], in0=S_(6 + k), in1=S_(11), op=ALU.mult), wr_=bGates)
        dve(lambda e: e.tensor_copy(out=rows_i[:, :, :], in_=rowf[:, :, :]), wr_=bRows)
        for t in range(16):
            for k in range(2):
                cx.dma("pool", None, None, [bh2b[t], bRows], [bXS], join=True, sem_buf=bh2b[t], fn=lambda e: e.indirect_dma_start(
                    out=XS[:, :], out_offset=bass.IndirectOffsetOnAxis(ap=rows_i[:, t, k:k + 1], axis=0),
                    in_=h2b[:, t, :], in_offset=None, bounds_check=bnd_reg, oob_is_err=False))
        cx.barrier()

    if stop_after == "P6":
        return

    with ExitStack() as es:
        NST = CAP // 128
        xtok = [sb(es, "xtok%d" % i, [128, NST, D], BF16) for i in range(2)]
        xsT = [sb(es, "xsT%d" % i, [128, 16, CAP], BF16) for i in range(2)]
        slab = [sb(es, "slab%d" % i, [128, 8, FF], BF16) for i in range(4)]
        wdb = [sb(es, "wdb%d" % i, [128, 8, D], BF16) for i in range(2)]
        sg = sb(es, "sg", [128, 8, CAP], F32)
        hT = [sb(es, "hT%d" % i, [128, 8, CAP], BF16) for i in range(2)]
        yst = [sb(es, "yst%d" % i, [128, D], BF16) for i in range(2)]
        bxtok = [cx.buf("xtok%d" % i) for i in range(2)]
        bxsT = [cx.buf("xsT%d" % i) for i in range(2)]
        bslab = [cx.buf("slab%d" % i) for i in range(4)]
        bwdb = [cx.buf("wdb%d" % i) for i in range(2)]
        bsg = [cx.buf("sg%d" % i) for i in range(4)]
        bhT = [cx.buf("hT%d" % i) for i in range(2)]
        byst = [cx.buf("yst%d" % i) for i in range(2)]
        ucount = 0
        ntr = 0
        ndn = 0
        for ex in range(NE):
            s = ex % 2
            cx.dma("sp", xtok[s][:, :, :], XS[ex * CAP:(ex + 1) * CAP, :].rearrange("(st p) d -> p st d", p=128), [bXS], [bxtok[s]])
            for st_ in range(NST):
                for half in range(2):
                    bk = 6 + ntr % 2
                    pbf = pb[bk].bitcast(BF16)
                    for k in range(8):
                        kc = half * 8 + k
                        cx.op("pe", [bxtok[s], bC], [bpb[bk]], lambda e: e.transpose(pbf[:, k * 128:(k + 1) * 128], xtok[s][:, st_, kc * 128:(kc + 1) * 128], ident_bf[:, :]))
                    dst = xsT[s][:, half * 8:(half + 1) * 8, st_ * 128:(st_ + 1) * 128]
                    src = pbf[:, :].rearrange("p (k c) -> p k c", k=8)
                    if ntr % 2 == 0:
                        cx.op("act", [bpb[bk]], [bxsT[s]], lambda e: e.activation(out=dst, in_=src, func=AF.Copy))
                    else:
                        cx.op("dve", [bpb[bk]], [bxsT[s]], lambda e: e.tensor_copy(out=dst, in_=src))
                    ntr += 1
            for (wname, isgate) in (("wg", True), ("wu", False)):
                sls = [slab[ucount % 4], slab[(ucount + 1) % 4]]
                bsls = [bslab[ucount % 4], bslab[(ucount + 1) % 4]]
                ucount += 2
                wv = A[wname][ex].rearrange("(kc p) f -> p kc f", p=128)
                for kh in range(2):
                    for hf in range(2):
                        cx.dma("pool", sls[kh][:, hf * 4:(hf + 1) * 4, :], wv[:, kh * 8 + hf * 4:kh * 8 + (hf + 1) * 4, :], [bW], [bsls[kh]], join=(hf > 0))
                for f in range(8):
                    bk = f // 2
                    c0 = (f % 2) * CAP
                    for kc in range(16):
                        sl = sls[kc // 8]
                        cx.op("pe", [bsls[kc // 8], bxsT[s]], [bpb[bk]], lambda e: e.matmul(pb[bk][:, c0:c0 + CAP], lhsT=sl[:, kc % 8, f * 128:(f + 1) * 128], rhs=xsT[s][:, kc, :], start=(kc == 0), stop=(kc == 15)))
                    if f % 2 == 1:
                        sgv = sg[:, f - 1:f + 1, :].rearrange("p a c -> p (a c)")
                        if isgate:
                            cx.op("act", [bpb[bk]], [bsg[bk]], lambda e: e.activation(out=sgv, in_=pb[bk][:, :], func=AF.Silu))
                        else:
                            dst = hT[s][:, f - 1:f + 1, :].rearrange("p a c -> p (a c)")
                            cx.op("dve", [bpb[bk], bsg[bk]], [bhT[s]], lambda e: e.tensor_tensor(out=dst, in0=pb[bk][:, :], in1=sgv, op=ALU.mult))
            wdv = A["wd"][ex].rearrange("(kc p) f -> p kc f", p=128)
            for hf in range(4):
                cx.dma("pool", wdb[s][:, hf * 2:(hf + 1) * 2, :], wdv[:, hf * 2:(hf + 1) * 2, :], [bW], [bwdb[s]], join=(hf > 0))
            for st_ in range(NST):
                ys_ = yst[st_ % 2]
                for dc in range(4):
                    bk = 4 + ndn % 2
                    for kc in range(8):
                        cx.op("pe", [bhT[s], bwdb[s]], [bpb[bk]], lambda e: e.matmul(pb[bk][:, :], lhsT=hT[s][:, kc, st_ * 128:(st_ + 1) * 128], rhs=wdb[s][:, kc, dc * 512:(dc + 1) * 512], start=(kc == 0), stop=(kc == 7)))
                    if ndn % 2 == 0:
                        cx.op("act", [bpb[bk]], [byst[st_ % 2]], lambda e: e.activation(out=ys_[:, dc * 512:(dc + 1) * 512], in_=pb[bk][:, :], func=AF.Copy))
                    else:
                        cx.op("dve", [bpb[bk]], [byst[st_ % 2]], lambda e: e.tensor_copy(out=ys_[:, dc * 512:(dc + 1) * 512], in_=pb[bk][:, :]))
                    ndn += 1
                r0 = ex * CAP + st_ * 128
                cx.dma("sp", YS[r0:r0 + 128, :], ys_[:, :], [byst[st_ % 2]], [bYS], join=True, sem_buf=byst[st_ % 2])
        cx.barrier()

    with ExitStack() as es:
        R8 = 3
        y1 = [sb(es, "y1_%d" % i, [128, D], BF16) for i in range(R8)]
        y2 = [sb(es, "y2_%d" % i, [128, D], BF16) for i in range(R8)]
        x1t = [sb(es, "x8t%d" % i, [128, D], F32) for i in range(R8)]
        acc = [sb(es, "acc8_%d" % i, [128, D], F32) for i in range(2)]
        gfin = sb(es, "gfin", [128, D], F32)
        junk = sb(es, "junk8", [128, D], BF16)
        ot = [sb(es, "ot%d" % i, [128, D], F32) for i in range(2)]
        sm = [sb(es, "sm8_%d" % i, [128, 4], F32) for i in range(2)]
        by1 = [cx.buf("y1_%d" % i) for i in range(R8)]
        by2 = [cx.buf("y2_%d" % i) for i in range(R8)]
        bx = [cx.buf("x8t%d" % i) for i in range(R8)]
        bacc = [cx.buf("acc8_%d" % i) for i in range(2)]
        bg = cx.buf("gfin")
        bj = cx.buf("junk8")
        bot = [cx.buf("ot%d" % i) for i in range(2)]
        bs = [cx.buf("sm8_%d" % i) for i in range(2)]
        cx.dma("sp", gfin[:, :], A["g_fin"].to_broadcast([128, D]), [bW], [bg])
        for i in range(R8):
            cx.op("dve", [], [by1[i]], lambda e: e.memset(y1[i][:, :], 0.0))
            cx.op("pool", [], [by2[i]], lambda e: e.memset(y2[i][:, :], 0.0))
        def fetch(t):
            s = t % R8
            for (yy, byy, k) in ((y1, by1, 0), (y2, by2, 1)):
                cx.dma("pool", None, None, [bYS, bRows], [byy[s]], fn=lambda e: e.indirect_dma_start(
                    out=yy[s][:, :], out_offset=None, in_=YS[:, :],
                    in_offset=bass.IndirectOffsetOnAxis(ap=rows_i[:, t, k:k + 1], axis=0),
                    bounds_check=bnd_reg, oob_is_err=False))
            cx.dma("sp", x1t[s][:, :], X1[t * 128:(t + 1) * 128, :], [bX1], [bx[s]])

        def p8_front(t):
            s = t % R8
            a = t % 2
            cx.op("dve", [by1[s], bx[s], bGates], [bacc[a]], lambda e: e.scalar_tensor_tensor(out=acc[a][:, :], in0=y1[s][:, :], scalar=gates[:, t, 0:1], in1=x1t[s][:, :], op0=ALU.mult, op1=ALU.add))
            cx.op("dve", [by2[s], bGates, bacc[a]], [bacc[a]], lambda e: e.scalar_tensor_tensor(out=acc[a][:, :], in0=y2[s][:, :], scalar=gates[:, t, 1:2], in1=acc[a][:, :], op0=ALU.mult, op1=ALU.add))
            if t + R8 < 16:
                fetch(t + R8)
            cx.op("act", [bacc[a]], [bj, bs[a]], lambda e: e.activation(out=junk[:, :], in_=acc[a][:, :], func=AF.Square, accum_out=sm[a][:, 0:1]))
            cx.op("act", [bs[a], bC], [bs[a]], lambda e: e.activation(out=sm[a][:, 1:2], in_=sm[a][:, 0:1], func=AF.Ln, scale=1.0 / D, bias=eps_t[:, 0:1]))
            cx.op("act", [bs[a]], [bs[a]], lambda e: e.activation(out=sm[a][:, 2:3], in_=sm[a][:, 1:2], func=AF.Exp, scale=-0.5))

        def p8_back(t):
            a = t % 2
            cx.op("dve", [bacc[a], bs[a], bg], [bot[a]], lambda e: e.scalar_tensor_tensor(out=ot[a][:, :], in0=acc[a][:, :], scalar=sm[a][:, 2:3], in1=gfin[:, :], op0=ALU.mult, op1=ALU.mult))
            cx.dma("sp", out_d[t * 128:(t + 1) * 128, :], ot[a][:, :], [bot[a]], [bOut], join=True, sem_buf=bot[a])

        for t in range(R8):
            fetch(t)
        p8_front(0)
        for t in range(16):
            if t + 1 < 16:
                p8_front(t + 1)
            p8_back(t)


def kernel(**inputs):
    wl = _layout_weights(inputs)
    cs = _consts()
    base = dict(wl)
    base.update(cs)
    x = np.asarray(inputs["x"], dtype=np.float32)
    nb = x.shape[0]
    nc = build()
    in_maps = []
    for b in range(nb):
        m = dict(base)
        m["x"] = np.ascontiguousarray(x[b])
        in_maps.append(m)
    res = run_bass_kernel_spmd(nc, in_maps, core_ids=list(range(nb)))
    return np.stack([np.asarray(r["out"], dtype=np.float32) for r in res.results], axis=0)
```
